# Optimizing a Trainium2 kernel written in Bass

```python
import numpy as np
import jax, jax.numpy as jnp
from jax import lax


D_MODEL = 1024
BATCH = 32
SEQ = 2048
DEPTH = 4

N_MIXERS = 3
GRID_W = 64
PLE_DIM = 256
RMS_EPS = 1e-6
LN_EPS = 1e-5
NEG = -1e30
HEAD_DIM = 64
MIX_WIDTH = 1024
A_WIDTH = 1024
A_GROUPS = 8
A_CHUNK = 128
B_HEADS = 16
B_KV_HEADS = 4
B_WINDOW = 128
B_BLOCK = 128
C_HEADS = 16
C_WIN_ROWS = 8
C_WIN_COLS = 16
C_QCOLS = 16
C_KCOLS = C_QCOLS + C_WIN_COLS
N_EXPERTS = 16
EXPERT_FF = 1024
EC_CAPACITY_FACTOR = 2
N_A = len(range(0, DEPTH, N_MIXERS))
N_B = len(range(1, DEPTH, N_MIXERS))
N_C = len(range(2, DEPTH, N_MIXERS))

kernel_name = 'hybrid_gmlp_swa_natten_ec_encoder'


def rms_norm(x, g):
    xf = x.astype(jnp.float32)
    y = xf * lax.rsqrt(jnp.mean(xf * xf, axis=-1, keepdims=True) + RMS_EPS)
    return (y * g.astype(jnp.float32)).astype(x.dtype)


def layer_norm(x, g):
    xf = x.astype(jnp.float32)
    mu = jnp.mean(xf, axis=-1, keepdims=True)
    var = jnp.mean(jnp.square(xf - mu), axis=-1, keepdims=True)
    return ((xf - mu) * lax.rsqrt(var + LN_EPS) * g.astype(jnp.float32)).astype(x.dtype)


def mixer_a(xn, w_in, vnorm_g, w_s, b_s):
    b_, s_, _ = xn.shape
    z = jax.nn.gelu(xn @ w_in)
    u, v = jnp.split(z, 2, axis=-1)
    v = layer_norm(v, vnorm_g)
    v = v.reshape(b_, s_ // A_CHUNK, A_CHUNK, A_GROUPS, A_WIDTH // A_GROUPS)
    s = jnp.einsum('gts,bnsgc->bntgc', w_s, v) + b_s.T[:, :, None]
    return u * s.reshape(b_, s_, A_WIDTH)


def alibi_slopes(n):
    return np.array([2.0 ** (-8.0 * (h + 1) / n) for h in range(n)], dtype=np.float32)


def mixer_b(xn, w_in, qn_g, kn_g, sink):
    b_, s_, _ = xn.shape
    grp = B_HEADS // B_KV_HEADS
    qkv = xn @ w_in
    q, k, v = jnp.split(qkv, [B_HEADS * HEAD_DIM, (B_HEADS + B_KV_HEADS) * HEAD_DIM], axis=-1)
    q = rms_norm(q.reshape(b_, s_, B_KV_HEADS, grp, HEAD_DIM), qn_g)
    k = rms_norm(k.reshape(b_, s_, B_KV_HEADS, HEAD_DIM), kn_g)
    v = v.reshape(b_, s_, B_KV_HEADS, HEAD_DIM)
    pad = ((0, 0), (B_BLOCK, B_BLOCK), (0, 0), (0, 0))
    kp = jnp.pad(k, pad)
    vp = jnp.pad(v, pad)
    span = 3 * B_BLOCK
    rel = np.arange(span)[None, :] - B_BLOCK - np.arange(B_BLOCK)[:, None]
    in_window = jnp.asarray(np.abs(rel) <= B_WINDOW)
    alibi = jnp.asarray((-alibi_slopes(B_HEADS).reshape(B_KV_HEADS, grp, 1, 1)
                         * np.abs(rel)[None, None]).astype(np.float32))
    sink_f = sink.astype(jnp.float32).reshape(B_KV_HEADS, grp)[None, :, :, None]
    scale = HEAD_DIM ** -0.5

    def block(n):
        t0 = n * B_BLOCK
        qb = lax.dynamic_slice_in_dim(q, t0, B_BLOCK, axis=1)
        kb = lax.dynamic_slice_in_dim(kp, t0, span, axis=1)
        vb = lax.dynamic_slice_in_dim(vp, t0, span, axis=1)
        s = jnp.einsum('bqhgd,bkhd->bhgqk', qb, kb,
                       preferred_element_type=jnp.float32) * scale + alibi
        kpos = t0 - B_BLOCK + jnp.arange(span)
        valid = in_window & ((kpos >= 0) & (kpos < s_))[None, :]
        s = jnp.where(valid, s, NEG)
        m = jnp.maximum(s.max(axis=-1), sink_f)
        pe = jnp.exp(s - m[..., None])
        denom = pe.sum(axis=-1) + jnp.exp(sink_f - m)
        o = jnp.einsum('bhgqk,bkhd->bqhgd', pe, vb.astype(jnp.float32))
        o = o / denom.transpose(0, 3, 1, 2)[..., None]
        return o.astype(xn.dtype)

    out = lax.map(block, jnp.arange(s_ // B_BLOCK))
    return out.transpose(1, 0, 2, 3, 4, 5).reshape(b_, s_, B_HEADS * HEAD_DIM)


def natten_tables(w):
    ncb = w // C_QCOLS
    c = np.arange(w).reshape(ncb, C_QCOLS)
    cs = np.clip(c - C_WIN_COLS // 2, 0, w - C_WIN_COLS)
    kb = np.clip(np.arange(ncb) * C_QCOLS - C_WIN_COLS // 2, 0, w - C_KCOLS)
    kcol = kb[:, None] + np.arange(C_KCOLS)[None, :]
    colmask = (kcol[:, None, :] >= cs[:, :, None]) & (kcol[:, None, :] < cs[:, :, None] + C_WIN_COLS)
    coloff = np.clip(kcol[:, None, :] - c[:, :, None] + C_WIN_COLS - 1, 0, 2 * C_WIN_COLS - 2)
    return kcol, colmask, coloff


def mixer_c(xn, w_in, qn_g, kn_g, rpb):
    b_, s_, _ = xn.shape
    rows = s_ // GRID_W
    kr_n = min(C_WIN_ROWS, rows)
    ncb = GRID_W // C_QCOLS
    qkv = xn @ w_in
    q, k, v = jnp.split(qkv, 3, axis=-1)
    q = rms_norm(q.reshape(b_, rows, GRID_W, C_HEADS, HEAD_DIM), qn_g)
    k = rms_norm(k.reshape(b_, rows, GRID_W, C_HEADS, HEAD_DIM), kn_g)
    v = v.reshape(b_, rows, GRID_W, C_HEADS, HEAD_DIM)
    kcol, colmask, coloff = natten_tables(GRID_W)
    colmask_j = jnp.asarray(colmask)[None, None, :, :, None, :]
    rpb_f = rpb.astype(jnp.float32)
    scale = HEAD_DIM ** -0.5

    def row(r):
        rs = jnp.clip(r - kr_n // 2, 0, rows - kr_n)
        qr = lax.dynamic_index_in_dim(q, r, axis=1, keepdims=False)
        qr = qr.reshape(b_, ncb, C_QCOLS, C_HEADS, HEAD_DIM)
        kr = lax.dynamic_slice_in_dim(k, rs, kr_n, axis=1)[:, :, kcol]
        vr = lax.dynamic_slice_in_dim(v, rs, kr_n, axis=1)[:, :, kcol]
        s = jnp.einsum('bnqhd,brnkhd->bhnqrk', qr, kr,
                       preferred_element_type=jnp.float32) * scale
        row_idx = rs + jnp.arange(kr_n) - r + C_WIN_ROWS - 1
        bias = rpb_f[:, row_idx][:, :, coloff]
        s = s + bias.transpose(0, 2, 3, 1, 4)[None]
        s = jnp.where(colmask_j, s, NEG)
        s = s.reshape(b_, C_HEADS, ncb, C_QCOLS, kr_n * C_KCOLS)
        pr = jax.nn.softmax(s, axis=-1).reshape(b_, C_HEADS, ncb, C_QCOLS, kr_n, C_KCOLS)
        o = jnp.einsum('bhnqrk,brnkhd->bnqhd', pr, vr.astype(jnp.float32))
        return o.reshape(b_, GRID_W, C_HEADS * HEAD_DIM).astype(xn.dtype)

    out = lax.map(row, jnp.arange(rows))
    return out.transpose(1, 0, 2, 3).reshape(b_, s_, C_HEADS * HEAD_DIM)


def expert_choice_ffn(xn, router_w, w_gate, w_up, w_down):
    b_, s_, _ = xn.shape
    cap = max(1, EC_CAPACITY_FACTOR * s_ // N_EXPERTS)
    aff = jax.nn.softmax((xn @ router_w).astype(jnp.float32), axis=-1)
    gates, idx = lax.top_k(aff.transpose(0, 2, 1), cap)
    bidx = jnp.arange(b_)[:, None, None]
    xs = xn[bidx, idx]
    hdn = jax.nn.silu(jnp.einsum('becd,edf->becf', xs, w_gate)) * jnp.einsum('becd,edf->becf', xs, w_up)
    y = jnp.einsum('becf,efd->becd', hdn, w_down) * gates[..., None].astype(xn.dtype)
    return jnp.zeros_like(xn).at[bidx, idx].add(y)


def setup_inputs(seed: int = 0) -> dict:
    key = jax.random.key(seed)
    ks = iter(jax.random.split(key, 32))
    f32 = jnp.float32
    nrm = lambda shape, s: jax.random.normal(next(ks), shape, f32) * s
    gain = lambda shape: 1.0 + nrm(shape, 0.05)
    d = D_MODEL
    return {
        'x': nrm((BATCH, SEQ, d), 1.0),
        'p': nrm((DEPTH, BATCH, SEQ, PLE_DIM), 1.0),
        'norm_mix_g': gain((DEPTH, d)),
        'norm_ffn_g': gain((DEPTH, d)),
        'w_out': nrm((DEPTH, MIX_WIDTH, d), MIX_WIDTH ** -0.5),
        'router_w': nrm((DEPTH, d, N_EXPERTS), d ** -0.5),
        'exp_w_gate': nrm((DEPTH, N_EXPERTS, d, EXPERT_FF), d ** -0.5),
        'exp_w_up': nrm((DEPTH, N_EXPERTS, d, EXPERT_FF), d ** -0.5),
        'exp_w_down': nrm((DEPTH, N_EXPERTS, EXPERT_FF, d), EXPERT_FF ** -0.5),
        'ple_norm_g': gain((DEPTH, d)),
        'ple_gate_w': nrm((DEPTH, d, d), d ** -0.5),
        'ple_proj_w': nrm((DEPTH, PLE_DIM, d), PLE_DIM ** -0.5),
        'a_w_in': nrm((N_A, d, 2 * A_WIDTH), d ** -0.5),
        'a_vnorm_g': gain((N_A, A_WIDTH)),
        'a_w_s': nrm((N_A, A_GROUPS, A_CHUNK, A_CHUNK), A_CHUNK ** -0.5),
        'a_b_s': 1.0 + nrm((N_A, A_GROUPS, A_CHUNK), 0.1),
        'b_w_in': nrm((N_B, d, (B_HEADS + 2 * B_KV_HEADS) * HEAD_DIM), d ** -0.5),
        'b_qnorm_g': gain((N_B, HEAD_DIM)),
        'b_knorm_g': gain((N_B, HEAD_DIM)),
        'b_sink': nrm((N_B, B_HEADS), 0.5),
        'c_w_in': nrm((N_C, d, 3 * C_HEADS * HEAD_DIM), d ** -0.5),
        'c_qnorm_g': gain((N_C, HEAD_DIM)),
        'c_knorm_g': gain((N_C, HEAD_DIM)),
        'c_rpb': nrm((N_C, C_HEADS, 2 * C_WIN_ROWS - 1, 2 * C_WIN_COLS - 1), 0.1),
    }


def reference(x, p, norm_mix_g, norm_ffn_g, w_out, router_w, exp_w_gate, exp_w_up, exp_w_down,
              ple_norm_g, ple_gate_w, ple_proj_w, a_w_in, a_vnorm_g, a_w_s, a_b_s,
              b_w_in, b_qnorm_g, b_knorm_g, b_sink, c_w_in, c_qnorm_g, c_knorm_g, c_rpb):
    h = x
    for i in range(DEPTH):
        kind = i % N_MIXERS
        j = i // N_MIXERS
        xn = rms_norm(h, norm_mix_g[i])
        if kind == 0:
            mix = mixer_a(xn, a_w_in[j], a_vnorm_g[j], a_w_s[j], a_b_s[j])
        elif kind == 1:
            mix = mixer_b(xn, b_w_in[j], b_qnorm_g[j], b_knorm_g[j], b_sink[j])
        else:
            mix = mixer_c(xn, c_w_in[j], c_qnorm_g[j], c_knorm_g[j], c_rpb[j])
        h = h + mix @ w_out[i]
        h = h + expert_choice_ffn(rms_norm(h, norm_ffn_g[i]), router_w[i],
                                  exp_w_gate[i], exp_w_up[i], exp_w_down[i])
        gate = jax.nn.sigmoid(rms_norm(h, ple_norm_g[i]) @ ple_gate_w[i])
        h = h + gate * (p[i] @ ple_proj_w[i])
    return h
```

```python
import numpy as np
from contextlib import ExitStack
import concourse.bass as bass
import concourse.mybir as mybir
from concourse.bass_utils import run_bass_kernel_spmd

F32 = mybir.dt.float32
BF16 = mybir.dt.bfloat16
AF = mybir.ActivationFunctionType
ALU = mybir.AluOpType
AX = mybir.AxisListType

D = 1024
S = 2048
NT = 16
DEPTH = 4
NE = 16
CAP = 256
PLE = 256
N_CORES = 8
SEQ_PER_CORE = 4
GELU = AF.Gelu_apprx_tanh
BIG = 1.0e9
DBG_ATT = 9


class _Op:
    __slots__ = ("eng", "fn", "deps", "dsem", "waits", "semid", "semval", "marked")

    def __init__(self, eng, fn, deps, dsem):
        self.eng = eng
        self.fn = fn
        self.deps = deps
        self.dsem = dsem
        self.waits = []
        self.semid = None
        self.semval = 0
        self.marked = False


class Prog:
    EPOCH_E = 24000
    EPOCH_D = 1500
    SAME_ENGINE_SYNC = True
    ENGS = ("pe", "act", "dve", "pool")

    def __init__(self):
        self.ops = []
        self.lastw = {}
        self.readers = {}
        self.last_on = {}
        self.pending = {}

    def barrier(self):
        front = set(self.last_on.get(e) for e in self.ENGS if self.last_on.get(e) is not None)
        for e in self.ENGS + ("sp",):
            self.pending[e] = set(front) | self.pending.get(e, set())

    def add(self, eng, fn, r=(), w=(), dsem=None):
        w = list(w) + [k for k in r if isinstance(k, tuple) and k[0] == "ps" and k not in w]
        deps = set()
        for k in r:
            d = self.lastw.get(k)
            if d is not None:
                deps.add(d)
        for k in w:
            d = self.lastw.get(k)
            if d is not None:
                deps.add(d)
            deps.update(self.readers.get(k, ()))
        pend = self.pending.get(eng)
        if pend:
            deps.update(pend)
            self.pending[eng] = set()
        idx = len(self.ops)
        self.ops.append(_Op(eng, fn, deps, dsem))
        for k in r:
            self.readers.setdefault(k, set()).add(idx)
        for k in w:
            self.lastw[k] = idx
            self.readers[k] = set()
        if dsem is None:
            self.last_on[eng] = idx
        return idx

    def finalize(self):
        ops = self.ops
        for i, op in enumerate(ops):
            best = {}
            for d in op.deps:
                src = ops[d]
                if src.dsem is not None:
                    key = ("d", src.dsem)
                else:
                    if src.eng == op.eng and (op.eng == "pe" or not self.SAME_ENGINE_SYNC):
                        continue
                    key = ("e", src.eng)
                if key not in best or best[key] < d:
                    best[key] = d
            op.waits = sorted(best.values())
            for d in op.waits:
                ops[d].marked = True
        cnt = {}
        semids = set()
        for op in ops:
            if op.dsem is not None:
                k = ("d", op.dsem)
                c = cnt.get(k, 0)
                cnt[k] = c + 1
                op.semid = (k, c // self.EPOCH_D)
                op.semval = (c % self.EPOCH_D + 1) * 16
                op.marked = True
                semids.add(op.semid)
            elif op.marked:
                k = ("e", op.eng)
                c = cnt.get(k, 0)
                cnt[k] = c + 1
                op.semid = (k, c // self.EPOCH_E)
                op.semval = c % self.EPOCH_E + 1
                semids.add(op.semid)
        return sorted(semids, key=str)

    def emit(self, nc, stack, final_waits=()):
        semids = self.finalize()
        sems = {}
        for n, sid in enumerate(semids):
            sems[sid] = stack.enter_context(nc.semaphore("s%d" % n))
        ops = self.ops
        block = stack.enter_context(nc.Block())

        def run(engname, e):
            waited = {}
            for op in ops:
                if op.eng != engname:
                    continue
                for d in op.waits:
                    src = ops[d]
                    if waited.get(src.semid, 0) >= src.semval:
                        continue
                    waited[src.semid] = src.semval
                    e.wait_ge(sems[src.semid], src.semval)
                ins = op.fn(e)
                if op.marked:
                    ins.then_inc(sems[op.semid], 16 if op.dsem is not None else 1)
            if engname == "sp":
                for d in final_waits:
                    src = ops[d]
                    e.wait_ge(sems[src.semid], src.semval)

        @block.tensor
        def _(e):
            run("pe", e)

        @block.scalar
        def _(e):
            run("act", e)

        @block.vector
        def _(e):
            run("dve", e)

        @block.gpsimd
        def _(e):
            run("pool", e)

        @block.sync
        def _(e):
            run("sp", e)


def natten_win(m, kt):
    q = np.arange(128)
    k = np.arange(128)
    r = 2 * m + q // 64
    qc = q % 64
    rs = np.clip(r - 4, 0, 24)
    cs = np.clip(qc - 8, 0, 48)
    kr = 2 * kt + k // 64
    kc = k % 64
    rowok = (kr[:, None] >= rs[None, :]) & (kr[:, None] < rs[None, :] + 8)
    colok = (kc[:, None] >= cs[None, :]) & (kc[:, None] < cs[None, :] + 16)
    return (rowok & colok).astype(np.float32)


def c_key_tiles(m):
    if m <= 1:
        return [0, 1, 2, 3]
    if m >= 14:
        return [12, 13, 14, 15]
    return [m - 2, m - 1, m, m + 1, m + 2]


def c_mask_classes():
    classes = []
    index = {}
    for m in range(NT):
        for kt in c_key_tiles(m):
            mk = natten_win(m, kt)
            key = mk.tobytes()
            found = None
            for ci, (kb, _) in enumerate(classes):
                if kb == key:
                    found = ci
                    break
            if found is None:
                classes.append((key, mk))
                found = len(classes) - 1
            index[(m, kt)] = found
    return np.stack([c[1] for c in classes], 0), index


C_MASKS, C_MASK_INDEX = c_mask_classes()
N_CMASK = C_MASKS.shape[0]


def host_consts():
    k = np.arange(128)[:, None]
    q = np.arange(128)[None, :]
    dist = np.zeros((128, 3, 128), np.float32)
    for i, dm in enumerate((-1, 0, 1)):
        rel = np.abs(dm * 128 + k - q).astype(np.float32)
        dist[:, i, :] = np.where(rel <= 128, rel, BIG)
    sel = np.zeros((16, 16, 128), np.float32)
    for e in range(16):
        sel[e, e, :] = 1.0
    return {
        "ident": np.eye(128, dtype=np.float32),
        "iota_row": np.broadcast_to(np.arange(256, dtype=np.float32)[None, :], (128, 256)).copy(),
        "iota_part": np.stack([np.arange(128), np.arange(128) + 128], 1).astype(np.float32),
        "sel": sel,
        "distB": dist,
        "maskC": np.ascontiguousarray(C_MASKS.transpose(1, 0, 2)),
    }


def host_gains(norm_mix_g, norm_ffn_g, ple_norm_g):
    g = np.stack([norm_mix_g, norm_ffn_g, ple_norm_g], axis=1)
    return np.ascontiguousarray(g.reshape(DEPTH, 3, 8, 128).transpose(0, 1, 3, 2))


def host_rpb_tables(rpb):
    k = np.arange(128)
    q = np.arange(128)
    out = np.zeros((4, 128, 7, 4, 128), np.float32)
    for di, dm in enumerate(range(-3, 4)):
        dr = (2 * dm + k[:, None] // 64) - (q[None, :] // 64)
        dc = (k[:, None] % 64) - (q[None, :] % 64)
        ok = (np.abs(dr) <= 7) & (np.abs(dc) <= 15)
        ri = np.clip(dr + 7, 0, 14)
        ci = np.clip(dc + 15, 0, 30)
        for h in range(16):
            tab = rpb[h][ri, ci]
            tab = np.where(ok, tab, np.float32(0.0))
            hh = h % 4
            pp = (hh % 2) * 2 + hh // 2
            out[h // 4, :, di, pp, :] = tab
    return out.reshape(4, 128, 7 * 4 * 128)


def alibi_slope(h):
    return float(np.float32(2.0 ** (-8.0 * (h + 1) / 16)))


class Ctx:
    pass


def build_program(n_seq=SEQ_PER_CORE, layers=(0, 1, 2, 3), parts=("mix", "ffn", "ple")):
    nc = bass.Bass("TRN2", target_bir_lowering=False)
    P = Prog()
    c = Ctx()
    stack = ExitStack()

    def dram(name, shape, dt=F32, kind="ExternalInput"):
        return nc.dram_tensor(name, list(shape), dt, kind=kind).ap()

    c.x = dram("x", [n_seq, S, D])
    c.out = dram("out", [n_seq, S, D], kind="ExternalOutput")
    c.gains = dram("gains", [DEPTH, 3, 128, 8])
    c.ident = dram("ident", [128, 128])
    c.iota_row_d = dram("iota_row", [128, 256])
    c.iota_part_d = dram("iota_part", [128, 2])
    c.sel_d = dram("sel", [16, 16, 128])
    c.distB_d = dram("distB", [128, 3, 128])
    c.maskC_d = dram("maskC", [128, N_CMASK, 128])
    L = {}
    for i in layers:
        d = Ctx()
        L[i] = d
        kind = i % 3
        d.kind = kind
        if "ple" in parts:
            d.p = dram("p_L%d" % i, [n_seq, S, PLE])
            d.ple_gate_w = dram("ple_gate_w_L%d" % i, [D, D])
            d.ple_proj_w = dram("ple_proj_w_L%d" % i, [PLE, D])
        if "ffn" in parts:
            d.router_w = dram("router_w_L%d" % i, [D, NE])
            d.wg = dram("exp_w_gate_L%d" % i, [NE, D, D])
            d.wu = dram("exp_w_up_L%d" % i, [NE, D, D])
            d.wd = dram("exp_w_down_L%d" % i, [NE, D, D])
        if "mix" in parts:
            d.w_out = dram("w_out_L%d" % i, [D, D])
            if kind == 0:
                d.w_in = dram("w_in_L%d" % i, [D, 2048])
                d.vnorm_g = dram("a_vnorm_g_L%d" % i, [1, D])
                d.w_sT = dram("a_w_sT_L%d" % i, [8, 128, 128])
                d.b_s = dram("a_b_s_L%d" % i, [1, D])
            elif kind == 1:
                d.w_in = dram("w_in_L%d" % i, [D, 1536])
                d.qk_g = dram("qk_g_L%d" % i, [128, 2])
                d.sink = dram("b_sink_L%d" % i, [1, 16])
            else:
                d.w_in = dram("w_in_L%d" % i, [D, 3072])
                d.qk_g = dram("qk_g_L%d" % i, [128, 2])
                d.rpbt = dram("c_rpbt_L%d" % i, [4, 128, 7 * 4 * 128])

    def sb(name, shape, dt):
        return stack.enter_context(nc.sbuf_tensor(name, list(shape), dt))

    def ps(name, shape, dt):
        return stack.enter_context(nc.psum_tensor(name, list(shape), dt))

    c.h = sb("h", [128, NT, D], F32)
    c.R1 = sb("R1", [128, 8 * S], BF16)
    c.R2 = sb("R2", [128, 8 * S], BF16)
    c.W = [sb("W%d" % i, [128, 8, 512], BF16) for i in range(6)]
    c.R3 = sb("R3", [128, 10240], BF16)
    c.gains_sb = sb("gains_sb", [128, DEPTH * 3 * 8], F32)
    c.ident_f = sb("ident_f", [128, 128], F32)
    c.ident_b = sb("ident_b", [128, 128], BF16)
    c.iota_row = sb("iota_row_sb", [128, 256], F32)
    c.iota_part = sb("iota_part_sb", [128, 2], F32)
    c.sel = sb("sel_sb", [16, 16, 128], BF16)
    c.ones_row = sb("ones_row", [1, 128], BF16)
    c.mhalf = sb("mhalf", [128, 32], F32)
    c.stat = sb("stat", [128, 96], F32)
    c.aff_tok = sb("aff_tok", [128, NT, NE], F32)
    c.slot_tok = sb("slot_tok", [128, NT, NE], F32)
    c.rw = sb("rw", [128, 8, NE], F32)
    c.rwg = sb("rwg", [128, 8, NE], F32)
    c.qkg = sb("qkg", [128, 4], F32)
    c.esink = sb("esink", [128, 16], F32)
    c.psb = [ps("psb%d" % i, [128, 512], F32) for i in range(8)]

    def r2(off, n, dt):
        v = c.R2[:, off // 2:(off + n) // 2]
        return v if dt == BF16 else v.bitcast(F32)

    def r3(off, n, dt, parts=128):
        v = c.R3[0:parts, off // 2:(off + n) // 2]
        return v if dt == BF16 else v.bitcast(F32)

    xnT = c.R1[:].rearrange("p (c t) -> p c t", c=8)
    hs = c.R1[:].rearrange("p (t d) -> p t d", t=NT)
    mixT = c.R2[:].rearrange("p (c t) -> p c t", c=8)

    def psbf(b):
        return c.psb[b][:].bitcast(BF16)

    def MM(out, lhsT, rhs, start, stop, r, w, skip=False):
        return P.add("pe", lambda e: e.matmul(out, lhsT=lhsT, rhs=rhs, start=start, stop=stop, skip_group_check=skip),
                     r=r, w=w)

    def TR(out, in_, ident, r, w):
        return P.add("pe", lambda e: e.transpose(out=out, in_=in_, identity=ident), r=r, w=w)

    def ACT(out, in_, func, r, w, **kw):
        return P.add("act", lambda e: e.activation(out=out, in_=in_, func=func, **kw), r=r, w=w)

    def TT(eng, out, in0, in1, op, r, w):
        return P.add(eng, lambda e: e.tensor_tensor(out=out, in0=in0, in1=in1, op=op), r=r, w=w)

    def TS(eng, out, in0, s1, s2, op0, op1, r, w):
        if op1 is None:
            return P.add(eng, lambda e: e.tensor_scalar(out=out, in0=in0, scalar1=s1, scalar2=None, op0=op0), r=r, w=w)
        return P.add(eng, lambda e: e.tensor_scalar(out=out, in0=in0, scalar1=s1, scalar2=s2, op0=op0, op1=op1), r=r, w=w)

    def STT(out, in0, scalar, in1, op0, op1, r, w):
        return P.add("dve", lambda e: e.scalar_tensor_tensor(out=out, in0=in0, scalar=scalar, in1=in1, op0=op0, op1=op1),
                     r=r, w=w)

    def CP(eng, out, in_, r, w):
        return P.add(eng, lambda e: e.tensor_copy(out=out, in_=in_), r=r, w=w)

    def DMA(eng, out, in_, r, w, dsem):
        return P.add(eng, lambda e: e.dma_start(out=out, in_=in_), r=r, w=w, dsem=dsem)

    DMA("sp", c.gains_sb[:].rearrange("p (a c) -> p a c", c=8), c.gains.rearrange("l k p c -> p (l k) c"),
        [], ["gains_sb"], "c_gains")
    DMA("sp", c.ident_f[:], c.ident, [], ["ident_f"], "c_ident")
    DMA("sp", c.iota_row[:], c.iota_row_d, [], ["iota_row"], "c_iotar")
    DMA("sp", c.iota_part[:], c.iota_part_d, [], ["iota_part"], "c_iotap")
    DMA("pool", c.sel[:], c.sel_d, [], ["sel"], "c_sel")
    CP("dve", c.ident_b[:], c.ident_f[:], ["ident_f"], ["ident_b"])
    P.add("pool", lambda e: e.memset(c.ones_row[:], 1.0), w=["ones_row"])
    P.add("pool", lambda e: e.memset(c.mhalf[:], -0.5), w=["mhalf"])

    def gain_ap(layer, which, dc):
        o = (layer * 3 + which) * 8 + dc
        return c.gains_sb[:, o:o + 1]

    def load_w(slot, src2d, kc, n0, n=512, col0=0):
        dst = c.W[slot][:, 0:kc, col0:col0 + n]
        return DMA("pool", dst, src2d[:, n0:n0 + n].rearrange("(c p) n -> p c n", p=128), [], [("W", slot)], ("W", slot))

    def rstd_rms(dst_col0):
        junk = r3(0, 2048, BF16)
        for tt in range(NT):
            ACT(junk, c.h[:, tt, :], AF.Square, [("h", tt)], ["junk", ("ss", tt)], accum_out=c.stat[:, tt:tt + 1])
        TS("pool", c.stat[:, 16:32], c.stat[:, 0:16], 1.0 / D, 1e-6, ALU.mult, ALU.add,
           [("ss", t) for t in range(NT)], ["ms"])
        TT("pool", c.stat[:, dst_col0:dst_col0 + 16], c.stat[:, 16:32], c.mhalf[:, 0:16], ALU.pow, ["ms", "mhalf"], ["rstd"])

    def rms_to_xnT(layer, which):
        P.barrier()
        rstd_rms(32)
        hsb = r3(2048, 4096, BF16).rearrange("p (s d) -> p s d", s=2)
        for g4 in range(NT // 4):
            for j in range(4):
                tt = g4 * 4 + j
                slot = tt % 2
                TS("dve", hsb[:, slot, :], c.h[:, tt, :], c.stat[:, 32 + tt:33 + tt], None, ALU.mult, None,
                   [("h", tt), "rstd"], [("hsb", slot)])
                for dc in range(8):
                    TR(psbf(dc)[:, j * 128:(j + 1) * 128], hsb[:, slot, dc * 128:(dc + 1) * 128], c.ident_b[:],
                       [("hsb", slot), "ident_b"], [("ps", dc)])
            for dc in range(8):
                if dc % 2 == 0:
                    ACT(xnT[:, dc, g4 * 512:(g4 + 1) * 512], psbf(dc)[:, 0:512], AF.Copy,
                        [("ps", dc), "gains_sb"], [("xnT", g4)], scale=gain_ap(layer, which, dc))
                else:
                    TS("dve", xnT[:, dc, g4 * 512:(g4 + 1) * 512], psbf(dc)[:, 0:512], gain_ap(layer, which, dc), None,
                       ALU.mult, None, [("ps", dc), "gains_sb"], [("xnT", g4)])

    def ple_block(layer, seq):
        d = L[layer]
        rms_to_xnT(layer, 2)
        pbuf = r2(0, 16384, F32).rearrange("p (t k) -> p t k", k=PLE)
        pT = r2(16384, 8192, BF16).rearrange("p (k t) -> p k t", k=2)
        hsb = r3(2048, 4096, BF16).rearrange("p (s d) -> p s d", s=2)
        tmpA = r3(6144, 4096, F32).rearrange("p (s d) -> p s d", s=2)
        tmpB = r3(10240, 4096, F32).rearrange("p (s d) -> p s d", s=2)
        load_w(0, d.ple_gate_w, 8, 0)
        load_w(1, d.ple_gate_w, 8, 512)
        load_w(2, d.ple_proj_w, 2, 0)
        load_w(3, d.ple_proj_w, 2, 512)
        DMA("sp", pbuf, d.p[seq].rearrange("(t p) k -> p t k", p=128), [], ["pbuf"], "pbuf")
        for tt in range(NT):
            slot = tt % 2
            CP("dve", hsb[:, slot, 0:PLE], pbuf[:, tt, :], ["pbuf"], [("hsb", slot)])
            bank = tt % 2
            for kc in range(2):
                TR(psbf(bank)[:, kc * 128:(kc + 1) * 128], hsb[:, slot, kc * 128:(kc + 1) * 128], c.ident_b[:],
                   [("hsb", slot), "ident_b"], [("ps", bank)])
            ACT(pT[:, :, tt * 128:(tt + 1) * 128], psbf(bank)[:, 0:256].rearrange("p (k t) -> p k t", k=2), AF.Copy,
                [("ps", bank)], ["pT"])
        n = 0
        for tt in range(NT):
            for half in range(2):
                bG = 2 + (n % 3) * 2
                bP = bG + 1
                s2 = n % 2
                n += 1
                for kc in range(8):
                    MM(c.psb[bG][:], xnT[:, kc, tt * 128:(tt + 1) * 128], c.W[half][:, kc, :], kc == 0, kc == 7,
                       [("xnT", tt // 4), ("W", half)], [("ps", bG)])
                for kc in range(2):
                    MM(c.psb[bP][:], pT[:, kc, tt * 128:(tt + 1) * 128], c.W[2 + half][:, kc, :], kc == 0, kc == 1,
                       ["pT", ("W", 2 + half)], [("ps", bP)])
                ACT(tmpA[:, s2, :], c.psb[bG][:], AF.Sigmoid, [("ps", bG)], [("tmpA", s2)])
                TT("dve", tmpB[:, s2, :], tmpA[:, s2, :], c.psb[bP][:], ALU.mult, [("tmpA", s2), ("ps", bP)], [("tmpB", s2)])
                hv = c.h[:, tt, half * 512:(half + 1) * 512]
                TT("pool", hv, hv, tmpB[:, s2, :], ALU.add, [("tmpB", s2), ("h", tt)], [("h", tt)])

    def wout_full(layer):
        n = 0
        for tt in range(NT):
            for half in range(2):
                b = n % 4
                n += 1
                for kc in range(8):
                    MM(c.psb[b][:], mixT[:, kc, tt * 128:(tt + 1) * 128], c.W[4 + half][:, kc, :], kc == 0, kc == 7,
                       [("mixT", tt // 4), ("W", 4 + half)], [("ps", b)])
                hv = c.h[:, tt, half * 512:(half + 1) * 512]
                TT("dve", hv, hv, c.psb[b][:], ALU.add, [("ps", b), ("h", tt)], [("h", tt)])

    def mixer_a(layer, seq):
        d = L[layer]
        rms_to_xnT(layer, 0)
        load_w(2, d.w_in, 8, 1024)
        load_w(3, d.w_in, 8, 1536)
        load_w(0, d.w_in, 8, 0)
        load_w(1, d.w_in, 8, 512)
        load_w(4, d.w_out, 8, 0)
        load_w(5, d.w_out, 8, 512)
        vg = r3(6144, 4096, F32)
        wsT = r3(10240, 2048, BF16).rearrange("p (g t) -> p g t", g=8)
        bs = r3(12288, 2048, BF16, parts=1)
        vt = r3(14336, 4096, F32)
        vn = r3(18432, 2048, BF16)
        DMA("sp", vg, d.vnorm_g[0:1, :].to_broadcast([128, D]), [], ["vg"], "a_vg")
        DMA("pool", wsT, d.w_sT.rearrange("g s t -> s g t"), [], ["wsT"], "a_wsT")
        DMA("pool", bs, d.b_s, [], ["bs"], "a_bs")
        st6 = c.stat[:, 48:60].rearrange("p (a b) -> p a b", a=2)
        mv = c.stat[:, 60:62]
        for tt in range(NT):
            for half in range(2):
                b = half
                for kc in range(8):
                    MM(c.psb[b][:], xnT[:, kc, tt * 128:(tt + 1) * 128], c.W[2 + half][:, kc, :], kc == 0, kc == 7,
                       [("xnT", tt // 4), ("W", 2 + half)], [("ps", b)])
                ACT(vt[:, half * 512:(half + 1) * 512], c.psb[b][:], GELU, [("ps", b)], ["vt"])
                P.add("dve", lambda e, half=half: e.bn_stats(out=st6[:, half, :], in_=vt[:, half * 512:(half + 1) * 512]),
                      r=["vt"], w=["st6"])
            P.add("dve", lambda e: e.bn_aggr(out=mv, in_=c.stat[:, 48:60]), r=["st6"], w=["mv"])
            TS("pool", c.stat[:, 62:63], c.stat[:, 61:62], 1e-5, None, ALU.add, None, ["mv"], ["lnv"])
            TT("pool", c.stat[:, 63:64], c.stat[:, 62:63], c.mhalf[:, 0:1], ALU.pow, ["lnv", "mhalf"], ["lnr"])
            TS("dve", vt, vt, c.stat[:, 60:61], c.stat[:, 63:64], ALU.subtract, ALU.mult, ["vt", "mv", "lnr"], ["vt"])
            TT("dve", vn, vt, vg, ALU.mult, ["vt", "vg"], ["vn"])
            for g in range(8):
                b = 2 + g // 4
                col = (g % 4) * 128
                MM(c.psb[b][:, col:col + 128], vn[:, g * 128:(g + 1) * 128], wsT[:, g, :], True, False,
                   ["vn", "wsT"], [("ps", b)])
                MM(c.psb[b][:, col:col + 128], c.ones_row[0:1, :], bs[0:1, g * 128:(g + 1) * 128], False, True,
                   ["ones_row", "bs"], [("ps", b)])
            for b2 in range(2):
                outv = mixT[:, b2 * 4:(b2 + 1) * 4, tt * 128:(tt + 1) * 128]
                inv = c.psb[2 + b2][:].rearrange("p (g t) -> p g t", g=4)
                if b2 == 0:
                    ACT(outv, inv, AF.Copy, [("ps", 2 + b2)], [("mixT", tt // 4)])
                else:
                    CP("dve", outv, inv, [("ps", 2 + b2)], [("mixT", tt // 4)])
        P.barrier()
        gu = r3(14336, 4096, F32).rearrange("p (s d) -> p s d", s=2)
        n = 0
        for fc in range(8):
            for tq in range(4):
                b = n % 4
                s2 = n % 2
                n += 1
                for kc in range(8):
                    MM(c.psb[b][:], c.W[fc // 4][:, kc, (fc % 4) * 128:(fc % 4 + 1) * 128], xnT[:, kc, tq * 512:(tq + 1) * 512],
                       kc == 0, kc == 7, [("xnT", tq), ("W", fc // 4)], [("ps", b)])
                ACT(gu[:, s2, :], c.psb[b][:], GELU, [("ps", b)], [("gu", s2)])
                mv_ = mixT[:, fc, tq * 512:(tq + 1) * 512]
                TT("dve" if n % 2 else "pool", mv_, mv_, gu[:, s2, :], ALU.mult, [("gu", s2), ("mixT", tq)], [("mixT", tq)])
        wout_full(layer)

    def attention(layer, seq):
        d = L[layer]
        isB = (d.kind == 1)
        rms_to_xnT(layer, 0)
        P.barrier()
        qT = r2(0, 8192, BF16).rearrange("p (c t) -> p c t", c=2)
        kT = r2(8192, 8192, BF16).rearrange("p (c t) -> p c t", c=2)
        V = r2(16384, 8448, BF16).rearrange("p (t h e) -> p t h e", t=NT, h=4)
        sq = r3(0, 2048, F32)
        qtok = r3(2048, 1024, BF16).rearrange("p (s d) -> p s d", s=2)
        ktok = r3(3072, 1024, BF16).rearrange("p (s d) -> p s d", s=2)
        tS = r3(4096, 4096, F32).rearrange("p (s h q) -> p s h q", s=2, h=4)
        Eb = r3(8192, 2048, BF16).rearrange("p (s h q) -> p s h q", s=2, h=4)
        PTb = r3(10240, 2048, BF16).rearrange("p (s h q) -> p s h q", s=2, h=4)
        otok = r3(12288, 1024, BF16).rearrange("p (s d) -> p s d", s=2)
        oT = r3(13312, 1024, BF16).rearrange("p (s k t) -> p s k t", s=2, k=2)
        b4 = b5 = distB = maskC = None
        if isB:
            distB = r3(14336, 1536, F32).rearrange("p (a q) -> p a q", a=3)
            DMA("sp", distB, c.distB_d, [], ["tab"], "tabB")
            DMA("sp", c.esink[:], d.sink[0:1, :].to_broadcast([128, 16]), [], ["esink"], "sink")
            ACT(c.esink[:], c.esink[:], AF.Exp, ["esink"], ["esink"])
        else:
            maskC = r3(14336, N_CMASK * 256, BF16).rearrange("p (a q) -> p a q", a=N_CMASK)
            DMA("pool", maskC, c.maskC_d, [], ["tab"], "tabC")
            b4 = c.W[4][:].rearrange("p a b -> p (a b)").bitcast(F32).rearrange("p (m h q) -> p m h q", m=4, h=4)
            b5 = c.W[5][:].rearrange("p a b -> p (a b)").bitcast(F32).rearrange("p (m h q) -> p m h q", m=4, h=4)
        DMA("sp", c.qkg[:, 0:2], d.qk_g, [], ["qkg"], "qkg")
        TS("pool", c.qkg[:, 2:3], c.qkg[:, 0:1], 0.125, None, ALU.mult, None, ["qkg"], ["qkg2"])
        P.add("pool", lambda e: e.memset(V[:, :, :, 64:65], 1.0), w=["V"])
        load_w(2, d.w_out, 8, 0)
        load_w(3, d.w_out, 8, 512)
        for gi in range(4):
            if isB:
                load_w(0, d.w_in, 8, gi * 256, n=256)
                load_w(1, d.w_in, 8, 1024 + gi * 64, n=64, col0=0)
                load_w(1, d.w_in, 8, 1280 + gi * 64, n=64, col0=64)
            else:
                load_w(0, d.w_in, 8, gi * 256, n=256, col0=0)
                load_w(0, d.w_in, 8, 1024 + gi * 256, n=256, col0=256)
                load_w(1, d.w_in, 8, 2048 + gi * 256, n=256, col0=0)
                src = d.rpbt[gi].rearrange("p (m h q) -> p m h q", m=7, h=4)
                DMA("sp", b4, src[:, 0:4], [], [("W", 4)], ("W", 4))
                DMA("sp", b5[:, 0:3], src[:, 4:7], [], [("W", 5)], ("W", 5))
            for tt in range(NT if DBG_ATT >= 1 else 0):
                s2 = tt % 2
                bq = s2
                bkv = 2 + s2
                for kc in range(8):
                    MM(c.psb[bq][:, 0:256], xnT[:, kc, tt * 128:(tt + 1) * 128], c.W[0][:, kc, 0:256], kc == 0, kc == 7,
                       [("xnT", tt // 4), ("W", 0)], [("ps", bq)])
                if isB:
                    for kc in range(8):
                        MM(c.psb[bkv][:, 0:128], xnT[:, kc, tt * 128:(tt + 1) * 128], c.W[1][:, kc, 0:128], kc == 0, kc == 7,
                           [("xnT", tt // 4), ("W", 1)], [("ps", bkv)])
                    kps = c.psb[bkv][:, 0:64]
                    vps = c.psb[bkv][:, 64:128]
                    nk = 64
                else:
                    for kc in range(8):
                        MM(c.psb[bkv][:, 0:256], xnT[:, kc, tt * 128:(tt + 1) * 128], c.W[0][:, kc, 256:512], kc == 0, kc == 7,
                           [("xnT", tt // 4), ("W", 0)], [("ps", bkv)])
                    for kc in range(8):
                        MM(c.psb[bkv][:, 256:512], xnT[:, kc, tt * 128:(tt + 1) * 128], c.W[1][:, kc, 0:256], kc == 0, kc == 7,
                           [("xnT", tt // 4), ("W", 1)], [("ps", bkv)])
                    kps = c.psb[bkv][:, 0:256]
                    vps = c.psb[bkv][:, 256:512]
                    nk = 256
                ACT(sq[:, 0:256], c.psb[bq][:, 0:256], AF.Square, [("ps", bq)], ["sq"])
                ACT(sq[:, 256:256 + nk], kps, AF.Square, [("ps", bkv)], ["sq"])
                nh = 4 + nk // 64
                ssq = c.stat[:, 64:64 + nh]
                P.add("dve", lambda e, nh=nh, ssq=ssq: e.tensor_reduce(
                    out=ssq, in_=sq[:, 0:nh * 64].rearrange("p (h e) -> p h e", e=64), axis=AX.X, op=ALU.add),
                    r=["sq"], w=["ssq"])
                TS("pool", c.stat[:, 72:72 + nh], ssq, 1.0 / 64, 1e-6, ALU.mult, ALU.add, ["ssq"], ["qms"])
                TT("pool", c.stat[:, 80:80 + nh], c.stat[:, 72:72 + nh], c.mhalf[:, 0:nh], ALU.pow, ["qms", "mhalf"], ["qrs"])
                TT("dve", qtok[:, s2, :].rearrange("p (h e) -> p h e", e=64),
                   c.psb[bq][:, 0:256].rearrange("p (h e) -> p h e", e=64),
                   c.stat[:, 80:84].unsqueeze(2).to_broadcast([128, 4, 64]), ALU.mult,
                   [("ps", bq), "qrs"], [("qtok", s2)])
                if isB:
                    for dup in range(2):
                        TS("dve", ktok[:, s2, dup * 64:(dup + 1) * 64], kps, c.stat[:, 84:85], None, ALU.mult, None,
                           [("ps", bkv), "qrs"], [("ktok", s2)])
                    ACT(V[:, tt, 0, 0:64], vps, AF.Copy, [("ps", bkv)], ["V"])
                else:
                    TT("dve", ktok[:, s2, :].rearrange("p (h e) -> p h e", e=64),
                       kps.rearrange("p (h e) -> p h e", e=64),
                       c.stat[:, 84:88].unsqueeze(2).to_broadcast([128, 4, 64]), ALU.mult,
                       [("ps", bkv), "qrs"], [("ktok", s2)])
                    ACT(V[:, tt, :, 0:64], vps.rearrange("p (h e) -> p h e", e=64), AF.Copy, [("ps", bkv)], ["V"])
                bt = 4 + s2
                for pr in range(2):
                    TR(psbf(bt)[:, pr * 128:(pr + 1) * 128], qtok[:, s2, pr * 128:(pr + 1) * 128], c.ident_b[:],
                       [("qtok", s2), "ident_b"], [("ps", bt)])
                nkp = 1 if isB else 2
                for pr in range(nkp):
                    TR(psbf(bt)[:, 256 + pr * 128:256 + (pr + 1) * 128], ktok[:, s2, pr * 128:(pr + 1) * 128], c.ident_b[:],
                       [("ktok", s2), "ident_b"], [("ps", bt)])
                ACT(qT[:, :, tt * 128:(tt + 1) * 128], psbf(bt)[:, 0:256].rearrange("p (c t) -> p c t", c=2), AF.Copy,
                    [("ps", bt), "qkg2"], ["qT"], scale=c.qkg[:, 2:3])
                TS("dve", kT[:, 0:nkp, tt * 128:(tt + 1) * 128],
                   psbf(bt)[:, 256:256 + nkp * 128].rearrange("p (c t) -> p c t", c=nkp), c.qkg[:, 1:2], None, ALU.mult, None,
                   [("ps", bt), "qkg"], ["kT"])
            step = 0
            for m in range(NT if DBG_ATT >= 2 else 0):
                if isB:
                    kts = [kt for kt in (m - 1, m, m + 1) if 0 <= kt < NT]
                else:
                    kts = c_key_tiles(m)
                bo = 4 + m % 2
                ops_ = c.psb[bo][:, 0:260].rearrange("p (h e) -> p h e", e=65)
                for ki, kt in enumerate(kts):
                    s2 = step % 2
                    step += 1
                    for pp in range(4):
                        hf, j = pp // 2, pp % 2
                        bsc = s2 * 2 + hf
                        kpr = 0 if isB else j
                        MM(c.psb[bsc][:, j * 128:(j + 1) * 128],
                           kT[hf * 64:(hf + 1) * 64, kpr, kt * 128:(kt + 1) * 128],
                           qT[hf * 64:(hf + 1) * 64, j, m * 128:(m + 1) * 128], True, True,
                           ["qT", "kT"], [("ps", bsc)])
                    if isB:
                        dmi = kt - m + 1
                        for pp in range(4):
                            hf, j = pp // 2, pp % 2
                            bsc = s2 * 2 + hf
                            STT(tS[:, s2, pp, :], distB[:, dmi, :], -alibi_slope(gi * 4 + 2 * j + hf),
                                c.psb[bsc][:, j * 128:(j + 1) * 128], ALU.mult, ALU.add,
                                [("ps", bsc), "tab"], [("tS", s2)])
                        ACT(PTb[:, s2], tS[:, s2], AF.Exp, [("tS", s2)], [("PT", s2)])
                    else:
                        dmi = kt - m + 3
                        btab = (b4[:, dmi] if dmi < 4 else b5[:, dmi - 4])
                        for hf in range(2):
                            bsc = s2 * 2 + hf
                            TT("dve", tS[:, s2, 2 * hf:2 * hf + 2, :],
                               c.psb[bsc][:, 0:256].rearrange("p (h q) -> p h q", h=2), btab[:, 2 * hf:2 * hf + 2, :], ALU.add,
                               [("ps", bsc), ("W", 4), ("W", 5)], [("tS", s2)])
                        ACT(Eb[:, s2], tS[:, s2], AF.Exp, [("tS", s2)], [("E", s2)])
                        ci = C_MASK_INDEX[(m, kt)]
                        TT("pool", PTb[:, s2], Eb[:, s2], maskC[:, ci:ci + 1, :].to_broadcast([128, 4, 128]), ALU.mult,
                           [("E", s2), "tab"], [("PT", s2)])
                    for pp in range(4 if DBG_ATT >= 3 else 0):
                        hh = 2 * (pp % 2) + pp // 2
                        vh = 0 if isB else hh
                        MM(ops_[:, hh, :], PTb[:, s2, pp, :], V[:, kt, vh, 0:65], ki == 0 and pp == 0, ki == len(kts) - 1,
                           [("PT", s2), "V"], [("ps", bo)], skip=True)
                if DBG_ATT < 4:
                    continue
                o2 = m % 2
                den = c.stat[:, 88:92]
                if isB:
                    TT("dve", den, ops_[:, :, 64], c.esink[:, gi * 4:(gi + 1) * 4], ALU.add, [("ps", bo), "esink"], ["den"])
                else:
                    CP("dve", den, ops_[:, :, 64], [("ps", bo)], ["den"])
                P.add("dve", lambda e, den=den: e.reciprocal(out=c.stat[:, 92:96], in_=den), r=["den"], w=["rden"])
                TT("dve", otok[:, o2, :].rearrange("p (h e) -> p h e", e=64), ops_[:, :, 0:64],
                   c.stat[:, 92:96].unsqueeze(2).to_broadcast([128, 4, 64]), ALU.mult,
                   [("ps", bo), "rden"], [("otok", o2)])
                bt = 6
                for pr in range(2):
                    TR(psbf(bt)[:, pr * 128:(pr + 1) * 128], otok[:, o2, pr * 128:(pr + 1) * 128], c.ident_b[:],
                       [("otok", o2), "ident_b"], [("ps", bt)])
                ACT(oT[:, o2], psbf(bt)[:, 0:256].rearrange("p (k t) -> p k t", k=2), AF.Copy, [("ps", bt)], [("oT", o2)])
                for half in range(2):
                    bw = 7
                    for k2 in range(2):
                        MM(c.psb[bw][:], oT[:, o2, k2, :], c.W[2 + half][:, gi * 2 + k2, :], k2 == 0, k2 == 1,
                           [("oT", o2), ("W", 2 + half)], [("ps", bw)])
                    hv = c.h[:, m, half * 512:(half + 1) * 512]
                    TT("dve", hv, hv, c.psb[bw][:], ALU.add, [("ps", bw), ("h", m)], [("h", m)])

    def ffn(layer, seq):
        d = L[layer]
        P.barrier()
        DMA("sp", c.rw[:], d.router_w.rearrange("(c p) e -> p c e", p=128), [], ["rw"], "rw")
        o = (layer * 3 + 1) * 8
        TT("dve", c.rwg[:], c.rw[:], c.gains_sb[:, o:o + 8].unsqueeze(2).to_broadcast([128, 8, NE]), ALU.mult,
           ["rw", "gains_sb"], ["rwg"])
        wslot = {"g": (0, 1), "u": (2, 3), "d": (4, 5)}

        def load_expert(e, which, half):
            src = {"g": d.wg, "u": d.wu, "d": d.wd}[which][e]
            load_w(wslot[which][half], src, 8, half * 512)

        for which in ("g", "u", "d"):
            for half in range(2):
                load_expert(0, which, half)
        rstd_rms(32)
        hsf = r3(2048, 8192, F32).rearrange("p (s d) -> p s d", s=2)
        hsfT = r3(10240, 8192, F32).rearrange("p (s c t) -> p s c t", s=2, c=8)
        bL = 7
        for tt in range(NT):
            s2 = tt % 2
            TS("dve", hs[:, tt, :], c.h[:, tt, :], c.stat[:, 32 + tt:33 + tt], None, ALU.mult, None,
               [("h", tt), "rstd"], [("hs", tt)])
            ACT(hsf[:, s2, :], c.h[:, tt, :], AF.Copy, [("h", tt), "rstd"], [("hsf", s2)], scale=c.stat[:, 32 + tt:33 + tt])
            for dc in range(8):
                b = s2 * 2 + dc // 4
                TR(c.psb[b][:, (dc % 4) * 128:(dc % 4 + 1) * 128], hsf[:, s2, dc * 128:(dc + 1) * 128], c.ident_f[:],
                   [("hsf", s2), "ident_f"], [("ps", b)])
            for b2 in range(2):
                b = s2 * 2 + b2
                outv = hsfT[:, s2, b2 * 4:(b2 + 1) * 4, :]
                inv = c.psb[b][:].rearrange("p (c t) -> p c t", c=4)
                if b2 == 0:
                    ACT(outv, inv, AF.Copy, [("ps", b)], [("hsfT", s2)])
                else:
                    CP("dve", outv, inv, [("ps", b)], [("hsfT", s2)])
            for dc in range(8):
                MM(c.psb[bL][:, tt * 16:(tt + 1) * 16], hsfT[:, s2, dc, :], c.rwg[:, dc, :], dc == 0, dc == 7,
                   [("hsfT", s2), "rwg"], [("ps", bL)])
        lg = c.psb[bL][:, 0:256].rearrange("p (t e) -> p t e", e=NE)
        mx = c.stat[:, 0:16]
        P.add("dve", lambda e: e.tensor_reduce(out=mx, in_=lg, axis=AX.X, op=ALU.max), r=[("ps", bL)], w=["mx"])
        TT("dve", c.aff_tok[:], lg, mx.unsqueeze(2).to_broadcast([128, NT, NE]), ALU.subtract, [("ps", bL), "mx"], ["aff"])
        ACT(c.aff_tok[:], c.aff_tok[:], AF.Exp, ["aff"], ["aff"])
        sm = c.stat[:, 16:32]
        P.add("dve", lambda e: e.tensor_reduce(out=sm, in_=c.aff_tok[:], axis=AX.X, op=ALU.add), r=["aff"], w=["sm"])
        P.add("dve", lambda e: e.reciprocal(out=sm, in_=sm), r=["sm"], w=["sm"])
        TT("dve", c.aff_tok[:], c.aff_tok[:], sm.unsqueeze(2).to_broadcast([128, NT, NE]), ALU.mult, ["aff", "sm"], ["aff"])
        P.barrier()
        affT = r2(0, 8192, F32)[0:16, :]
        work = r2(8192, 8192, F32)[0:16, :]
        cum = r2(16384, 8192, F32)[0:16, :]
        slotbf = r3(0, 4096, BF16)[0:16, :]
        for q4 in range(4):
            for j in range(4):
                tt = q4 * 4 + j
                TR(c.psb[q4][0:16, j * 128:(j + 1) * 128], c.aff_tok[:, tt, :], c.ident_f[:], ["aff", "ident_f"], [("ps", q4)])
            ACT(affT[:, q4 * 512:(q4 + 1) * 512], c.psb[q4][0:16, :], AF.Copy, [("ps", q4)], ["affT"])
        CP("dve", work, affT, ["affT"], ["work"])
        m8 = c.stat[0:16, 48:56]
        for it in range(CAP // 8):
            P.add("dve", lambda e: e.max(out=m8, in_=work), r=["work"], w=["m8"])
            if it < CAP // 8 - 1:
                P.add("dve", lambda e: e.match_replace(out=work, in_to_replace=m8, in_values=work, imm_value=-1.0),
                      r=["work", "m8"], w=["work"])
        TS("dve", work, affT, c.stat[0:16, 55:56], None, ALU.is_ge, None, ["affT", "m8"], ["work"])
        P.add("dve", lambda e: e.tensor_tensor_scan(out=cum, data0=work, data1=work, initial=0.0, op0=ALU.add, op1=ALU.max),
              r=["work"], w=["cum"])
        STT(cum, cum, -513.0, work, ALU.add, ALU.mult, ["cum", "work"], ["cum"])
        TS("dve", cum, cum, 512.0, None, ALU.add, None, ["cum"], ["cum"])
        CP("dve", slotbf, cum, ["cum"], ["slotbf"])
        bS = 4
        for tt in range(NT):
            TR(c.psb[bS][:, tt * 16:(tt + 1) * 16], cum[:, tt * 128:(tt + 1) * 128], c.ident_f[0:16, 0:16],
               ["cum", "ident_f"], [("ps", bS)])
        CP("dve", c.slot_tok[:].rearrange("p t e -> p (t e)"), c.psb[bS][:, 0:256], [("ps", bS)], ["slot_tok"])
        P.barrier()
        Pm = r2(0, 8192, BF16).rearrange("p (t s) -> p t s", t=NT)
        PT = r2(8192, 8192, BF16).rearrange("p (s t) -> p s t", s=2)
        xsT = r2(16384, 4096, BF16).rearrange("p (c s) -> p c s", c=8)
        hT = r2(20480, 4096, BF16).rearrange("p (c s) -> p c s", c=8)
        Y = r2(24576, 4096, BF16).rearrange("p (s d) -> p s d", s=2)
        sg = r3(4096, 2048, F32).rearrange("p (s d) -> p s d", s=2)
        PTsb = r3(6144, 4096, BF16)
        for ex in range(NE):
            TT("dve", Pm, c.iota_row[:].unsqueeze(1).to_broadcast([128, NT, CAP]),
               c.slot_tok[:, :, ex:ex + 1].to_broadcast([128, NT, CAP]), ALU.is_equal,
               ["iota_row", "slot_tok"], ["Pm"])
            for q4 in range(4):
                MM(c.psb[q4][:], c.sel[:, ex, :], slotbf[:, q4 * 512:(q4 + 1) * 512], True, True,
                   ["sel", "slotbf"], [("ps", q4)])
                ACT(PTsb[:, q4 * 512:(q4 + 1) * 512], c.psb[q4][:], AF.Copy, [("ps", q4)], ["PTsb"])
            for st in range(2):
                TS("pool", PT[:, st, :], PTsb, c.iota_part[:, st:st + 1], None, ALU.is_equal, None,
                   ["PTsb", "iota_part"], ["PT"])
            for dc in range(8):
                b = 4 + (dc // 2) % 2
                col = (dc % 2) * 256
                for tt in range(NT):
                    MM(c.psb[b][:, col:col + 256], hs[:, tt, dc * 128:(dc + 1) * 128], Pm[:, tt, :], tt == 0, tt == NT - 1,
                       [("hs", tt), "Pm"], [("ps", b)])
                ACT(xsT[:, dc, :], c.psb[b][:, col:col + 256], AF.Copy, [("ps", b), "gains_sb"], ["xsT"],
                    scale=gain_ap(layer, 1, dc))
            for fc in range(8):
                half, fo = fc // 4, (fc % 4) * 128
                b = 6 + fc % 2
                for dc in range(8):
                    MM(c.psb[b][:, 0:256], c.W[0 + half][:, dc, fo:fo + 128], xsT[:, dc, :], dc == 0, dc == 7,
                       ["xsT", ("W", 0 + half)], [("ps", b)])
                for dc in range(8):
                    MM(c.psb[b][:, 256:512], c.W[2 + half][:, dc, fo:fo + 128], xsT[:, dc, :], dc == 0, dc == 7,
                       ["xsT", ("W", 2 + half)], [("ps", b)])
                s2 = fc % 2
                ACT(sg[:, s2, :], c.psb[b][:, 0:256], AF.Silu, [("ps", b)], [("sg", s2)])
                TT("dve", hT[:, fc, :], sg[:, s2, :], c.psb[b][:, 256:512], ALU.mult, [("sg", s2), ("ps", b)], ["hT"])
                if fc == 3 and ex + 1 < NE:
                    load_expert(ex + 1, "g", 0)
                    load_expert(ex + 1, "u", 0)
            if ex + 1 < NE:
                load_expert(ex + 1, "g", 1)
                load_expert(ex + 1, "u", 1)
            n = 0
            for half in range(2):
                for st in range(2):
                    b = 4 + n % 2
                    n += 1
                    for fc in range(8):
                        MM(c.psb[b][:], hT[:, fc, st * 128:(st + 1) * 128], c.W[4 + half][:, fc, :], fc == 0, fc == 7,
                           ["hT", ("W", 4 + half)], [("ps", b)])
                    ACT(Y[:, st, half * 512:(half + 1) * 512], c.psb[b][:], AF.Copy, [("ps", b)], ["Y"])
                if ex + 1 < NE:
                    load_expert(ex + 1, "d", half)
            n = 0
            for tt in range(NT):
                for half in range(2):
                    b = n % 4
                    n += 1
                    for st in range(2):
                        MM(c.psb[b][:], PT[:, st, tt * 128:(tt + 1) * 128], Y[:, st, half * 512:(half + 1) * 512], st == 0, st == 1,
                           ["PT", "Y"], [("ps", b)])
                    hv = c.h[:, tt, half * 512:(half + 1) * 512]
                    STT(hv, c.psb[b][:], c.aff_tok[:, tt, ex:ex + 1], hv, ALU.mult, ALU.add,
                        [("ps", b), "aff", ("h", tt)], [("h", tt)])

    finals = []
    for seq in range(n_seq):
        for q in range(4):
            DMA("sp", c.h[:, q * 4:(q + 1) * 4, :], c.x[seq, q * 512:(q + 1) * 512, :].rearrange("(t p) d -> p t d", p=128),
                [], [("h", q * 4 + j) for j in range(4)], ("hload", q))
        for layer in layers:
            if "mix" in parts:
                if L[layer].kind == 0:
                    mixer_a(layer, seq)
                else:
                    attention(layer, seq)
            if "ffn" in parts:
                ffn(layer, seq)
            if "ple" in parts:
                ple_block(layer, seq)
        P.barrier()
        for q in range(4):
            finals.append(DMA("sp", c.out[seq, q * 512:(q + 1) * 512, :].rearrange("(t p) d -> p t d", p=128),
                              c.h[:, q * 4:(q + 1) * 4, :], [("h", q * 4 + j) for j in range(4)], [], ("hstore", q)))

    n_ops = len(P.ops)
    P.emit(nc, stack, final_waits=finals)
    stack.close()
    return nc, n_ops


def core_inputs(inp, seqs, layers, parts=("mix", "ffn", "ple"), x_override=None, consts=None):
    f = np.float32
    im = dict(consts if consts is not None else host_consts())
    xs = inp["x"] if x_override is None else x_override
    im["x"] = np.ascontiguousarray(xs[seqs])
    im["gains"] = host_gains(inp["norm_mix_g"], inp["norm_ffn_g"], inp["ple_norm_g"])
    for i in layers:
        kind, j = i % 3, i // 3
        if "ple" in parts:
            im["p_L%d" % i] = np.ascontiguousarray(inp["p"][i][seqs])
            im["ple_gate_w_L%d" % i] = inp["ple_gate_w"][i]
            im["ple_proj_w_L%d" % i] = inp["ple_proj_w"][i]
        if "ffn" in parts:
            im["router_w_L%d" % i] = inp["router_w"][i]
            im["exp_w_gate_L%d" % i] = inp["exp_w_gate"][i]
            im["exp_w_up_L%d" % i] = inp["exp_w_up"][i]
            im["exp_w_down_L%d" % i] = inp["exp_w_down"][i]
        if "mix" in parts:
            im["w_out_L%d" % i] = inp["w_out"][i]
            if kind == 0:
                im["w_in_L%d" % i] = inp["a_w_in"][j]
                im["a_vnorm_g_L%d" % i] = np.ascontiguousarray(inp["a_vnorm_g"][j].reshape(1, D))
                im["a_w_sT_L%d" % i] = np.ascontiguousarray(inp["a_w_s"][j].transpose(0, 2, 1))
                im["a_b_s_L%d" % i] = np.ascontiguousarray(inp["a_b_s"][j].reshape(1, D))
            elif kind == 1:
                im["w_in_L%d" % i] = inp["b_w_in"][j]
                im["qk_g_L%d" % i] = np.ascontiguousarray(
                    np.stack([np.tile(inp["b_qnorm_g"][j], 2), np.tile(inp["b_knorm_g"][j], 2)], 1).astype(f))
                im["b_sink_L%d" % i] = np.ascontiguousarray(inp["b_sink"][j].reshape(1, 16))
            else:
                im["w_in_L%d" % i] = inp["c_w_in"][j]
                im["qk_g_L%d" % i] = np.ascontiguousarray(
                    np.stack([np.tile(inp["c_qnorm_g"][j], 2), np.tile(inp["c_knorm_g"][j], 2)], 1).astype(f))
                im["c_rpbt_L%d" % i] = host_rpb_tables(inp["c_rpb"][j])
    return {k: np.ascontiguousarray(v, dtype=f) for k, v in im.items()}


LAUNCH_GROUPS = ((0,), (1,), (2,), (3,))


def kernel(**inputs):
    inp = {k: np.asarray(v) for k, v in inputs.items()}
    consts = host_consts()
    h = np.ascontiguousarray(inp["x"], dtype=np.float32)
    for layers in LAUNCH_GROUPS:
        nc, _ = build_program(SEQ_PER_CORE, layers)
        in_maps = [core_inputs(inp, list(range(cid * SEQ_PER_CORE, (cid + 1) * SEQ_PER_CORE)), layers,
                               x_override=h, consts=consts) for cid in range(N_CORES)]
        res = run_bass_kernel_spmd(nc, in_maps, core_ids=list(range(N_CORES)))
        h = np.concatenate([np.asarray(res.results[cid]["out"]) for cid in range(N_CORES)], axis=0)
    return h.astype(np.float32)
```

```python
import numpy as np
from contextlib import ExitStack
import concourse.bass as bass
import concourse.mybir as mybir
from concourse.bass_utils import run_bass_kernel_spmd

F32 = mybir.dt.float32
BF16 = mybir.dt.bfloat16
AF = mybir.ActivationFunctionType
ALU = mybir.AluOpType
AX = mybir.AxisListType

D = 1024
S = 2048
NT = 16
DEPTH = 4
NE = 16
CAP = 256
PLE = 256
N_CORES = 8
SEQ_PER_CORE = 4
GELU = AF.Gelu_apprx_tanh
BIG = 1.0e9
DBG_ATT = 9


class _Op:
    __slots__ = ("eng", "fn", "deps", "dsem", "waits", "semid", "semval", "marked")

    def __init__(self, eng, fn, deps, dsem):
        self.eng = eng
        self.fn = fn
        self.deps = deps
        self.dsem = dsem
        self.waits = []
        self.semid = None
        self.semval = 0
        self.marked = False


class Prog:
    EPOCH_E = 24000
    EPOCH_D = 1500
    SAME_ENGINE_SYNC = True
    ENGS = ("pe", "act", "dve", "pool")

    def __init__(self):
        self.ops = []
        self.lastw = {}
        self.readers = {}
        self.last_on = {}
        self.pending = {}

    def barrier(self):
        front = set(self.last_on.get(e) for e in self.ENGS if self.last_on.get(e) is not None)
        for e in self.ENGS + ("sp",):
            self.pending[e] = set(front) | self.pending.get(e, set())

    def add(self, eng, fn, r=(), w=(), dsem=None):
        w = list(w) + [k for k in r if isinstance(k, tuple) and k[0] == "ps" and k not in w]
        deps = set()
        for k in r:
            d = self.lastw.get(k)
            if d is not None:
                deps.add(d)
        for k in w:
            d = self.lastw.get(k)
            if d is not None:
                deps.add(d)
            deps.update(self.readers.get(k, ()))
        pend = self.pending.get(eng)
        if pend:
            deps.update(pend)
            self.pending[eng] = set()
        idx = len(self.ops)
        self.ops.append(_Op(eng, fn, deps, dsem))
        for k in r:
            self.readers.setdefault(k, set()).add(idx)
        for k in w:
            self.lastw[k] = idx
            self.readers[k] = set()
        if dsem is None:
            self.last_on[eng] = idx
        return idx

    def finalize(self):
        ops = self.ops
        for i, op in enumerate(ops):
            best = {}
            for d in op.deps:
                src = ops[d]
                if src.dsem is not None:
                    key = ("d", src.dsem)
                else:
                    if src.eng == op.eng and (op.eng == "pe" or not self.SAME_ENGINE_SYNC):
                        continue
                    key = ("e", src.eng)
                if key not in best or best[key] < d:
                    best[key] = d
            op.waits = sorted(best.values())
            for d in op.waits:
                ops[d].marked = True
        cnt = {}
        semids = set()
        for op in ops:
            if op.dsem is not None:
                k = ("d", op.dsem)
                c = cnt.get(k, 0)
                cnt[k] = c + 1
                op.semid = (k, c // self.EPOCH_D)
                op.semval = (c % self.EPOCH_D + 1) * 16
                op.marked = True
                semids.add(op.semid)
            elif op.marked:
                k = ("e", op.eng)
                c = cnt.get(k, 0)
                cnt[k] = c + 1
                op.semid = (k, c // self.EPOCH_E)
                op.semval = c % self.EPOCH_E + 1
                semids.add(op.semid)
        return sorted(semids, key=str)

    def emit(self, nc, stack, final_waits=()):
        semids = self.finalize()
        sems = {}
        for n, sid in enumerate(semids):
            sems[sid] = stack.enter_context(nc.semaphore("s%d" % n))
        ops = self.ops
        block = stack.enter_context(nc.Block())

        def run(engname, e):
            waited = {}
            for op in ops:
                if op.eng != engname:
                    continue
                for d in op.waits:
                    src = ops[d]
                    if waited.get(src.semid, 0) >= src.semval:
                        continue
                    waited[src.semid] = src.semval
                    e.wait_ge(sems[src.semid], src.semval)
                ins = op.fn(e)
                if op.marked:
                    ins.then_inc(sems[op.semid], 16 if op.dsem is not None else 1)
            if engname == "sp":
                for d in final_waits:
                    src = ops[d]
                    e.wait_ge(sems[src.semid], src.semval)

        @block.tensor
        def _(e):
            run("pe", e)

        @block.scalar
        def _(e):
            run("act", e)

        @block.vector
        def _(e):
            run("dve", e)

        @block.gpsimd
        def _(e):
            run("pool", e)

        @block.sync
        def _(e):
            run("sp", e)


def natten_win(m, kt):
    q = np.arange(128)
    k = np.arange(128)
    r = 2 * m + q // 64
    qc = q % 64
    rs = np.clip(r - 4, 0, 24)
    cs = np.clip(qc - 8, 0, 48)
    kr = 2 * kt + k // 64
    kc = k % 64
    rowok = (kr[:, None] >= rs[None, :]) & (kr[:, None] < rs[None, :] + 8)
    colok = (kc[:, None] >= cs[None, :]) & (kc[:, None] < cs[None, :] + 16)
    return (rowok & colok).astype(np.float32)


def c_key_tiles(m):
    if m <= 1:
        return [0, 1, 2, 3]
    if m >= 14:
        return [12, 13, 14, 15]
    return [m - 2, m - 1, m, m + 1, m + 2]


def c_mask_classes():
    classes = []
    index = {}
    for m in range(NT):
        for kt in c_key_tiles(m):
            mk = natten_win(m, kt)
            key = mk.tobytes()
            found = None
            for ci, (kb, _) in enumerate(classes):
                if kb == key:
                    found = ci
                    break
            if found is None:
                classes.append((key, mk))
                found = len(classes) - 1
            index[(m, kt)] = found
    return np.stack([c[1] for c in classes], 0), index


C_MASKS, C_MASK_INDEX = c_mask_classes()
N_CMASK = C_MASKS.shape[0]


def host_consts():
    k = np.arange(128)[:, None]
    q = np.arange(128)[None, :]
    dist = np.zeros((128, 3, 128), np.float32)
    for i, dm in enumerate((-1, 0, 1)):
        rel = np.abs(dm * 128 + k - q).astype(np.float32)
        dist[:, i, :] = np.where(rel <= 128, rel, BIG)
    sel = np.zeros((16, 16, 128), np.float32)
    for e in range(16):
        sel[e, e, :] = 1.0
    return {
        "ident": np.eye(128, dtype=np.float32),
        "iota_row": np.broadcast_to(np.arange(256, dtype=np.float32)[None, :], (128, 256)).copy(),
        "iota_part": np.stack([np.arange(128), np.arange(128) + 128], 1).astype(np.float32),
        "sel": sel,
        "distB": dist,
        "maskC": np.ascontiguousarray(C_MASKS.transpose(1, 0, 2)),
    }


def host_gains(norm_mix_g, norm_ffn_g, ple_norm_g):
    g = np.stack([norm_mix_g, norm_ffn_g, ple_norm_g], axis=1)
    return np.ascontiguousarray(g.reshape(DEPTH, 3, 8, 128).transpose(0, 1, 3, 2))


def host_rpb_tables(rpb):
    k = np.arange(128)
    q = np.arange(128)
    out = np.zeros((4, 128, 7, 4, 128), np.float32)
    for di, dm in enumerate(range(-3, 4)):
        dr = (2 * dm + k[:, None] // 64) - (q[None, :] // 64)
        dc = (k[:, None] % 64) - (q[None, :] % 64)
        ok = (np.abs(dr) <= 7) & (np.abs(dc) <= 15)
        ri = np.clip(dr + 7, 0, 14)
        ci = np.clip(dc + 15, 0, 30)
        for h in range(16):
            tab = rpb[h][ri, ci]
            tab = np.where(ok, tab, np.float32(0.0))
            hh = h % 4
            pp = (hh % 2) * 2 + hh // 2
            out[h // 4, :, di, pp, :] = tab
    return out.reshape(4, 128, 7 * 4 * 128)


def alibi_slope(h):
    return float(np.float32(2.0 ** (-8.0 * (h + 1) / 16)))


class Ctx:
    pass


def build_program(n_seq=SEQ_PER_CORE, layers=(0, 1, 2, 3), parts=("mix", "ffn", "ple")):
    nc = bass.Bass("TRN2", target_bir_lowering=False)
    P = Prog()
    c = Ctx()
    stack = ExitStack()

    def dram(name, shape, dt=F32, kind="ExternalInput"):
        return nc.dram_tensor(name, list(shape), dt, kind=kind).ap()

    c.x = dram("x", [n_seq, S, D])
    c.out = dram("out", [n_seq, S, D], kind="ExternalOutput")
    c.gains = dram("gains", [DEPTH, 3, 128, 8])
    c.ident = dram("ident", [128, 128])
    c.iota_row_d = dram("iota_row", [128, 256])
    c.iota_part_d = dram("iota_part", [128, 2])
    c.sel_d = dram("sel", [16, 16, 128])
    c.distB_d = dram("distB", [128, 3, 128])
    c.maskC_d = dram("maskC", [128, N_CMASK, 128])
    L = {}
    for i in layers:
        d = Ctx()
        L[i] = d
        kind = i % 3
        d.kind = kind
        if "ple" in parts:
            d.p = dram("p_L%d" % i, [n_seq, S, PLE])
            d.ple_gate_w = dram("ple_gate_w_L%d" % i, [D, D])
            d.ple_proj_w = dram("ple_proj_w_L%d" % i, [PLE, D])
        if "ffn" in parts:
            d.router_w = dram("router_w_L%d" % i, [D, NE])
            d.wg = dram("exp_w_gate_L%d" % i, [NE, D, D])
            d.wu = dram("exp_w_up_L%d" % i, [NE, D, D])
            d.wd = dram("exp_w_down_L%d" % i, [NE, D, D])
        if "mix" in parts:
            d.w_out = dram("w_out_L%d" % i, [D, D])
            if kind == 0:
                d.w_in = dram("w_in_L%d" % i, [D, 2048])
                d.vnorm_g = dram("a_vnorm_g_L%d" % i, [1, D])
                d.w_sT = dram("a_w_sT_L%d" % i, [8, 128, 128])
                d.b_s = dram("a_b_s_L%d" % i, [1, D])
            elif kind == 1:
                d.w_in = dram("w_in_L%d" % i, [D, 1536])
                d.qk_g = dram("qk_g_L%d" % i, [128, 2])
                d.sink = dram("b_sink_L%d" % i, [1, 16])
            else:
                d.w_in = dram("w_in_L%d" % i, [D, 3072])
                d.qk_g = dram("qk_g_L%d" % i, [128, 2])
                d.rpbt = dram("c_rpbt_L%d" % i, [4, 128, 7 * 4 * 128])

    def sb(name, shape, dt):
        return stack.enter_context(nc.sbuf_tensor(name, list(shape), dt))

    def ps(name, shape, dt):
        return stack.enter_context(nc.psum_tensor(name, list(shape), dt))

    c.h = sb("h", [128, NT, D], F32)
    c.R1 = sb("R1", [128, 8 * S], BF16)
    c.R2 = sb("R2", [128, 8 * S], BF16)
    c.W = [sb("W%d" % i, [128, 8, 512], BF16) for i in range(6)]
    c.R3 = sb("R3", [128, 10240], BF16)
    c.gains_sb = sb("gains_sb", [128, DEPTH * 3 * 8], F32)
    c.ident_f = sb("ident_f", [128, 128], F32)
    c.ident_b = sb("ident_b", [128, 128], BF16)
    c.iota_row = sb("iota_row_sb", [128, 256], F32)
    c.iota_part = sb("iota_part_sb", [128, 2], F32)
    c.sel = sb("sel_sb", [16, 16, 128], BF16)
    c.ones_row = sb("ones_row", [1, 128], BF16)
    c.mhalf = sb("mhalf", [128, 32], F32)
    c.stat = sb("stat", [128, 96], F32)
    c.aff_tok = sb("aff_tok", [128, NT, NE], F32)
    c.slot_tok = sb("slot_tok", [128, NT, NE], F32)
    c.rw = sb("rw", [128, 8, NE], F32)
    c.rwg = sb("rwg", [128, 8, NE], F32)
    c.qkg = sb("qkg", [128, 4], F32)
    c.esink = sb("esink", [128, 16], F32)
    c.psb = [ps("psb%d" % i, [128, 512], F32) for i in range(8)]

    def r2(off, n, dt):
        v = c.R2[:, off // 2:(off + n) // 2]
        return v if dt == BF16 else v.bitcast(F32)

    def r3(off, n, dt, parts=128):
        v = c.R3[0:parts, off // 2:(off + n) // 2]
        return v if dt == BF16 else v.bitcast(F32)

    xnT = c.R1[:].rearrange("p (c t) -> p c t", c=8)
    hs = c.R1[:].rearrange("p (t d) -> p t d", t=NT)
    mixT = c.R2[:].rearrange("p (c t) -> p c t", c=8)

    def psbf(b):
        return c.psb[b][:].bitcast(BF16)

    def MM(out, lhsT, rhs, start, stop, r, w, skip=False):
        return P.add("pe", lambda e: e.matmul(out, lhsT=lhsT, rhs=rhs, start=start, stop=stop, skip_group_check=skip),
                     r=r, w=w)

    def TR(out, in_, ident, r, w):
        return P.add("pe", lambda e: e.transpose(out=out, in_=in_, identity=ident), r=r, w=w)

    def ACT(out, in_, func, r, w, **kw):
        return P.add("act", lambda e: e.activation(out=out, in_=in_, func=func, **kw), r=r, w=w)

    def TT(eng, out, in0, in1, op, r, w):
        return P.add(eng, lambda e: e.tensor_tensor(out=out, in0=in0, in1=in1, op=op), r=r, w=w)

    def TS(eng, out, in0, s1, s2, op0, op1, r, w):
        if op1 is None:
            return P.add(eng, lambda e: e.tensor_scalar(out=out, in0=in0, scalar1=s1, scalar2=None, op0=op0), r=r, w=w)
        return P.add(eng, lambda e: e.tensor_scalar(out=out, in0=in0, scalar1=s1, scalar2=s2, op0=op0, op1=op1), r=r, w=w)

    def STT(out, in0, scalar, in1, op0, op1, r, w):
        return P.add("dve", lambda e: e.scalar_tensor_tensor(out=out, in0=in0, scalar=scalar, in1=in1, op0=op0, op1=op1),
                     r=r, w=w)

    def CP(eng, out, in_, r, w):
        return P.add(eng, lambda e: e.tensor_copy(out=out, in_=in_), r=r, w=w)

    def DMA(eng, out, in_, r, w, dsem):
        return P.add(eng, lambda e: e.dma_start(out=out, in_=in_), r=r, w=w, dsem=dsem)

    DMA("sp", c.gains_sb[:].rearrange("p (a c) -> p a c", c=8), c.gains.rearrange("l k p c -> p (l k) c"),
        [], ["gains_sb"], "c_gains")
    DMA("sp", c.ident_f[:], c.ident, [], ["ident_f"], "c_ident")
    DMA("sp", c.iota_row[:], c.iota_row_d, [], ["iota_row"], "c_iotar")
    DMA("sp", c.iota_part[:], c.iota_part_d, [], ["iota_part"], "c_iotap")
    DMA("pool", c.sel[:], c.sel_d, [], ["sel"], "c_sel")
    CP("dve", c.ident_b[:], c.ident_f[:], ["ident_f"], ["ident_b"])
    P.add("pool", lambda e: e.memset(c.ones_row[:], 1.0), w=["ones_row"])
    P.add("pool", lambda e: e.memset(c.mhalf[:], -0.5), w=["mhalf"])

    def gain_ap(layer, which, dc):
        o = (layer * 3 + which) * 8 + dc
        return c.gains_sb[:, o:o + 1]

    def load_w(slot, src2d, kc, n0, n=512, col0=0):
        dst = c.W[slot][:, 0:kc, col0:col0 + n]
        return DMA("pool", dst, src2d[:, n0:n0 + n].rearrange("(c p) n -> p c n", p=128), [], [("W", slot)], ("W", slot))

    def rstd_rms(dst_col0):
        junk = r3(0, 2048, BF16)
        for tt in range(NT):
            ACT(junk, c.h[:, tt, :], AF.Square, [("h", tt)], ["junk", ("ss", tt)], accum_out=c.stat[:, tt:tt + 1])
        TS("pool", c.stat[:, 16:32], c.stat[:, 0:16], 1.0 / D, 1e-6, ALU.mult, ALU.add,
           [("ss", t) for t in range(NT)], ["ms"])
        TT("pool", c.stat[:, dst_col0:dst_col0 + 16], c.stat[:, 16:32], c.mhalf[:, 0:16], ALU.pow, ["ms", "mhalf"], ["rstd"])

    def rms_to_xnT(layer, which):
        P.barrier()
        rstd_rms(32)
        hsb = r3(2048, 4096, BF16).rearrange("p (s d) -> p s d", s=2)
        for g4 in range(NT // 4):
            for j in range(4):
                tt = g4 * 4 + j
                slot = tt % 2
                TS("dve", hsb[:, slot, :], c.h[:, tt, :], c.stat[:, 32 + tt:33 + tt], None, ALU.mult, None,
                   [("h", tt), "rstd"], [("hsb", slot)])
                for dc in range(8):
                    TR(psbf(dc)[:, j * 128:(j + 1) * 128], hsb[:, slot, dc * 128:(dc + 1) * 128], c.ident_b[:],
                       [("hsb", slot), "ident_b"], [("ps", dc)])
            for dc in range(8):
                if dc % 2 == 0:
                    ACT(xnT[:, dc, g4 * 512:(g4 + 1) * 512], psbf(dc)[:, 0:512], AF.Copy,
                        [("ps", dc), "gains_sb"], [("xnT", g4)], scale=gain_ap(layer, which, dc))
                else:
                    TS("dve", xnT[:, dc, g4 * 512:(g4 + 1) * 512], psbf(dc)[:, 0:512], gain_ap(layer, which, dc), None,
                       ALU.mult, None, [("ps", dc), "gains_sb"], [("xnT", g4)])

    def ple_block(layer, seq):
        d = L[layer]
        rms_to_xnT(layer, 2)
        pbuf = r2(0, 16384, F32).rearrange("p (t k) -> p t k", k=PLE)
        pT = r2(16384, 8192, BF16).rearrange("p (k t) -> p k t", k=2)
        hsb = r3(2048, 4096, BF16).rearrange("p (s d) -> p s d", s=2)
        tmpA = r3(6144, 4096, F32).rearrange("p (s d) -> p s d", s=2)
        tmpB = r3(10240, 4096, F32).rearrange("p (s d) -> p s d", s=2)
        load_w(0, d.ple_gate_w, 8, 0)
        load_w(1, d.ple_gate_w, 8, 512)
        load_w(2, d.ple_proj_w, 2, 0)
        load_w(3, d.ple_proj_w, 2, 512)
        DMA("sp", pbuf, d.p[seq].rearrange("(t p) k -> p t k", p=128), [], ["pbuf"], "pbuf")
        for tt in range(NT):
            slot = tt % 2
            CP("dve", hsb[:, slot, 0:PLE], pbuf[:, tt, :], ["pbuf"], [("hsb", slot)])
            bank = tt % 2
            for kc in range(2):
                TR(psbf(bank)[:, kc * 128:(kc + 1) * 128], hsb[:, slot, kc * 128:(kc + 1) * 128], c.ident_b[:],
                   [("hsb", slot), "ident_b"], [("ps", bank)])
            ACT(pT[:, :, tt * 128:(tt + 1) * 128], psbf(bank)[:, 0:256].rearrange("p (k t) -> p k t", k=2), AF.Copy,
                [("ps", bank)], ["pT"])
        n = 0
        for tt in range(NT):
            for half in range(2):
                bG = 2 + (n % 3) * 2
                bP = bG + 1
                s2 = n % 2
                n += 1
                for kc in range(8):
                    MM(c.psb[bG][:], xnT[:, kc, tt * 128:(tt + 1) * 128], c.W[half][:, kc, :], kc == 0, kc == 7,
                       [("xnT", tt // 4), ("W", half)], [("ps", bG)])
                for kc in range(2):
                    MM(c.psb[bP][:], pT[:, kc, tt * 128:(tt + 1) * 128], c.W[2 + half][:, kc, :], kc == 0, kc == 1,
                       ["pT", ("W", 2 + half)], [("ps", bP)])
                ACT(tmpA[:, s2, :], c.psb[bG][:], AF.Sigmoid, [("ps", bG)], [("tmpA", s2)])
                TT("dve", tmpB[:, s2, :], tmpA[:, s2, :], c.psb[bP][:], ALU.mult, [("tmpA", s2), ("ps", bP)], [("tmpB", s2)])
                hv = c.h[:, tt, half * 512:(half + 1) * 512]
                TT("pool", hv, hv, tmpB[:, s2, :], ALU.add, [("tmpB", s2), ("h", tt)], [("h", tt)])

    def wout_full(layer):
        n = 0
        for tt in range(NT):
            for half in range(2):
                b = n % 4
                n += 1
                for kc in range(8):
                    MM(c.psb[b][:], mixT[:, kc, tt * 128:(tt + 1) * 128], c.W[4 + half][:, kc, :], kc == 0, kc == 7,
                       [("mixT", tt // 4), ("W", 4 + half)], [("ps", b)])
                hv = c.h[:, tt, half * 512:(half + 1) * 512]
                TT("dve", hv, hv, c.psb[b][:], ALU.add, [("ps", b), ("h", tt)], [("h", tt)])

    def mixer_a(layer, seq):
        d = L[layer]
        rms_to_xnT(layer, 0)
        load_w(2, d.w_in, 8, 1024)
        load_w(3, d.w_in, 8, 1536)
        load_w(0, d.w_in, 8, 0)
        load_w(1, d.w_in, 8, 512)
        load_w(4, d.w_out, 8, 0)
        load_w(5, d.w_out, 8, 512)
        vg = r3(6144, 4096, F32)
        wsT = r3(10240, 2048, BF16).rearrange("p (g t) -> p g t", g=8)
        bs = r3(12288, 2048, BF16, parts=1)
        vt = r3(14336, 4096, F32)
        vn = r3(18432, 2048, BF16)
        DMA("sp", vg, d.vnorm_g[0:1, :].to_broadcast([128, D]), [], ["vg"], "a_vg")
        DMA("pool", wsT, d.w_sT.rearrange("g s t -> s g t"), [], ["wsT"], "a_wsT")
        DMA("pool", bs, d.b_s, [], ["bs"], "a_bs")
        st6 = c.stat[:, 48:60].rearrange("p (a b) -> p a b", a=2)
        mv = c.stat[:, 60:62]
        for tt in range(NT):
            for half in range(2):
                b = half
                for kc in range(8):
                    MM(c.psb[b][:], xnT[:, kc, tt * 128:(tt + 1) * 128], c.W[2 + half][:, kc, :], kc == 0, kc == 7,
                       [("xnT", tt // 4), ("W", 2 + half)], [("ps", b)])
                ACT(vt[:, half * 512:(half + 1) * 512], c.psb[b][:], GELU, [("ps", b)], ["vt"])
                P.add("dve", lambda e, half=half: e.bn_stats(out=st6[:, half, :], in_=vt[:, half * 512:(half + 1) * 512]),
                      r=["vt"], w=["st6"])
            P.add("dve", lambda e: e.bn_aggr(out=mv, in_=c.stat[:, 48:60]), r=["st6"], w=["mv"])
            TS("pool", c.stat[:, 62:63], c.stat[:, 61:62], 1e-5, None, ALU.add, None, ["mv"], ["lnv"])
            TT("pool", c.stat[:, 63:64], c.stat[:, 62:63], c.mhalf[:, 0:1], ALU.pow, ["lnv", "mhalf"], ["lnr"])
            TS("dve", vt, vt, c.stat[:, 60:61], c.stat[:, 63:64], ALU.subtract, ALU.mult, ["vt", "mv", "lnr"], ["vt"])
            TT("dve", vn, vt, vg, ALU.mult, ["vt", "vg"], ["vn"])
            for g in range(8):
                b = 2 + g // 4
                col = (g % 4) * 128
                MM(c.psb[b][:, col:col + 128], vn[:, g * 128:(g + 1) * 128], wsT[:, g, :], True, False,
                   ["vn", "wsT"], [("ps", b)])
                MM(c.psb[b][:, col:col + 128], c.ones_row[0:1, :], bs[0:1, g * 128:(g + 1) * 128], False, True,
                   ["ones_row", "bs"], [("ps", b)])
            for b2 in range(2):
                outv = mixT[:, b2 * 4:(b2 + 1) * 4, tt * 128:(tt + 1) * 128]
                inv = c.psb[2 + b2][:].rearrange("p (g t) -> p g t", g=4)
                if b2 == 0:
                    ACT(outv, inv, AF.Copy, [("ps", 2 + b2)], [("mixT", tt // 4)])
                else:
                    CP("dve", outv, inv, [("ps", 2 + b2)], [("mixT", tt // 4)])
        P.barrier()
        gu = r3(14336, 4096, F32).rearrange("p (s d) -> p s d", s=2)
        n = 0
        for fc in range(8):
            for tq in range(4):
                b = n % 4
                s2 = n % 2
                n += 1
                for kc in range(8):
                    MM(c.psb[b][:], c.W[fc // 4][:, kc, (fc % 4) * 128:(fc % 4 + 1) * 128], xnT[:, kc, tq * 512:(tq + 1) * 512],
                       kc == 0, kc == 7, [("xnT", tq), ("W", fc // 4)], [("ps", b)])
                ACT(gu[:, s2, :], c.psb[b][:], GELU, [("ps", b)], [("gu", s2)])
                mv_ = mixT[:, fc, tq * 512:(tq + 1) * 512]
                TT("dve" if n % 2 else "pool", mv_, mv_, gu[:, s2, :], ALU.mult, [("gu", s2), ("mixT", tq)], [("mixT", tq)])
        wout_full(layer)

    def attention(layer, seq):
        d = L[layer]
        isB = (d.kind == 1)
        rms_to_xnT(layer, 0)
        P.barrier()
        qT = r2(0, 8192, BF16).rearrange("p (c t) -> p c t", c=2)
        kT = r2(8192, 8192, BF16).rearrange("p (c t) -> p c t", c=2)
        V = r2(16384, 8448, BF16).rearrange("p (t h e) -> p t h e", t=NT, h=4)
        sq = r3(0, 2048, F32)
        qtok = r3(2048, 1024, BF16).rearrange("p (s d) -> p s d", s=2)
        ktok = r3(3072, 1024, BF16).rearrange("p (s d) -> p s d", s=2)
        tS = r3(4096, 4096, F32).rearrange("p (s h q) -> p s h q", s=2, h=4)
        Eb = r3(8192, 2048, BF16).rearrange("p (s h q) -> p s h q", s=2, h=4)
        PTb = r3(10240, 2048, BF16).rearrange("p (s h q) -> p s h q", s=2, h=4)
        otok = r3(12288, 1024, BF16).rearrange("p (s d) -> p s d", s=2)
        oT = r3(13312, 1024, BF16).rearrange("p (s k t) -> p s k t", s=2, k=2)
        b4 = b5 = distB = maskC = None
        if isB:
            distB = r3(14336, 1536, F32).rearrange("p (a q) -> p a q", a=3)
            DMA("sp", distB, c.distB_d, [], ["tab"], "tabB")
            DMA("sp", c.esink[:], d.sink[0:1, :].to_broadcast([128, 16]), [], ["esink"], "sink")
            ACT(c.esink[:], c.esink[:], AF.Exp, ["esink"], ["esink"])
        else:
            maskC = r3(14336, N_CMASK * 256, BF16).rearrange("p (a q) -> p a q", a=N_CMASK)
            DMA("pool", maskC, c.maskC_d, [], ["tab"], "tabC")
            b4 = c.W[4][:].rearrange("p a b -> p (a b)").bitcast(F32).rearrange("p (m h q) -> p m h q", m=4, h=4)
            b5 = c.W[5][:].rearrange("p a b -> p (a b)").bitcast(F32).rearrange("p (m h q) -> p m h q", m=4, h=4)
        DMA("sp", c.qkg[:, 0:2], d.qk_g, [], ["qkg"], "qkg")
        TS("pool", c.qkg[:, 2:3], c.qkg[:, 0:1], 0.125, None, ALU.mult, None, ["qkg"], ["qkg2"])
        P.add("pool", lambda e: e.memset(V[:, :, :, 64:65], 1.0), w=["V"])
        load_w(2, d.w_out, 8, 0)
        load_w(3, d.w_out, 8, 512)
        for gi in range(4):
            if isB:
                load_w(0, d.w_in, 8, gi * 256, n=256)
                load_w(1, d.w_in, 8, 1024 + gi * 64, n=64, col0=0)
                load_w(1, d.w_in, 8, 1280 + gi * 64, n=64, col0=64)
            else:
                load_w(0, d.w_in, 8, gi * 256, n=256, col0=0)
                load_w(0, d.w_in, 8, 1024 + gi * 256, n=256, col0=256)
                load_w(1, d.w_in, 8, 2048 + gi * 256, n=256, col0=0)
                src = d.rpbt[gi].rearrange("p (m h q) -> p m h q", m=7, h=4)
                DMA("sp", b4, src[:, 0:4], [], [("W", 4)], ("W", 4))
                DMA("sp", b5[:, 0:3], src[:, 4:7], [], [("W", 5)], ("W", 5))
            for tt in range(NT if DBG_ATT >= 1 else 0):
                s2 = tt % 2
                bq = s2
                bkv = 2 + s2
                for kc in range(8):
                    MM(c.psb[bq][:, 0:256], xnT[:, kc, tt * 128:(tt + 1) * 128], c.W[0][:, kc, 0:256], kc == 0, kc == 7,
                       [("xnT", tt // 4), ("W", 0)], [("ps", bq)])
                if isB:
                    for kc in range(8):
                        MM(c.psb[bkv][:, 0:128], xnT[:, kc, tt * 128:(tt + 1) * 128], c.W[1][:, kc, 0:128], kc == 0, kc == 7,
                           [("xnT", tt // 4), ("W", 1)], [("ps", bkv)])
                    kps = c.psb[bkv][:, 0:64]
                    vps = c.psb[bkv][:, 64:128]
                    nk = 64
                else:
                    for kc in range(8):
                        MM(c.psb[bkv][:, 0:256], xnT[:, kc, tt * 128:(tt + 1) * 128], c.W[0][:, kc, 256:512], kc == 0, kc == 7,
                           [("xnT", tt // 4), ("W", 0)], [("ps", bkv)])
                    for kc in range(8):
                        MM(c.psb[bkv][:, 256:512], xnT[:, kc, tt * 128:(tt + 1) * 128], c.W[1][:, kc, 0:256], kc == 0, kc == 7,
                           [("xnT", tt // 4), ("W", 1)], [("ps", bkv)])
                    kps = c.psb[bkv][:, 0:256]
                    vps = c.psb[bkv][:, 256:512]
                    nk = 256
                ACT(sq[:, 0:256], c.psb[bq][:, 0:256], AF.Square, [("ps", bq)], ["sq"])
                ACT(sq[:, 256:256 + nk], kps, AF.Square, [("ps", bkv)], ["sq"])
                nh = 4 + nk // 64
                ssq = c.stat[:, 64:64 + nh]
                P.add("dve", lambda e, nh=nh, ssq=ssq: e.tensor_reduce(
                    out=ssq, in_=sq[:, 0:nh * 64].rearrange("p (h e) -> p h e", e=64), axis=AX.X, op=ALU.add),
                    r=["sq"], w=["ssq"])
                TS("pool", c.stat[:, 72:72 + nh], ssq, 1.0 / 64, 1e-6, ALU.mult, ALU.add, ["ssq"], ["qms"])
                TT("pool", c.stat[:, 80:80 + nh], c.stat[:, 72:72 + nh], c.mhalf[:, 0:nh], ALU.pow, ["qms", "mhalf"], ["qrs"])
                TT("dve", qtok[:, s2, :].rearrange("p (h e) -> p h e", e=64),
                   c.psb[bq][:, 0:256].rearrange("p (h e) -> p h e", e=64),
                   c.stat[:, 80:84].unsqueeze(2).to_broadcast([128, 4, 64]), ALU.mult,
                   [("ps", bq), "qrs"], [("qtok", s2)])
                if isB:
                    for dup in range(2):
                        TS("dve", ktok[:, s2, dup * 64:(dup + 1) * 64], kps, c.stat[:, 84:85], None, ALU.mult, None,
                           [("ps", bkv), "qrs"], [("ktok", s2)])
                    ACT(V[:, tt, 0, 0:64], vps, AF.Copy, [("ps", bkv)], ["V"])
                else:
                    TT("dve", ktok[:, s2, :].rearrange("p (h e) -> p h e", e=64),
                       kps.rearrange("p (h e) -> p h e", e=64),
                       c.stat[:, 84:88].unsqueeze(2).to_broadcast([128, 4, 64]), ALU.mult,
                       [("ps", bkv), "qrs"], [("ktok", s2)])
                    ACT(V[:, tt, :, 0:64], vps.rearrange("p (h e) -> p h e", e=64), AF.Copy, [("ps", bkv)], ["V"])
                bt = 4 + s2
                for pr in range(2):
                    TR(psbf(bt)[:, pr * 128:(pr + 1) * 128], qtok[:, s2, pr * 128:(pr + 1) * 128], c.ident_b[:],
                       [("qtok", s2), "ident_b"], [("ps", bt)])
                nkp = 1 if isB else 2
                for pr in range(nkp):
                    TR(psbf(bt)[:, 256 + pr * 128:256 + (pr + 1) * 128], ktok[:, s2, pr * 128:(pr + 1) * 128], c.ident_b[:],
                       [("ktok", s2), "ident_b"], [("ps", bt)])
                ACT(qT[:, :, tt * 128:(tt + 1) * 128], psbf(bt)[:, 0:256].rearrange("p (c t) -> p c t", c=2), AF.Copy,
                    [("ps", bt), "qkg2"], ["qT"], scale=c.qkg[:, 2:3])
                TS("dve", kT[:, 0:nkp, tt * 128:(tt + 1) * 128],
                   psbf(bt)[:, 256:256 + nkp * 128].rearrange("p (c t) -> p c t", c=nkp), c.qkg[:, 1:2], None, ALU.mult, None,
                   [("ps", bt), "qkg"], ["kT"])
            step = 0
            for m in range(NT if DBG_ATT >= 2 else 0):
                if isB:
                    kts = [kt for kt in (m - 1, m, m + 1) if 0 <= kt < NT]
                else:
                    kts = c_key_tiles(m)
                bo = 4 + m % 2
                ops_ = c.psb[bo][:, 0:260].rearrange("p (h e) -> p h e", e=65)
                for ki, kt in enumerate(kts):
                    s2 = step % 2
                    step += 1
                    for pp in range(4):
                        hf, j = pp // 2, pp % 2
                        bsc = s2 * 2 + hf
                        kpr = 0 if isB else j
                        MM(c.psb[bsc][:, j * 128:(j + 1) * 128],
                           kT[hf * 64:(hf + 1) * 64, kpr, kt * 128:(kt + 1) * 128],
                           qT[hf * 64:(hf + 1) * 64, j, m * 128:(m + 1) * 128], True, True,
                           ["qT", "kT"], [("ps", bsc)])
                    if isB:
                        dmi = kt - m + 1
                        for pp in range(4):
                            hf, j = pp // 2, pp % 2
                            bsc = s2 * 2 + hf
                            STT(tS[:, s2, pp, :], distB[:, dmi, :], -alibi_slope(gi * 4 + 2 * j + hf),
                                c.psb[bsc][:, j * 128:(j + 1) * 128], ALU.mult, ALU.add,
                                [("ps", bsc), "tab"], [("tS", s2)])
                        ACT(PTb[:, s2], tS[:, s2], AF.Exp, [("tS", s2)], [("PT", s2)])
                    else:
                        dmi = kt - m + 3
                        btab = (b4[:, dmi] if dmi < 4 else b5[:, dmi - 4])
                        for hf in range(2):
                            bsc = s2 * 2 + hf
                            TT("dve", tS[:, s2, 2 * hf:2 * hf + 2, :],
                               c.psb[bsc][:, 0:256].rearrange("p (h q) -> p h q", h=2), btab[:, 2 * hf:2 * hf + 2, :], ALU.add,
                               [("ps", bsc), ("W", 4), ("W", 5)], [("tS", s2)])
                        ACT(Eb[:, s2], tS[:, s2], AF.Exp, [("tS", s2)], [("E", s2)])
                        ci = C_MASK_INDEX[(m, kt)]
                        TT("pool", PTb[:, s2], Eb[:, s2], maskC[:, ci:ci + 1, :].to_broadcast([128, 4, 128]), ALU.mult,
                           [("E", s2), "tab"], [("PT", s2)])
                    for pp in range(4 if DBG_ATT >= 3 else 0):
                        hh = 2 * (pp % 2) + pp // 2
                        vh = 0 if isB else hh
                        MM(ops_[:, hh, :], PTb[:, s2, pp, :], V[:, kt, vh, 0:65], ki == 0 and pp == 0, ki == len(kts) - 1,
                           [("PT", s2), "V"], [("ps", bo)], skip=True)
                if DBG_ATT < 4:
                    continue
                o2 = m % 2
                den = c.stat[:, 88:92]
                if isB:
                    TT("dve", den, ops_[:, :, 64], c.esink[:, gi * 4:(gi + 1) * 4], ALU.add, [("ps", bo), "esink"], ["den"])
                else:
                    CP("dve", den, ops_[:, :, 64], [("ps", bo)], ["den"])
                P.add("dve", lambda e, den=den: e.reciprocal(out=c.stat[:, 92:96], in_=den), r=["den"], w=["rden"])
                TT("dve", otok[:, o2, :].rearrange("p (h e) -> p h e", e=64), ops_[:, :, 0:64],
                   c.stat[:, 92:96].unsqueeze(2).to_broadcast([128, 4, 64]), ALU.mult,
                   [("ps", bo), "rden"], [("otok", o2)])
                bt = 6
                for pr in range(2):
                    TR(psbf(bt)[:, pr * 128:(pr + 1) * 128], otok[:, o2, pr * 128:(pr + 1) * 128], c.ident_b[:],
                       [("otok", o2), "ident_b"], [("ps", bt)])
                ACT(oT[:, o2], psbf(bt)[:, 0:256].rearrange("p (k t) -> p k t", k=2), AF.Copy, [("ps", bt)], [("oT", o2)])
                for half in range(2):
                    bw = 7
                    for k2 in range(2):
                        MM(c.psb[bw][:], oT[:, o2, k2, :], c.W[2 + half][:, gi * 2 + k2, :], k2 == 0, k2 == 1,
                           [("oT", o2), ("W", 2 + half)], [("ps", bw)])
                    hv = c.h[:, m, half * 512:(half + 1) * 512]
                    TT("dve", hv, hv, c.psb[bw][:], ALU.add, [("ps", bw), ("h", m)], [("h", m)])

    def ffn(layer, seq):
        d = L[layer]
        P.barrier()
        DMA("sp", c.rw[:], d.router_w.rearrange("(c p) e -> p c e", p=128), [], ["rw"], "rw")
        o = (layer * 3 + 1) * 8
        TT("dve", c.rwg[:], c.rw[:], c.gains_sb[:, o:o + 8].unsqueeze(2).to_broadcast([128, 8, NE]), ALU.mult,
           ["rw", "gains_sb"], ["rwg"])
        wslot = {"g": (0, 1), "u": (2, 3), "d": (4, 5)}

        def load_expert(e, which, half):
            src = {"g": d.wg, "u": d.wu, "d": d.wd}[which][e]
            load_w(wslot[which][half], src, 8, half * 512)

        for which in ("g", "u", "d"):
            for half in range(2):
                load_expert(0, which, half)
        rstd_rms(32)
        hsf = r3(2048, 8192, F32).rearrange("p (s d) -> p s d", s=2)
        hsfT = r3(10240, 8192, F32).rearrange("p (s c t) -> p s c t", s=2, c=8)
        bL = 7
        for tt in range(NT):
            s2 = tt % 2
            TS("dve", hs[:, tt, :], c.h[:, tt, :], c.stat[:, 32 + tt:33 + tt], None, ALU.mult, None,
               [("h", tt), "rstd"], [("hs", tt)])
            ACT(hsf[:, s2, :], c.h[:, tt, :], AF.Copy, [("h", tt), "rstd"], [("hsf", s2)], scale=c.stat[:, 32 + tt:33 + tt])
            for dc in range(8):
                b = s2 * 2 + dc // 4
                TR(c.psb[b][:, (dc % 4) * 128:(dc % 4 + 1) * 128], hsf[:, s2, dc * 128:(dc + 1) * 128], c.ident_f[:],
                   [("hsf", s2), "ident_f"], [("ps", b)])
            for b2 in range(2):
                b = s2 * 2 + b2
                outv = hsfT[:, s2, b2 * 4:(b2 + 1) * 4, :]
                inv = c.psb[b][:].rearrange("p (c t) -> p c t", c=4)
                if b2 == 0:
                    ACT(outv, inv, AF.Copy, [("ps", b)], [("hsfT", s2)])
                else:
                    CP("dve", outv, inv, [("ps", b)], [("hsfT", s2)])
            for dc in range(8):
                MM(c.psb[bL][:, tt * 16:(tt + 1) * 16], hsfT[:, s2, dc, :], c.rwg[:, dc, :], dc == 0, dc == 7,
                   [("hsfT", s2), "rwg"], [("ps", bL)])
        lg = c.psb[bL][:, 0:256].rearrange("p (t e) -> p t e", e=NE)
        mx = c.stat[:, 0:16]
        P.add("dve", lambda e: e.tensor_reduce(out=mx, in_=lg, axis=AX.X, op=ALU.max), r=[("ps", bL)], w=["mx"])
        TT("dve", c.aff_tok[:], lg, mx.unsqueeze(2).to_broadcast([128, NT, NE]), ALU.subtract, [("ps", bL), "mx"], ["aff"])
        ACT(c.aff_tok[:], c.aff_tok[:], AF.Exp, ["aff"], ["aff"])
        sm = c.stat[:, 16:32]
        P.add("dve", lambda e: e.tensor_reduce(out=sm, in_=c.aff_tok[:], axis=AX.X, op=ALU.add), r=["aff"], w=["sm"])
        P.add("dve", lambda e: e.reciprocal(out=sm, in_=sm), r=["sm"], w=["sm"])
        TT("dve", c.aff_tok[:], c.aff_tok[:], sm.unsqueeze(2).to_broadcast([128, NT, NE]), ALU.mult, ["aff", "sm"], ["aff"])
        P.barrier()
        affT = r2(0, 8192, F32)[0:16, :]
        work = r2(8192, 8192, F32)[0:16, :]
        cum = r2(16384, 8192, F32)[0:16, :]
        slotbf = r3(0, 4096, BF16)[0:16, :]
        for q4 in range(4):
            for j in range(4):
                tt = q4 * 4 + j
                TR(c.psb[q4][0:16, j * 128:(j + 1) * 128], c.aff_tok[:, tt, :], c.ident_f[:], ["aff", "ident_f"], [("ps", q4)])
            ACT(affT[:, q4 * 512:(q4 + 1) * 512], c.psb[q4][0:16, :], AF.Copy, [("ps", q4)], ["affT"])
        CP("dve", work, affT, ["affT"], ["work"])
        m8 = c.stat[0:16, 48:56]
        for it in range(CAP // 8):
            P.add("dve", lambda e: e.max(out=m8, in_=work), r=["work"], w=["m8"])
            if it < CAP // 8 - 1:
                P.add("dve", lambda e: e.match_replace(out=work, in_to_replace=m8, in_values=work, imm_value=-1.0),
                      r=["work", "m8"], w=["work"])
        TS("dve", work, affT, c.stat[0:16, 55:56], None, ALU.is_ge, None, ["affT", "m8"], ["work"])
        P.add("dve", lambda e: e.tensor_tensor_scan(out=cum, data0=work, data1=work, initial=0.0, op0=ALU.add, op1=ALU.max),
              r=["work"], w=["cum"])
        STT(cum, cum, -513.0, work, ALU.add, ALU.mult, ["cum", "work"], ["cum"])
        TS("dve", cum, cum, 512.0, None, ALU.add, None, ["cum"], ["cum"])
        CP("dve", slotbf, cum, ["cum"], ["slotbf"])
        bS = 4
        for tt in range(NT):
            TR(c.psb[bS][:, tt * 16:(tt + 1) * 16], cum[:, tt * 128:(tt + 1) * 128], c.ident_f[0:16, 0:16],
               ["cum", "ident_f"], [("ps", bS)])
        CP("dve", c.slot_tok[:].rearrange("p t e -> p (t e)"), c.psb[bS][:, 0:256], [("ps", bS)], ["slot_tok"])
        P.barrier()
        Pm = r2(0, 8192, BF16).rearrange("p (t s) -> p t s", t=NT)
        PT = r2(8192, 8192, BF16).rearrange("p (s t) -> p s t", s=2)
        xsT = r2(16384, 4096, BF16).rearrange("p (c s) -> p c s", c=8)
        hT = r2(20480, 4096, BF16).rearrange("p (c s) -> p c s", c=8)
        Y = r2(24576, 4096, BF16).rearrange("p (s d) -> p s d", s=2)
        sg = r3(4096, 2048, F32).rearrange("p (s d) -> p s d", s=2)
        PTsb = r3(6144, 4096, BF16)
        for ex in range(NE):
            TT("dve", Pm, c.iota_row[:].unsqueeze(1).to_broadcast([128, NT, CAP]),
               c.slot_tok[:, :, ex:ex + 1].to_broadcast([128, NT, CAP]), ALU.is_equal,
               ["iota_row", "slot_tok"], ["Pm"])
            for q4 in range(4):
                MM(c.psb[q4][:], c.sel[:, ex, :], slotbf[:, q4 * 512:(q4 + 1) * 512], True, True,
                   ["sel", "slotbf"], [("ps", q4)])
                ACT(PTsb[:, q4 * 512:(q4 + 1) * 512], c.psb[q4][:], AF.Copy, [("ps", q4)], ["PTsb"])
            for st in range(2):
                TS("pool", PT[:, st, :], PTsb, c.iota_part[:, st:st + 1], None, ALU.is_equal, None,
                   ["PTsb", "iota_part"], ["PT"])
            for dc in range(8):
                b = 4 + (dc // 2) % 2
                col = (dc % 2) * 256
                for tt in range(NT):
                    MM(c.psb[b][:, col:col + 256], hs[:, tt, dc * 128:(dc + 1) * 128], Pm[:, tt, :], tt == 0, tt == NT - 1,
                       [("hs", tt), "Pm"], [("ps", b)])
                ACT(xsT[:, dc, :], c.psb[b][:, col:col + 256], AF.Copy, [("ps", b), "gains_sb"], ["xsT"],
                    scale=gain_ap(layer, 1, dc))
            for fc in range(8):
                half, fo = fc // 4, (fc % 4) * 128
                b = 6 + fc % 2
                for dc in range(8):
                    MM(c.psb[b][:, 0:256], c.W[0 + half][:, dc, fo:fo + 128], xsT[:, dc, :], dc == 0, dc == 7,
                       ["xsT", ("W", 0 + half)], [("ps", b)])
                for dc in range(8):
                    MM(c.psb[b][:, 256:512], c.W[2 + half][:, dc, fo:fo + 128], xsT[:, dc, :], dc == 0, dc == 7,
                       ["xsT", ("W", 2 + half)], [("ps", b)])
                s2 = fc % 2
                ACT(sg[:, s2, :], c.psb[b][:, 0:256], AF.Silu, [("ps", b)], [("sg", s2)])
                TT("dve", hT[:, fc, :], sg[:, s2, :], c.psb[b][:, 256:512], ALU.mult, [("sg", s2), ("ps", b)], ["hT"])
                if fc == 3 and ex + 1 < NE:
                    load_expert(ex + 1, "g", 0)
                    load_expert(ex + 1, "u", 0)
            if ex + 1 < NE:
                load_expert(ex + 1, "g", 1)
                load_expert(ex + 1, "u", 1)
            n = 0
            for half in range(2):
                for st in range(2):
                    b = 4 + n % 2
                    n += 1
                    for fc in range(8):
                        MM(c.psb[b][:], hT[:, fc, st * 128:(st + 1) * 128], c.W[4 + half][:, fc, :], fc == 0, fc == 7,
                           ["hT", ("W", 4 + half)], [("ps", b)])
                    ACT(Y[:, st, half * 512:(half + 1) * 512], c.psb[b][:], AF.Copy, [("ps", b)], ["Y"])
                if ex + 1 < NE:
                    load_expert(ex + 1, "d", half)
            n = 0
            for tt in range(NT):
                for half in range(2):
                    b = n % 4
                    n += 1
                    for st in range(2):
                        MM(c.psb[b][:], PT[:, st, tt * 128:(tt + 1) * 128], Y[:, st, half * 512:(half + 1) * 512], st == 0, st == 1,
                           ["PT", "Y"], [("ps", b)])
                    hv = c.h[:, tt, half * 512:(half + 1) * 512]
                    STT(hv, c.psb[b][:], c.aff_tok[:, tt, ex:ex + 1], hv, ALU.mult, ALU.add,
                        [("ps", b), "aff", ("h", tt)], [("h", tt)])

    finals = []
    for seq in range(n_seq):
        for q in range(4):
            DMA("sp", c.h[:, q * 4:(q + 1) * 4, :], c.x[seq, q * 512:(q + 1) * 512, :].rearrange("(t p) d -> p t d", p=128),
                [], [("h", q * 4 + j) for j in range(4)], ("hload", q))
        for layer in layers:
            if "mix" in parts:
                if L[layer].kind == 0:
                    mixer_a(layer, seq)
                else:
                    attention(layer, seq)
            if "ffn" in parts:
                ffn(layer, seq)
            if "ple" in parts:
                ple_block(layer, seq)
        P.barrier()
        for q in range(4):
            finals.append(DMA("sp", c.out[seq, q * 512:(q + 1) * 512, :].rearrange("(t p) d -> p t d", p=128),
                              c.h[:, q * 4:(q + 1) * 4, :], [("h", q * 4 + j) for j in range(4)], [], ("hstore", q)))

    n_ops = len(P.ops)
    P.emit(nc, stack, final_waits=finals)
    stack.close()
    return nc, n_ops


def core_inputs(inp, seqs, layers, parts=("mix", "ffn", "ple"), x_override=None, consts=None):
    f = np.float32
    im = dict(consts if consts is not None else host_consts())
    xs = inp["x"] if x_override is None else x_override
    im["x"] = np.ascontiguousarray(xs[seqs])
    im["gains"] = host_gains(inp["norm_mix_g"], inp["norm_ffn_g"], inp["ple_norm_g"])
    for i in layers:
        kind, j = i % 3, i // 3
        if "ple" in parts:
            im["p_L%d" % i] = np.ascontiguousarray(inp["p"][i][seqs])
            im["ple_gate_w_L%d" % i] = inp["ple_gate_w"][i]
            im["ple_proj_w_L%d" % i] = inp["ple_proj_w"][i]
        if "ffn" in parts:
            im["router_w_L%d" % i] = inp["router_w"][i]
            im["exp_w_gate_L%d" % i] = inp["exp_w_gate"][i]
            im["exp_w_up_L%d" % i] = inp["exp_w_up"][i]
            im["exp_w_down_L%d" % i] = inp["exp_w_down"][i]
        if "mix" in parts:
            im["w_out_L%d" % i] = inp["w_out"][i]
            if kind == 0:
                im["w_in_L%d" % i] = inp["a_w_in"][j]
                im["a_vnorm_g_L%d" % i] = np.ascontiguousarray(inp["a_vnorm_g"][j].reshape(1, D))
                im["a_w_sT_L%d" % i] = np.ascontiguousarray(inp["a_w_s"][j].transpose(0, 2, 1))
                im["a_b_s_L%d" % i] = np.ascontiguousarray(inp["a_b_s"][j].reshape(1, D))
            elif kind == 1:
                im["w_in_L%d" % i] = inp["b_w_in"][j]
                im["qk_g_L%d" % i] = np.ascontiguousarray(
                    np.stack([np.tile(inp["b_qnorm_g"][j], 2), np.tile(inp["b_knorm_g"][j], 2)], 1).astype(f))
                im["b_sink_L%d" % i] = np.ascontiguousarray(inp["b_sink"][j].reshape(1, 16))
            else:
                im["w_in_L%d" % i] = inp["c_w_in"][j]
                im["qk_g_L%d" % i] = np.ascontiguousarray(
                    np.stack([np.tile(inp["c_qnorm_g"][j], 2), np.tile(inp["c_knorm_g"][j], 2)], 1).astype(f))
                im["c_rpbt_L%d" % i] = host_rpb_tables(inp["c_rpb"][j])
    return {k: np.ascontiguousarray(v, dtype=f) for k, v in im.items()}


LAUNCH_GROUPS = ((0, 1, 2, 3),)


def kernel(**inputs):
    inp = {k: np.asarray(v) for k, v in inputs.items()}
    consts = host_consts()
    h = np.ascontiguousarray(inp["x"], dtype=np.float32)
    for layers in LAUNCH_GROUPS:
        nc, _ = build_program(SEQ_PER_CORE, layers)
        in_maps = [core_inputs(inp, list(range(cid * SEQ_PER_CORE, (cid + 1) * SEQ_PER_CORE)), layers,
                               x_override=h, consts=consts) for cid in range(N_CORES)]
        res = run_bass_kernel_spmd(nc, in_maps, core_ids=list(range(N_CORES)))
        h = np.concatenate([np.asarray(res.results[cid]["out"]) for cid in range(N_CORES)], axis=0)
    return h.astype(np.float32)
```

```python
import numpy as np
from contextlib import ExitStack
import concourse.bass as bass
import concourse.mybir as mybir
from concourse.bass_utils import run_bass_kernel_spmd

F32 = mybir.dt.float32
BF16 = mybir.dt.bfloat16
AF = mybir.ActivationFunctionType
ALU = mybir.AluOpType
AX = mybir.AxisListType

D = 1024
S = 2048
NT = 16
DEPTH = 4
NE = 16
CAP = 256
PLE = 256
N_CORES = 8
SEQ_PER_CORE = 4
GELU = AF.Gelu_apprx_tanh
BIG = 1.0e9
DBG_ATT = 9


class _Op:
    __slots__ = ("eng", "fn", "deps", "raw", "dsem", "waits", "semid", "semval", "marked")

    def __init__(self, eng, fn, deps, dsem, raw=()):
        self.eng = eng
        self.fn = fn
        self.deps = deps
        self.raw = raw
        self.dsem = dsem
        self.waits = []
        self.semid = None
        self.semval = 0
        self.marked = False


class Prog:
    EPOCH_E = 24000
    EPOCH_D = 1500
    SAME_ENGINE_SYNC = True
    ENGS = ("pe", "act", "dve", "pool")

    def __init__(self):
        self.ops = []
        self.lastw = {}
        self.readers = {}
        self.last_on = {}
        self.pending = {}

    def barrier(self):
        front = set(self.last_on.get(e) for e in self.ENGS if self.last_on.get(e) is not None)
        for e in self.ENGS + ("sp",):
            self.pending[e] = set(front) | self.pending.get(e, set())

    def add(self, eng, fn, r=(), w=(), dsem=None):
        w = list(w) + [k for k in r if isinstance(k, tuple) and k[0] == "ps" and k not in w]
        deps = set()
        for k in r:
            d = self.lastw.get(k)
            if d is not None:
                deps.add(d)
        raw = set(deps)
        for k in w:
            d = self.lastw.get(k)
            if d is not None:
                deps.add(d)
            deps.update(self.readers.get(k, ()))
        pend = self.pending.get(eng)
        if pend:
            deps.update(pend)
            self.pending[eng] = set()
        idx = len(self.ops)
        self.ops.append(_Op(eng, fn, deps, dsem, raw))
        for k in r:
            self.readers.setdefault(k, set()).add(idx)
        for k in w:
            self.lastw[k] = idx
            self.readers[k] = set()
        if dsem is None:
            self.last_on[eng] = idx
        return idx

    def finalize(self):
        ops = self.ops
        for i, op in enumerate(ops):
            best = {}
            for d in op.deps:
                src = ops[d]
                if src.dsem is not None:
                    key = ("d", src.dsem)
                else:
                    if src.eng == op.eng and (op.eng == "pe" or not self.SAME_ENGINE_SYNC):
                        continue
                    key = ("e", src.eng)
                if key not in best or best[key] < d:
                    best[key] = d
            op.waits = sorted(best.values())
            for d in op.waits:
                ops[d].marked = True
        cnt = {}
        semids = set()
        for op in ops:
            if op.dsem is not None:
                k = ("d", op.dsem)
                c = cnt.get(k, 0)
                cnt[k] = c + 1
                op.semid = (k, c // self.EPOCH_D)
                op.semval = (c % self.EPOCH_D + 1) * 16
                op.marked = True
                semids.add(op.semid)
            elif op.marked:
                k = ("e", op.eng)
                c = cnt.get(k, 0)
                cnt[k] = c + 1
                op.semid = (k, c // self.EPOCH_E)
                op.semval = c % self.EPOCH_E + 1
                semids.add(op.semid)
        return sorted(semids, key=str)

    def emit(self, nc, stack, final_waits=()):
        semids = self.finalize()
        sems = {}
        for n, sid in enumerate(semids):
            sems[sid] = stack.enter_context(nc.semaphore("s%d" % n))
        ops = self.ops
        block = stack.enter_context(nc.Block())

        def run(engname, e):
            waited = {}
            for op in ops:
                if op.eng != engname:
                    continue
                for d in op.waits:
                    src = ops[d]
                    if waited.get(src.semid, 0) >= src.semval:
                        continue
                    waited[src.semid] = src.semval
                    e.wait_ge(sems[src.semid], src.semval)
                ins = op.fn(e)
                if op.marked:
                    ins.then_inc(sems[op.semid], 16 if op.dsem is not None else 1)
            if engname == "sp":
                for d in final_waits:
                    src = ops[d]
                    e.wait_ge(sems[src.semid], src.semval)

        @block.tensor
        def _(e):
            run("pe", e)

        @block.scalar
        def _(e):
            run("act", e)

        @block.vector
        def _(e):
            run("dve", e)

        @block.gpsimd
        def _(e):
            run("pool", e)

        @block.sync
        def _(e):
            run("sp", e)


def natten_win(m, kt):
    q = np.arange(128)
    k = np.arange(128)
    r = 2 * m + q // 64
    qc = q % 64
    rs = np.clip(r - 4, 0, 24)
    cs = np.clip(qc - 8, 0, 48)
    kr = 2 * kt + k // 64
    kc = k % 64
    rowok = (kr[:, None] >= rs[None, :]) & (kr[:, None] < rs[None, :] + 8)
    colok = (kc[:, None] >= cs[None, :]) & (kc[:, None] < cs[None, :] + 16)
    return (rowok & colok).astype(np.float32)


def c_key_tiles(m):
    if m <= 1:
        return [0, 1, 2, 3]
    if m >= 14:
        return [12, 13, 14, 15]
    return [m - 2, m - 1, m, m + 1, m + 2]


def c_mask_classes():
    classes = []
    index = {}
    for m in range(NT):
        for kt in c_key_tiles(m):
            mk = natten_win(m, kt)
            key = mk.tobytes()
            found = None
            for ci, (kb, _) in enumerate(classes):
                if kb == key:
                    found = ci
                    break
            if found is None:
                classes.append((key, mk))
                found = len(classes) - 1
            index[(m, kt)] = found
    return np.stack([c[1] for c in classes], 0), index


C_MASKS, C_MASK_INDEX = c_mask_classes()
N_CMASK = C_MASKS.shape[0]


def host_consts():
    k = np.arange(128)[:, None]
    q = np.arange(128)[None, :]
    dist = np.zeros((128, 3, 128), np.float32)
    for i, dm in enumerate((-1, 0, 1)):
        rel = np.abs(dm * 128 + k - q).astype(np.float32)
        dist[:, i, :] = np.where(rel <= 128, rel, BIG)
    sel = np.zeros((16, 16, 128), np.float32)
    for e in range(16):
        sel[e, e, :] = 1.0
    return {
        "ident": np.eye(128, dtype=np.float32),
        "iota_row": np.broadcast_to(np.arange(256, dtype=np.float32)[None, :], (128, 256)).copy(),
        "iota_part": np.stack([np.arange(128), np.arange(128) + 128], 1).astype(np.float32),
        "sel": sel,
        "distB": dist,
        "maskC": np.ascontiguousarray(C_MASKS.transpose(1, 0, 2)),
    }


def host_gains(norm_mix_g, norm_ffn_g, ple_norm_g):
    g = np.stack([norm_mix_g, norm_ffn_g, ple_norm_g], axis=1)
    return np.ascontiguousarray(g.reshape(DEPTH, 3, 8, 128).transpose(0, 1, 3, 2))


def host_rpb_tables(rpb):
    k = np.arange(128)
    q = np.arange(128)
    out = np.zeros((4, 128, 7, 4, 128), np.float32)
    for di, dm in enumerate(range(-3, 4)):
        dr = (2 * dm + k[:, None] // 64) - (q[None, :] // 64)
        dc = (k[:, None] % 64) - (q[None, :] % 64)
        ok = (np.abs(dr) <= 7) & (np.abs(dc) <= 15)
        ri = np.clip(dr + 7, 0, 14)
        ci = np.clip(dc + 15, 0, 30)
        for h in range(16):
            tab = rpb[h][ri, ci]
            tab = np.where(ok, tab, np.float32(0.0))
            hh = h % 4
            pp = (hh % 2) * 2 + hh // 2
            out[h // 4, :, di, pp, :] = tab
    return out.reshape(4, 128, 7 * 4 * 128)


def alibi_slope(h):
    return float(np.float32(2.0 ** (-8.0 * (h + 1) / 16)))


class Ctx:
    pass


def build_program(n_seq=SEQ_PER_CORE, layers=(0, 1, 2, 3), parts=("mix", "ffn", "ple")):
    nc = bass.Bass("TRN2", target_bir_lowering=False)
    P = Prog()
    c = Ctx()
    stack = ExitStack()

    def dram(name, shape, dt=F32, kind="ExternalInput"):
        return nc.dram_tensor(name, list(shape), dt, kind=kind).ap()

    c.x = dram("x", [n_seq, S, D])
    c.out = dram("out", [n_seq, S, D], kind="ExternalOutput")
    c.gains = dram("gains", [DEPTH, 3, 128, 8])
    c.ident = dram("ident", [128, 128])
    c.iota_row_d = dram("iota_row", [128, 256])
    c.iota_part_d = dram("iota_part", [128, 2])
    c.sel_d = dram("sel", [16, 16, 128])
    c.distB_d = dram("distB", [128, 3, 128])
    c.maskC_d = dram("maskC", [128, N_CMASK, 128])
    L = {}
    for i in layers:
        d = Ctx()
        L[i] = d
        kind = i % 3
        d.kind = kind
        if "ple" in parts:
            d.p = dram("p_L%d" % i, [n_seq, S, PLE])
            d.ple_gate_w = dram("ple_gate_w_L%d" % i, [D, D])
            d.ple_proj_w = dram("ple_proj_w_L%d" % i, [PLE, D])
        if "ffn" in parts:
            d.router_w = dram("router_w_L%d" % i, [D, NE])
            d.wg = dram("exp_w_gate_L%d" % i, [NE, D, D])
            d.wu = dram("exp_w_up_L%d" % i, [NE, D, D])
            d.wd = dram("exp_w_down_L%d" % i, [NE, D, D])
        if "mix" in parts:
            d.w_out = dram("w_out_L%d" % i, [D, D])
            if kind == 0:
                d.w_in = dram("w_in_L%d" % i, [D, 2048])
                d.vnorm_g = dram("a_vnorm_g_L%d" % i, [1, D])
                d.w_sT = dram("a_w_sT_L%d" % i, [8, 128, 128])
                d.b_s = dram("a_b_s_L%d" % i, [1, D])
            elif kind == 1:
                d.w_in = dram("w_in_L%d" % i, [D, 1536])
                d.qk_g = dram("qk_g_L%d" % i, [128, 2])
                d.sink = dram("b_sink_L%d" % i, [1, 16])
            else:
                d.w_in = dram("w_in_L%d" % i, [D, 3072])
                d.qk_g = dram("qk_g_L%d" % i, [128, 2])
                d.rpbt = dram("c_rpbt_L%d" % i, [4, 128, 7 * 4 * 128])

    def sb(name, shape, dt):
        return stack.enter_context(nc.sbuf_tensor(name, list(shape), dt))

    def ps(name, shape, dt):
        return stack.enter_context(nc.psum_tensor(name, list(shape), dt))

    c.h = sb("h", [128, NT, D], F32)
    c.R1 = sb("R1", [128, 8 * S], BF16)
    c.R2 = sb("R2", [128, 8 * S], BF16)
    c.W = [sb("W%d" % i, [128, 8, 512], BF16) for i in range(6)]
    c.R3 = sb("R3", [128, 10240], BF16)
    c.gains_sb = sb("gains_sb", [128, DEPTH * 3 * 8], F32)
    c.ident_f = sb("ident_f", [128, 128], F32)
    c.ident_b = sb("ident_b", [128, 128], BF16)
    c.iota_row = sb("iota_row_sb", [128, 256], F32)
    c.iota_part = sb("iota_part_sb", [128, 2], F32)
    c.sel = sb("sel_sb", [16, 16, 128], BF16)
    c.ones_row = sb("ones_row", [1, 128], BF16)
    c.mhalf = sb("mhalf", [128, 32], F32)
    c.stat = sb("stat", [128, 96], F32)
    c.aff_tok = sb("aff_tok", [128, NT, NE], F32)
    c.slot_tok = sb("slot_tok", [128, NT, NE], F32)
    c.rw = sb("rw", [128, 8, NE], F32)
    c.rwg = sb("rwg", [128, 8, NE], F32)
    c.qkg = sb("qkg", [128, 4], F32)
    c.esink = sb("esink", [128, 16], F32)
    c.psb = [ps("psb%d" % i, [128, 512], F32) for i in range(8)]

    def r2(off, n, dt):
        v = c.R2[:, off // 2:(off + n) // 2]
        return v if dt == BF16 else v.bitcast(F32)

    def r3(off, n, dt, parts=128):
        v = c.R3[0:parts, off // 2:(off + n) // 2]
        return v if dt == BF16 else v.bitcast(F32)

    xnT = c.R1[:].rearrange("p (c t) -> p c t", c=8)
    hs = c.R1[:].rearrange("p (t d) -> p t d", t=NT)
    mixT = c.R2[:].rearrange("p (c t) -> p c t", c=8)

    def psbf(b):
        return c.psb[b][:].bitcast(BF16)

    def MM(out, lhsT, rhs, start, stop, r, w, skip=False):
        return P.add("pe", lambda e: e.matmul(out, lhsT=lhsT, rhs=rhs, start=start, stop=stop, skip_group_check=skip),
                     r=r, w=w)

    def TR(out, in_, ident, r, w):
        return P.add("pe", lambda e: e.transpose(out=out, in_=in_, identity=ident), r=r, w=w)

    def ACT(out, in_, func, r, w, **kw):
        return P.add("act", lambda e: e.activation(out=out, in_=in_, func=func, **kw), r=r, w=w)

    def TT(eng, out, in0, in1, op, r, w):
        return P.add(eng, lambda e: e.tensor_tensor(out=out, in0=in0, in1=in1, op=op), r=r, w=w)

    def TS(eng, out, in0, s1, s2, op0, op1, r, w):
        if op1 is None:
            return P.add(eng, lambda e: e.tensor_scalar(out=out, in0=in0, scalar1=s1, scalar2=None, op0=op0), r=r, w=w)
        return P.add(eng, lambda e: e.tensor_scalar(out=out, in0=in0, scalar1=s1, scalar2=s2, op0=op0, op1=op1), r=r, w=w)

    def STT(out, in0, scalar, in1, op0, op1, r, w):
        return P.add("dve", lambda e: e.scalar_tensor_tensor(out=out, in0=in0, scalar=scalar, in1=in1, op0=op0, op1=op1),
                     r=r, w=w)

    def CP(eng, out, in_, r, w):
        return P.add(eng, lambda e: e.tensor_copy(out=out, in_=in_), r=r, w=w)

    def DMA(eng, out, in_, r, w, dsem):
        return P.add(eng, lambda e: e.dma_start(out=out, in_=in_), r=r, w=w, dsem=dsem)

    DMA("sp", c.gains_sb[:].rearrange("p (a c) -> p a c", c=8), c.gains.rearrange("l k p c -> p (l k) c"),
        [], ["gains_sb"], "c_gains")
    DMA("sp", c.ident_f[:], c.ident, [], ["ident_f"], "c_ident")
    DMA("sp", c.iota_row[:], c.iota_row_d, [], ["iota_row"], "c_iotar")
    DMA("sp", c.iota_part[:], c.iota_part_d, [], ["iota_part"], "c_iotap")
    DMA("pool", c.sel[:], c.sel_d, [], ["sel"], "c_sel")
    CP("dve", c.ident_b[:], c.ident_f[:], ["ident_f"], ["ident_b"])
    P.add("pool", lambda e: e.memset(c.ones_row[:], 1.0), w=["ones_row"])
    P.add("pool", lambda e: e.memset(c.mhalf[:], -0.5), w=["mhalf"])

    def gain_ap(layer, which, dc):
        o = (layer * 3 + which) * 8 + dc
        return c.gains_sb[:, o:o + 1]

    def load_w(slot, src2d, kc, n0, n=512, col0=0):
        dst = c.W[slot][:, 0:kc, col0:col0 + n]
        return DMA("pool", dst, src2d[:, n0:n0 + n].rearrange("(c p) n -> p c n", p=128), [], [("W", slot)], ("W", slot))

    def rstd_rms(dst_col0):
        junk = r3(0, 2048, BF16)
        for tt in range(NT):
            ACT(junk, c.h[:, tt, :], AF.Square, [("h", tt)], ["junk", ("ss", tt)], accum_out=c.stat[:, tt:tt + 1])
        TS("pool", c.stat[:, 16:32], c.stat[:, 0:16], 1.0 / D, 1e-6, ALU.mult, ALU.add,
           [("ss", t) for t in range(NT)], ["ms"])
        TT("pool", c.stat[:, dst_col0:dst_col0 + 16], c.stat[:, 16:32], c.mhalf[:, 0:16], ALU.pow, ["ms", "mhalf"], ["rstd"])

    def rms_to_xnT(layer, which):
        P.barrier()
        rstd_rms(32)
        hsb = r3(2048, 4096, BF16).rearrange("p (s d) -> p s d", s=2)
        for g4 in range(NT // 4):
            for j in range(4):
                tt = g4 * 4 + j
                slot = tt % 2
                TS("dve", hsb[:, slot, :], c.h[:, tt, :], c.stat[:, 32 + tt:33 + tt], None, ALU.mult, None,
                   [("h", tt), "rstd"], [("hsb", slot)])
                for dc in range(8):
                    TR(psbf(dc)[:, j * 128:(j + 1) * 128], hsb[:, slot, dc * 128:(dc + 1) * 128], c.ident_b[:],
                       [("hsb", slot), "ident_b"], [("ps", dc)])
            for dc in range(8):
                if dc % 2 == 0:
                    ACT(xnT[:, dc, g4 * 512:(g4 + 1) * 512], psbf(dc)[:, 0:512], AF.Copy,
                        [("ps", dc), "gains_sb"], [("xnT", g4)], scale=gain_ap(layer, which, dc))
                else:
                    TS("dve", xnT[:, dc, g4 * 512:(g4 + 1) * 512], psbf(dc)[:, 0:512], gain_ap(layer, which, dc), None,
                       ALU.mult, None, [("ps", dc), "gains_sb"], [("xnT", g4)])

    def ple_block(layer, seq):
        d = L[layer]
        rms_to_xnT(layer, 2)
        pbuf = r2(0, 16384, F32).rearrange("p (t k) -> p t k", k=PLE)
        pT = r2(16384, 8192, BF16).rearrange("p (k t) -> p k t", k=2)
        hsb = r3(2048, 4096, BF16).rearrange("p (s d) -> p s d", s=2)
        tmpA = r3(6144, 4096, F32).rearrange("p (s d) -> p s d", s=2)
        tmpB = r3(10240, 4096, F32).rearrange("p (s d) -> p s d", s=2)
        load_w(0, d.ple_gate_w, 8, 0)
        load_w(1, d.ple_gate_w, 8, 512)
        load_w(2, d.ple_proj_w, 2, 0)
        load_w(3, d.ple_proj_w, 2, 512)
        DMA("sp", pbuf, d.p[seq].rearrange("(t p) k -> p t k", p=128), [], ["pbuf"], "pbuf")
        for tt in range(NT):
            slot = tt % 2
            CP("dve", hsb[:, slot, 0:PLE], pbuf[:, tt, :], ["pbuf"], [("hsb", slot)])
            bank = tt % 2
            for kc in range(2):
                TR(psbf(bank)[:, kc * 128:(kc + 1) * 128], hsb[:, slot, kc * 128:(kc + 1) * 128], c.ident_b[:],
                   [("hsb", slot), "ident_b"], [("ps", bank)])
            ACT(pT[:, :, tt * 128:(tt + 1) * 128], psbf(bank)[:, 0:256].rearrange("p (k t) -> p k t", k=2), AF.Copy,
                [("ps", bank)], ["pT"])
        n = 0
        for tt in range(NT):
            for half in range(2):
                bG = 2 + (n % 3) * 2
                bP = bG + 1
                s2 = n % 2
                n += 1
                for kc in range(8):
                    MM(c.psb[bG][:], xnT[:, kc, tt * 128:(tt + 1) * 128], c.W[half][:, kc, :], kc == 0, kc == 7,
                       [("xnT", tt // 4), ("W", half)], [("ps", bG)])
                for kc in range(2):
                    MM(c.psb[bP][:], pT[:, kc, tt * 128:(tt + 1) * 128], c.W[2 + half][:, kc, :], kc == 0, kc == 1,
                       ["pT", ("W", 2 + half)], [("ps", bP)])
                ACT(tmpA[:, s2, :], c.psb[bG][:], AF.Sigmoid, [("ps", bG)], [("tmpA", s2)])
                TT("dve", tmpB[:, s2, :], tmpA[:, s2, :], c.psb[bP][:], ALU.mult, [("tmpA", s2), ("ps", bP)], [("tmpB", s2)])
                hv = c.h[:, tt, half * 512:(half + 1) * 512]
                TT("dve", hv, hv, tmpB[:, s2, :], ALU.add, [("tmpB", s2), ("h", tt)], [("h", tt)])

    def wout_full(layer):
        n = 0
        for tt in range(NT):
            for half in range(2):
                b = n % 4
                n += 1
                for kc in range(8):
                    MM(c.psb[b][:], mixT[:, kc, tt * 128:(tt + 1) * 128], c.W[4 + half][:, kc, :], kc == 0, kc == 7,
                       [("mixT", tt // 4), ("W", 4 + half)], [("ps", b)])
                hv = c.h[:, tt, half * 512:(half + 1) * 512]
                TT("dve", hv, hv, c.psb[b][:], ALU.add, [("ps", b), ("h", tt)], [("h", tt)])

    def mixer_a(layer, seq):
        d = L[layer]
        rms_to_xnT(layer, 0)
        load_w(2, d.w_in, 8, 1024)
        load_w(3, d.w_in, 8, 1536)
        load_w(0, d.w_in, 8, 0)
        load_w(1, d.w_in, 8, 512)
        load_w(4, d.w_out, 8, 0)
        load_w(5, d.w_out, 8, 512)
        vg = r3(6144, 4096, F32)
        wsT = r3(10240, 2048, BF16).rearrange("p (g t) -> p g t", g=8)
        bs = r3(12288, 2048, BF16, parts=1)
        vt = r3(14336, 4096, F32)
        vn = r3(18432, 2048, BF16)
        DMA("sp", vg, d.vnorm_g[0:1, :].to_broadcast([128, D]), [], ["vg"], "a_vg")
        DMA("pool", wsT, d.w_sT.rearrange("g s t -> s g t"), [], ["wsT"], "a_wsT")
        DMA("pool", bs, d.b_s, [], ["bs"], "a_bs")
        st6 = c.stat[:, 48:60].rearrange("p (a b) -> p a b", a=2)
        mv = c.stat[:, 60:62]
        for tt in range(NT):
            for half in range(2):
                b = half
                for kc in range(8):
                    MM(c.psb[b][:], xnT[:, kc, tt * 128:(tt + 1) * 128], c.W[2 + half][:, kc, :], kc == 0, kc == 7,
                       [("xnT", tt // 4), ("W", 2 + half)], [("ps", b)])
                ACT(vt[:, half * 512:(half + 1) * 512], c.psb[b][:], GELU, [("ps", b)], ["vt"])
                P.add("dve", lambda e, half=half: e.bn_stats(out=st6[:, half, :], in_=vt[:, half * 512:(half + 1) * 512]),
                      r=["vt"], w=["st6"])
            P.add("dve", lambda e: e.bn_aggr(out=mv, in_=c.stat[:, 48:60]), r=["st6"], w=["mv"])
            TS("pool", c.stat[:, 62:63], c.stat[:, 61:62], 1e-5, None, ALU.add, None, ["mv"], ["lnv"])
            TT("pool", c.stat[:, 63:64], c.stat[:, 62:63], c.mhalf[:, 0:1], ALU.pow, ["lnv", "mhalf"], ["lnr"])
            TS("dve", vt, vt, c.stat[:, 60:61], c.stat[:, 63:64], ALU.subtract, ALU.mult, ["vt", "mv", "lnr"], ["vt"])
            TT("dve", vn, vt, vg, ALU.mult, ["vt", "vg"], ["vn"])
            for g in range(8):
                b = 2 + g // 4
                col = (g % 4) * 128
                MM(c.psb[b][:, col:col + 128], vn[:, g * 128:(g + 1) * 128], wsT[:, g, :], True, False,
                   ["vn", "wsT"], [("ps", b)])
                MM(c.psb[b][:, col:col + 128], c.ones_row[0:1, :], bs[0:1, g * 128:(g + 1) * 128], False, True,
                   ["ones_row", "bs"], [("ps", b)])
            for b2 in range(2):
                outv = mixT[:, b2 * 4:(b2 + 1) * 4, tt * 128:(tt + 1) * 128]
                inv = c.psb[2 + b2][:].rearrange("p (g t) -> p g t", g=4)
                if b2 == 0:
                    ACT(outv, inv, AF.Copy, [("ps", 2 + b2)], [("mixT", tt // 4)])
                else:
                    CP("dve", outv, inv, [("ps", 2 + b2)], [("mixT", tt // 4)])
        P.barrier()
        gu = r3(14336, 4096, F32).rearrange("p (s d) -> p s d", s=2)
        n = 0
        for fc in range(8):
            for tq in range(4):
                b = n % 4
                s2 = n % 2
                n += 1
                for kc in range(8):
                    MM(c.psb[b][:], c.W[fc // 4][:, kc, (fc % 4) * 128:(fc % 4 + 1) * 128], xnT[:, kc, tq * 512:(tq + 1) * 512],
                       kc == 0, kc == 7, [("xnT", tq), ("W", fc // 4)], [("ps", b)])
                ACT(gu[:, s2, :], c.psb[b][:], GELU, [("ps", b)], [("gu", s2)])
                mv_ = mixT[:, fc, tq * 512:(tq + 1) * 512]
                TT("dve", mv_, mv_, gu[:, s2, :], ALU.mult, [("gu", s2), ("mixT", tq)], [("mixT", tq)])
        wout_full(layer)

    def attention(layer, seq):
        d = L[layer]
        isB = (d.kind == 1)
        rms_to_xnT(layer, 0)
        P.barrier()
        qT = r2(0, 8192, BF16).rearrange("p (c t) -> p c t", c=2)
        kT = r2(8192, 8192, BF16).rearrange("p (c t) -> p c t", c=2)
        V = r2(16384, 8448, BF16).rearrange("p (t h e) -> p t h e", t=NT, h=4)
        sq = r3(0, 2048, F32)
        qtok = r3(2048, 1024, BF16).rearrange("p (s d) -> p s d", s=2)
        ktok = r3(3072, 1024, BF16).rearrange("p (s d) -> p s d", s=2)
        tS = r3(4096, 4096, F32).rearrange("p (s h q) -> p s h q", s=2, h=4)
        Eb = r3(8192, 2048, BF16).rearrange("p (s h q) -> p s h q", s=2, h=4)
        PTb = r3(10240, 2048, BF16).rearrange("p (s h q) -> p s h q", s=2, h=4)
        otok = r3(12288, 1024, BF16).rearrange("p (s d) -> p s d", s=2)
        oT = r3(13312, 1024, BF16).rearrange("p (s k t) -> p s k t", s=2, k=2)
        b4 = b5 = distB = maskC = None
        if isB:
            distB = r3(14336, 1536, F32).rearrange("p (a q) -> p a q", a=3)
            DMA("sp", distB, c.distB_d, [], ["tab"], "tabB")
            DMA("sp", c.esink[:], d.sink[0:1, :].to_broadcast([128, 16]), [], ["esink"], "sink")
            ACT(c.esink[:], c.esink[:], AF.Exp, ["esink"], ["esink"])
        else:
            maskC = r3(14336, N_CMASK * 256, BF16).rearrange("p (a q) -> p a q", a=N_CMASK)
            DMA("pool", maskC, c.maskC_d, [], ["tab"], "tabC")
            b4 = c.W[4][:].rearrange("p a b -> p (a b)").bitcast(F32).rearrange("p (m h q) -> p m h q", m=4, h=4)
            b5 = c.W[5][:].rearrange("p a b -> p (a b)").bitcast(F32).rearrange("p (m h q) -> p m h q", m=4, h=4)
        DMA("sp", c.qkg[:, 0:2], d.qk_g, [], ["qkg"], "qkg")
        TS("pool", c.qkg[:, 2:3], c.qkg[:, 0:1], 0.125, None, ALU.mult, None, ["qkg"], ["qkg2"])
        P.add("pool", lambda e: e.memset(V[:, :, :, 64:65], 1.0), w=["V"])
        load_w(2, d.w_out, 8, 0)
        load_w(3, d.w_out, 8, 512)
        for gi in range(4):
            if isB:
                load_w(0, d.w_in, 8, gi * 256, n=256)
                load_w(1, d.w_in, 8, 1024 + gi * 64, n=64, col0=0)
                load_w(1, d.w_in, 8, 1280 + gi * 64, n=64, col0=64)
            else:
                load_w(0, d.w_in, 8, gi * 256, n=256, col0=0)
                load_w(0, d.w_in, 8, 1024 + gi * 256, n=256, col0=256)
                load_w(1, d.w_in, 8, 2048 + gi * 256, n=256, col0=0)
                src = d.rpbt[gi].rearrange("p (m h q) -> p m h q", m=7, h=4)
                DMA("sp", b4, src[:, 0:4], [], [("W", 4)], ("W", 4))
                DMA("sp", b5[:, 0:3], src[:, 4:7], [], [("W", 5)], ("W", 5))
            nk = 64 if isB else 256
            nh = 4 + nk // 64
            nkp = 1 if isB else 2

            def qkv_mm(tt):
                s2 = tt % 2
                bq, bkv = s2, 2 + s2
                for kc in range(8):
                    MM(c.psb[bq][:, 0:256], xnT[:, kc, tt * 128:(tt + 1) * 128], c.W[0][:, kc, 0:256], kc == 0, kc == 7,
                       [("xnT", tt // 4), ("W", 0)], [("ps", bq)])
                if isB:
                    for kc in range(8):
                        MM(c.psb[bkv][:, 0:128], xnT[:, kc, tt * 128:(tt + 1) * 128], c.W[1][:, kc, 0:128], kc == 0, kc == 7,
                           [("xnT", tt // 4), ("W", 1)], [("ps", bkv)])
                else:
                    for kc in range(8):
                        MM(c.psb[bkv][:, 0:256], xnT[:, kc, tt * 128:(tt + 1) * 128], c.W[0][:, kc, 256:512], kc == 0, kc == 7,
                           [("xnT", tt // 4), ("W", 0)], [("ps", bkv)])
                    for kc in range(8):
                        MM(c.psb[bkv][:, 256:512], xnT[:, kc, tt * 128:(tt + 1) * 128], c.W[1][:, kc, 0:256], kc == 0, kc == 7,
                           [("xnT", tt // 4), ("W", 1)], [("ps", bkv)])

            def qkv_post(tt):
                s2 = tt % 2
                bq, bkv = s2, 2 + s2
                if isB:
                    kps = c.psb[bkv][:, 0:64]
                    vps = c.psb[bkv][:, 64:128]
                else:
                    kps = c.psb[bkv][:, 0:256]
                    vps = c.psb[bkv][:, 256:512]
                sqv = sq[:, s2 * 0:512] if False else sq
                ACT(sq[:, 0:256], c.psb[bq][:, 0:256], AF.Square, [("ps", bq)], ["sq"])
                ACT(sq[:, 256:256 + nk], kps, AF.Square, [("ps", bkv)], ["sq"])
                ssq = c.stat[:, 64:64 + nh]
                P.add("dve", lambda e: e.tensor_reduce(
                    out=ssq, in_=sq[:, 0:nh * 64].rearrange("p (h e) -> p h e", e=64), axis=AX.X, op=ALU.add),
                    r=["sq"], w=["ssq"])
                TS("pool", c.stat[:, 72:72 + nh], ssq, 1.0 / 64, 1e-6, ALU.mult, ALU.add, ["ssq"], ["qms"])
                TT("pool", c.stat[:, 80:80 + nh], c.stat[:, 72:72 + nh], c.mhalf[:, 0:nh], ALU.pow, ["qms", "mhalf"], ["qrs"])
                TT("dve", qtok[:, s2, :].rearrange("p (h e) -> p h e", e=64),
                   c.psb[bq][:, 0:256].rearrange("p (h e) -> p h e", e=64),
                   c.stat[:, 80:84].unsqueeze(2).to_broadcast([128, 4, 64]), ALU.mult,
                   [("ps", bq), "qrs"], [("qtok", s2)])
                if isB:
                    for dup in range(2):
                        TS("dve", ktok[:, s2, dup * 64:(dup + 1) * 64], kps, c.stat[:, 84:85], None, ALU.mult, None,
                           [("ps", bkv), "qrs"], [("ktok", s2)])
                    ACT(V[:, tt, 0, 0:64], vps, AF.Copy, [("ps", bkv)], ["V"])
                else:
                    TT("dve", ktok[:, s2, :].rearrange("p (h e) -> p h e", e=64),
                       kps.rearrange("p (h e) -> p h e", e=64),
                       c.stat[:, 84:88].unsqueeze(2).to_broadcast([128, 4, 64]), ALU.mult,
                       [("ps", bkv), "qrs"], [("ktok", s2)])
                    ACT(V[:, tt, :, 0:64], vps.rearrange("p (h e) -> p h e", e=64), AF.Copy, [("ps", bkv)], ["V"])

            def qkv_tr(tt):
                s2 = tt % 2
                bt = 4 + s2
                for pr in range(2):
                    TR(psbf(bt)[:, pr * 128:(pr + 1) * 128], qtok[:, s2, pr * 128:(pr + 1) * 128], c.ident_b[:],
                       [("qtok", s2), "ident_b"], [("ps", bt)])
                for pr in range(nkp):
                    TR(psbf(bt)[:, 256 + pr * 128:256 + (pr + 1) * 128], ktok[:, s2, pr * 128:(pr + 1) * 128], c.ident_b[:],
                       [("ktok", s2), "ident_b"], [("ps", bt)])
                ACT(qT[:, :, tt * 128:(tt + 1) * 128], psbf(bt)[:, 0:256].rearrange("p (c t) -> p c t", c=2), AF.Copy,
                    [("ps", bt), "qkg2"], ["qT"], scale=c.qkg[:, 2:3])
                TS("dve", kT[:, 0:nkp, tt * 128:(tt + 1) * 128],
                   psbf(bt)[:, 256:256 + nkp * 128].rearrange("p (c t) -> p c t", c=nkp), c.qkg[:, 1:2], None, ALU.mult, None,
                   [("ps", bt), "qkg"], ["kT"])

            if DBG_ATT >= 1:
                qkv_mm(0)
                for tt in range(NT):
                    if tt + 1 < NT:
                        qkv_mm(tt + 1)
                    qkv_post(tt)
                    qkv_tr(tt)

            steps = []
            for m in range(NT if DBG_ATT >= 2 else 0):
                kts = [kt for kt in (m - 1, m, m + 1) if 0 <= kt < NT] if isB else c_key_tiles(m)
                for ki, kt in enumerate(kts):
                    steps.append((m, ki, kt, len(kts)))

            def att_scores(si):
                m, ki, kt, nkt = steps[si]
                s2 = si % 2
                for pp in range(4):
                    hf, j = pp // 2, pp % 2
                    bsc = s2 * 2 + hf
                    kpr = 0 if isB else j
                    MM(c.psb[bsc][:, j * 128:(j + 1) * 128],
                       kT[hf * 64:(hf + 1) * 64, kpr, kt * 128:(kt + 1) * 128],
                       qT[hf * 64:(hf + 1) * 64, j, m * 128:(m + 1) * 128], True, True,
                       ["qT", "kT"], [("ps", bsc)])

            def att_prob(si):
                m, ki, kt, nkt = steps[si]
                s2 = si % 2
                if isB:
                    dmi = kt - m + 1
                    for pp in range(4):
                        hf, j = pp // 2, pp % 2
                        bsc = s2 * 2 + hf
                        STT(tS[:, s2, pp, :], distB[:, dmi, :], -alibi_slope(gi * 4 + 2 * j + hf),
                            c.psb[bsc][:, j * 128:(j + 1) * 128], ALU.mult, ALU.add,
                            [("ps", bsc), "tab"], [("tS", s2, pp)])
                    ACT(PTb[:, s2], tS[:, s2], AF.Exp, [("tS", s2, pp) for pp in range(4)], [("PT", s2)])
                else:
                    dmi = kt - m + 3
                    btab = (b4[:, dmi] if dmi < 4 else b5[:, dmi - 4])
                    for hf in range(2):
                        bsc = s2 * 2 + hf
                        TT("dve", tS[:, s2, 2 * hf:2 * hf + 2, :],
                           c.psb[bsc][:, 0:256].rearrange("p (h q) -> p h q", h=2), btab[:, 2 * hf:2 * hf + 2, :], ALU.add,
                           [("ps", bsc), ("W", 4), ("W", 5)], [("tS", s2, hf)])
                    ACT(Eb[:, s2], tS[:, s2], AF.Exp, [("tS", s2, 0), ("tS", s2, 1)], [("E", s2)])
                    ci = C_MASK_INDEX[(m, kt)]
                    TT("dve", PTb[:, s2], Eb[:, s2], maskC[:, ci:ci + 1, :].to_broadcast([128, 4, 128]), ALU.mult,
                       [("E", s2), "tab"], [("PT", s2)])

            def att_pv(si):
                m, ki, kt, nkt = steps[si]
                s2 = si % 2
                bo = 4 + m % 2
                ops_ = c.psb[bo][:, 0:260].rearrange("p (h e) -> p h e", e=65)
                for pp in range(4 if DBG_ATT >= 3 else 0):
                    hh = 2 * (pp % 2) + pp // 2
                    vh = 0 if isB else hh
                    MM(ops_[:, hh, :], PTb[:, s2, pp, :], V[:, kt, vh, 0:65], ki == 0 and pp == 0, ki == nkt - 1,
                       [("PT", s2), "V"], [("ps", bo)], skip=True)

            def att_finish(m):
                bo = 4 + m % 2
                ops_ = c.psb[bo][:, 0:260].rearrange("p (h e) -> p h e", e=65)
                o2 = m % 2
                den = c.stat[:, 88:92]
                if isB:
                    TT("dve", den, ops_[:, :, 64], c.esink[:, gi * 4:(gi + 1) * 4], ALU.add, [("ps", bo), "esink"], ["den"])
                else:
                    CP("dve", den, ops_[:, :, 64], [("ps", bo)], ["den"])
                P.add("dve", lambda e: e.reciprocal(out=c.stat[:, 92:96], in_=den), r=["den"], w=["rden"])
                TT("dve", otok[:, o2, :].rearrange("p (h e) -> p h e", e=64), ops_[:, :, 0:64],
                   c.stat[:, 92:96].unsqueeze(2).to_broadcast([128, 4, 64]), ALU.mult,
                   [("ps", bo), "rden"], [("otok", o2)])
                bt = 6
                for pr in range(2):
                    TR(psbf(bt)[:, pr * 128:(pr + 1) * 128], otok[:, o2, pr * 128:(pr + 1) * 128], c.ident_b[:],
                       [("otok", o2), "ident_b"], [("ps", bt)])
                ACT(oT[:, o2], psbf(bt)[:, 0:256].rearrange("p (k t) -> p k t", k=2), AF.Copy, [("ps", bt)], [("oT", o2)])
                for half in range(2):
                    bw = 7
                    for k2 in range(2):
                        MM(c.psb[bw][:], oT[:, o2, k2, :], c.W[2 + half][:, gi * 2 + k2, :], k2 == 0, k2 == 1,
                           [("oT", o2), ("W", 2 + half)], [("ps", bw)])
                    hv = c.h[:, m, half * 512:(half + 1) * 512]
                    TT("dve", hv, hv, c.psb[bw][:], ALU.add, [("ps", bw), ("h", m)], [("h", m)])

            if steps:
                att_scores(0)
            pend_finish = None
            for si in range(len(steps)):
                if si + 1 < len(steps):
                    att_scores(si + 1)
                att_prob(si)
                if pend_finish is not None and DBG_ATT >= 4:
                    att_finish(pend_finish)
                pend_finish = None
                att_pv(si)
                m, ki, kt, nkt = steps[si]
                if ki == nkt - 1:
                    pend_finish = m
            if pend_finish is not None and DBG_ATT >= 4:
                att_finish(pend_finish)

    def ffn(layer, seq):
        d = L[layer]
        P.barrier()
        DMA("sp", c.rw[:], d.router_w.rearrange("(c p) e -> p c e", p=128), [], ["rw"], "rw")
        o = (layer * 3 + 1) * 8
        TT("dve", c.rwg[:], c.rw[:], c.gains_sb[:, o:o + 8].unsqueeze(2).to_broadcast([128, 8, NE]), ALU.mult,
           ["rw", "gains_sb"], ["rwg"])
        wslot = {"g": (0, 1), "u": (2, 3), "d": (4, 5)}

        def load_expert(e, which, half):
            src = {"g": d.wg, "u": d.wu, "d": d.wd}[which][e]
            load_w(wslot[which][half], src, 8, half * 512)

        for which in ("g", "u", "d"):
            for half in range(2):
                load_expert(0, which, half)
        rstd_rms(32)
        hsf = r3(2048, 8192, F32).rearrange("p (s d) -> p s d", s=2)
        hsfT = r3(10240, 8192, F32).rearrange("p (s c t) -> p s c t", s=2, c=8)
        bL = 7
        for tt in range(NT):
            s2 = tt % 2
            TS("dve", hs[:, tt, :], c.h[:, tt, :], c.stat[:, 32 + tt:33 + tt], None, ALU.mult, None,
               [("h", tt), "rstd"], [("hs", tt)])
            ACT(hsf[:, s2, :], c.h[:, tt, :], AF.Copy, [("h", tt), "rstd"], [("hsf", s2)], scale=c.stat[:, 32 + tt:33 + tt])
            for dc in range(8):
                b = s2 * 2 + dc // 4
                TR(c.psb[b][:, (dc % 4) * 128:(dc % 4 + 1) * 128], hsf[:, s2, dc * 128:(dc + 1) * 128], c.ident_f[:],
                   [("hsf", s2), "ident_f"], [("ps", b)])
            for b2 in range(2):
                b = s2 * 2 + b2
                outv = hsfT[:, s2, b2 * 4:(b2 + 1) * 4, :]
                inv = c.psb[b][:].rearrange("p (c t) -> p c t", c=4)
                if b2 == 0:
                    ACT(outv, inv, AF.Copy, [("ps", b)], [("hsfT", s2)])
                else:
                    CP("dve", outv, inv, [("ps", b)], [("hsfT", s2)])
            for dc in range(8):
                MM(c.psb[bL][:, tt * 16:(tt + 1) * 16], hsfT[:, s2, dc, :], c.rwg[:, dc, :], dc == 0, dc == 7,
                   [("hsfT", s2), "rwg"], [("ps", bL)])
        lg = c.psb[bL][:, 0:256].rearrange("p (t e) -> p t e", e=NE)
        mx = c.stat[:, 0:16]
        P.add("dve", lambda e: e.tensor_reduce(out=mx, in_=lg, axis=AX.X, op=ALU.max), r=[("ps", bL)], w=["mx"])
        TT("dve", c.aff_tok[:], lg, mx.unsqueeze(2).to_broadcast([128, NT, NE]), ALU.subtract, [("ps", bL), "mx"], ["aff"])
        ACT(c.aff_tok[:], c.aff_tok[:], AF.Exp, ["aff"], ["aff"])
        sm = c.stat[:, 16:32]
        P.add("dve", lambda e: e.tensor_reduce(out=sm, in_=c.aff_tok[:], axis=AX.X, op=ALU.add), r=["aff"], w=["sm"])
        P.add("dve", lambda e: e.reciprocal(out=sm, in_=sm), r=["sm"], w=["sm"])
        TT("dve", c.aff_tok[:], c.aff_tok[:], sm.unsqueeze(2).to_broadcast([128, NT, NE]), ALU.mult, ["aff", "sm"], ["aff"])
        P.barrier()
        affT = r2(0, 8192, F32)[0:16, :]
        work = r2(8192, 8192, F32)[0:16, :]
        cum = r2(16384, 8192, F32)[0:16, :]
        slotbf = r3(0, 4096, BF16)[0:16, :]
        for q4 in range(4):
            for j in range(4):
                tt = q4 * 4 + j
                TR(c.psb[q4][0:16, j * 128:(j + 1) * 128], c.aff_tok[:, tt, :], c.ident_f[:], ["aff", "ident_f"], [("ps", q4)])
            ACT(affT[:, q4 * 512:(q4 + 1) * 512], c.psb[q4][0:16, :], AF.Copy, [("ps", q4)], ["affT"])
        CP("dve", work, affT, ["affT"], ["work"])
        m8 = c.stat[0:16, 48:56]
        for it in range(CAP // 8):
            P.add("dve", lambda e: e.max(out=m8, in_=work), r=["work"], w=["m8"])
            if it < CAP // 8 - 1:
                P.add("dve", lambda e: e.match_replace(out=work, in_to_replace=m8, in_values=work, imm_value=-1.0),
                      r=["work", "m8"], w=["work"])
        TS("dve", work, affT, c.stat[0:16, 55:56], None, ALU.is_ge, None, ["affT", "m8"], ["work"])
        P.add("dve", lambda e: e.tensor_tensor_scan(out=cum, data0=work, data1=work, initial=0.0, op0=ALU.add, op1=ALU.max),
              r=["work"], w=["cum"])
        STT(cum, cum, -513.0, work, ALU.add, ALU.mult, ["cum", "work"], ["cum"])
        TS("dve", cum, cum, 512.0, None, ALU.add, None, ["cum"], ["cum"])
        CP("dve", slotbf, cum, ["cum"], ["slotbf"])
        bS = 4
        for tt in range(NT):
            TR(c.psb[bS][:, tt * 16:(tt + 1) * 16], cum[:, tt * 128:(tt + 1) * 128], c.ident_f[0:16, 0:16],
               ["cum", "ident_f"], [("ps", bS)])
        CP("dve", c.slot_tok[:].rearrange("p t e -> p (t e)"), c.psb[bS][:, 0:256], [("ps", bS)], ["slot_tok"])
        P.barrier()
        Pm = r2(0, 8192, BF16).rearrange("p (t s) -> p t s", t=NT)
        PT = r2(8192, 8192, BF16).rearrange("p (s t) -> p s t", s=2)
        xsT = r2(16384, 4096, BF16).rearrange("p (c s) -> p c s", c=8)
        hT = r2(20480, 4096, BF16).rearrange("p (c s) -> p c s", c=8)
        Y = r2(24576, 4096, BF16).rearrange("p (s d) -> p s d", s=2)
        sg = r3(4096, 2048, F32).rearrange("p (s d) -> p s d", s=2)
        Pm2 = r3(6144, 8192, BF16).rearrange("p (t s) -> p t s", t=NT)
        Pms = (Pm, Pm2)
        def build_pm(ex):
            TT("dve", Pms[ex % 2], c.iota_row[:].unsqueeze(1).to_broadcast([128, NT, CAP]),
               c.slot_tok[:, :, ex:ex + 1].to_broadcast([128, NT, CAP]), ALU.is_equal,
               ["iota_row", "slot_tok"], [("Pm", ex % 2)])

        build_pm(0)
        for ex in range(NE):
            Pm = Pms[ex % 2]
            pmk = ("Pm", ex % 2)
            for dc in range(8):
                b = 4 + (dc // 2) % 2
                col = (dc % 2) * 256
                for tt in range(NT):
                    MM(c.psb[b][:, col:col + 256], hs[:, tt, dc * 128:(dc + 1) * 128], Pm[:, tt, :], tt == 0, tt == NT - 1,
                       [("hs", tt), pmk], [("ps", b)])
                ACT(xsT[:, dc, :], c.psb[b][:, col:col + 256], AF.Copy, [("ps", b), "gains_sb"], [("xsT", dc)],
                    scale=gain_ap(layer, 1, dc))
            for fc in range(8):
                half, fo = fc // 4, (fc % 4) * 128
                b = 6 + fc % 2
                for dc in range(8):
                    MM(c.psb[b][:, 0:256], c.W[0 + half][:, dc, fo:fo + 128], xsT[:, dc, :], dc == 0, dc == 7,
                       [("xsT", dc), ("W", 0 + half)], [("ps", b)])
                for dc in range(8):
                    MM(c.psb[b][:, 256:512], c.W[2 + half][:, dc, fo:fo + 128], xsT[:, dc, :], dc == 0, dc == 7,
                       [("xsT", dc), ("W", 2 + half)], [("ps", b)])
                s2 = fc % 2
                ACT(sg[:, s2, :], c.psb[b][:, 0:256], AF.Silu, [("ps", b)], [("sg", s2)])
                TT("dve", hT[:, fc, :], sg[:, s2, :], c.psb[b][:, 256:512], ALU.mult, [("sg", s2), ("ps", b)], [("hT", fc)])
                if fc == 3 and ex + 1 < NE:
                    load_expert(ex + 1, "g", 0)
                    load_expert(ex + 1, "u", 0)
            if ex + 1 < NE:
                load_expert(ex + 1, "g", 1)
                load_expert(ex + 1, "u", 1)
                build_pm(ex + 1)
            for q4 in range(4):
                MM(c.psb[q4][:], c.sel[:, ex, :], slotbf[:, q4 * 512:(q4 + 1) * 512], True, True,
                   ["sel", "slotbf"], [("ps", q4)])
                for st in range(2):
                    TS("dve", PT[:, st, q4 * 512:(q4 + 1) * 512], c.psb[q4][:], c.iota_part[:, st:st + 1], None,
                       ALU.is_equal, None, [("ps", q4), "iota_part"], [("PT", st, q4)])
            n = 0
            for half in range(2):
                for st in range(2):
                    b = 4 + n % 2
                    n += 1
                    for fc in range(8):
                        MM(c.psb[b][:], hT[:, fc, st * 128:(st + 1) * 128], c.W[4 + half][:, fc, :], fc == 0, fc == 7,
                           [("hT", fc), ("W", 4 + half)], [("ps", b)])
                    ACT(Y[:, st, half * 512:(half + 1) * 512], c.psb[b][:], AF.Copy, [("ps", b)], [("Y", st, half)])
                if ex + 1 < NE:
                    load_expert(ex + 1, "d", half)
            n = 0
            for tt in range(NT):
                for half in range(2):
                    b = n % 4
                    n += 1
                    for st in range(2):
                        MM(c.psb[b][:], PT[:, st, tt * 128:(tt + 1) * 128], Y[:, st, half * 512:(half + 1) * 512], st == 0, st == 1,
                           [("PT", st, tt // 4), ("Y", st, half)], [("ps", b)])
                    hv = c.h[:, tt, half * 512:(half + 1) * 512]
                    STT(hv, c.psb[b][:], c.aff_tok[:, tt, ex:ex + 1], hv, ALU.mult, ALU.add,
                        [("ps", b), "aff", ("h", tt)], [("h", tt)])

    finals = []
    for seq in range(n_seq):
        for q in range(4):
            DMA("sp", c.h[:, q * 4:(q + 1) * 4, :], c.x[seq, q * 512:(q + 1) * 512, :].rearrange("(t p) d -> p t d", p=128),
                [], [("h", q * 4 + j) for j in range(4)], ("hload", q))
        for layer in layers:
            if "mix" in parts:
                if L[layer].kind == 0:
                    mixer_a(layer, seq)
                else:
                    attention(layer, seq)
            if "ffn" in parts:
                ffn(layer, seq)
            if "ple" in parts:
                ple_block(layer, seq)
        P.barrier()
        for q in range(4):
            finals.append(DMA("sp", c.out[seq, q * 512:(q + 1) * 512, :].rearrange("(t p) d -> p t d", p=128),
                              c.h[:, q * 4:(q + 1) * 4, :], [("h", q * 4 + j) for j in range(4)], [], ("hstore", q)))

    n_ops = len(P.ops)
    P.emit(nc, stack, final_waits=finals)
    stack.close()
    return nc, n_ops


def core_inputs(inp, seqs, layers, parts=("mix", "ffn", "ple"), x_override=None, consts=None):
    f = np.float32
    im = dict(consts if consts is not None else host_consts())
    xs = inp["x"] if x_override is None else x_override
    im["x"] = np.ascontiguousarray(xs[seqs])
    im["gains"] = host_gains(inp["norm_mix_g"], inp["norm_ffn_g"], inp["ple_norm_g"])
    for i in layers:
        kind, j = i % 3, i // 3
        if "ple" in parts:
            im["p_L%d" % i] = np.ascontiguousarray(inp["p"][i][seqs])
            im["ple_gate_w_L%d" % i] = inp["ple_gate_w"][i]
            im["ple_proj_w_L%d" % i] = inp["ple_proj_w"][i]
        if "ffn" in parts:
            im["router_w_L%d" % i] = inp["router_w"][i]
            im["exp_w_gate_L%d" % i] = inp["exp_w_gate"][i]
            im["exp_w_up_L%d" % i] = inp["exp_w_up"][i]
            im["exp_w_down_L%d" % i] = inp["exp_w_down"][i]
        if "mix" in parts:
            im["w_out_L%d" % i] = inp["w_out"][i]
            if kind == 0:
                im["w_in_L%d" % i] = inp["a_w_in"][j]
                im["a_vnorm_g_L%d" % i] = np.ascontiguousarray(inp["a_vnorm_g"][j].reshape(1, D))
                im["a_w_sT_L%d" % i] = np.ascontiguousarray(inp["a_w_s"][j].transpose(0, 2, 1))
                im["a_b_s_L%d" % i] = np.ascontiguousarray(inp["a_b_s"][j].reshape(1, D))
            elif kind == 1:
                im["w_in_L%d" % i] = inp["b_w_in"][j]
                im["qk_g_L%d" % i] = np.ascontiguousarray(
                    np.stack([np.tile(inp["b_qnorm_g"][j], 2), np.tile(inp["b_knorm_g"][j], 2)], 1).astype(f))
                im["b_sink_L%d" % i] = np.ascontiguousarray(inp["b_sink"][j].reshape(1, 16))
            else:
                im["w_in_L%d" % i] = inp["c_w_in"][j]
                im["qk_g_L%d" % i] = np.ascontiguousarray(
                    np.stack([np.tile(inp["c_qnorm_g"][j], 2), np.tile(inp["c_knorm_g"][j], 2)], 1).astype(f))
                im["c_rpbt_L%d" % i] = host_rpb_tables(inp["c_rpb"][j])
    return {k: np.ascontiguousarray(v, dtype=f) for k, v in im.items()}


LAUNCH_GROUPS = ((0, 1, 2, 3),)


def kernel(**inputs):
    inp = {k: np.asarray(v) for k, v in inputs.items()}
    consts = host_consts()
    h = np.ascontiguousarray(inp["x"], dtype=np.float32)
    for layers in LAUNCH_GROUPS:
        nc, _ = build_program(SEQ_PER_CORE, layers)
        in_maps = [core_inputs(inp, list(range(cid * SEQ_PER_CORE, (cid + 1) * SEQ_PER_CORE)), layers,
                               x_override=h, consts=consts) for cid in range(N_CORES)]
        res = run_bass_kernel_spmd(nc, in_maps, core_ids=list(range(N_CORES)))
        h = np.concatenate([np.asarray(res.results[cid]["out"]) for cid in range(N_CORES)], axis=0)
    return h.astype(np.float32)
```

```python
import numpy as np
from contextlib import ExitStack
import concourse.bass as bass
import concourse.mybir as mybir
from concourse.bass_utils import run_bass_kernel_spmd

F32 = mybir.dt.float32
BF16 = mybir.dt.bfloat16
AF = mybir.ActivationFunctionType
ALU = mybir.AluOpType
AX = mybir.AxisListType

D = 1024
S = 2048
NT = 16
DEPTH = 4
NE = 16
CAP = 256
PLE = 256
N_CORES = 8
SEQ_PER_CORE = 4
GELU = AF.Gelu_apprx_tanh
BIG = 1.0e9
DBG_ATT = 9
SCORE_DEPTH = {1: 2, 2: 3}


class _Op:
    __slots__ = ("eng", "fn", "deps", "raw", "dsem", "waits", "semid", "semval", "marked")

    def __init__(self, eng, fn, deps, dsem, raw=()):
        self.eng = eng
        self.fn = fn
        self.deps = deps
        self.raw = raw
        self.dsem = dsem
        self.waits = []
        self.semid = None
        self.semval = 0
        self.marked = False


class Prog:
    EPOCH_E = 24000
    EPOCH_D = 1500
    SAME_ENGINE_SYNC = True
    ENGS = ("pe", "act", "dve", "pool")

    def __init__(self):
        self.ops = []
        self.lastw = {}
        self.readers = {}
        self.last_on = {}
        self.pending = {}

    def barrier(self):
        front = set(self.last_on.get(e) for e in self.ENGS if self.last_on.get(e) is not None)
        for e in self.ENGS + ("sp",):
            self.pending[e] = set(front) | self.pending.get(e, set())

    def add(self, eng, fn, r=(), w=(), dsem=None):
        deps = set()
        soft = set()
        for k in r:
            d = self.lastw.get(k)
            if d is not None:
                deps.add(d)
            if isinstance(k, tuple) and k[0] == "ps":
                soft.update(self.readers.get(k, ()))
        for k in w:
            d = self.lastw.get(k)
            if d is not None:
                deps.add(d)
            deps.update(self.readers.get(k, ()))
        pend = self.pending.get(eng)
        if pend:
            deps.update(pend)
            self.pending[eng] = set()
        idx = len(self.ops)
        soft = set(d for d in soft if self.ops[d].eng != eng) - deps
        self.ops.append(_Op(eng, fn, deps | soft, dsem, soft))
        for k in r:
            self.readers.setdefault(k, set()).add(idx)
        for k in w:
            self.lastw[k] = idx
            self.readers[k] = set()
        if dsem is None:
            self.last_on[eng] = idx
        return idx

    def finalize(self):
        ops = self.ops
        for i, op in enumerate(ops):
            best = {}
            for d in op.deps:
                src = ops[d]
                if src.dsem is not None:
                    key = ("d", src.dsem)
                else:
                    if src.eng == op.eng and (op.eng == "pe" or not self.SAME_ENGINE_SYNC):
                        continue
                    key = ("e", src.eng)
                if key not in best or best[key] < d:
                    best[key] = d
            op.waits = sorted(best.values())
            for d in op.waits:
                ops[d].marked = True
        cnt = {}
        semids = set()
        for op in ops:
            if op.dsem is not None:
                k = ("d", op.dsem)
                c = cnt.get(k, 0)
                cnt[k] = c + 1
                op.semid = (k, c // self.EPOCH_D)
                op.semval = (c % self.EPOCH_D + 1) * 16
                op.marked = True
                semids.add(op.semid)
            elif op.marked:
                k = ("e", op.eng)
                c = cnt.get(k, 0)
                cnt[k] = c + 1
                op.semid = (k, c // self.EPOCH_E)
                op.semval = c % self.EPOCH_E + 1
                semids.add(op.semid)
        return sorted(semids, key=str)

    def emit(self, nc, stack, final_waits=()):
        semids = self.finalize()
        sems = {}
        for n, sid in enumerate(semids):
            sems[sid] = stack.enter_context(nc.semaphore("s%d" % n))
        ops = self.ops
        block = stack.enter_context(nc.Block())

        def run(engname, e):
            waited = {}
            for op in ops:
                if op.eng != engname:
                    continue
                for d in op.waits:
                    src = ops[d]
                    if waited.get(src.semid, 0) >= src.semval:
                        continue
                    waited[src.semid] = src.semval
                    e.wait_ge(sems[src.semid], src.semval)
                ins = op.fn(e)
                if op.marked:
                    ins.then_inc(sems[op.semid], 16 if op.dsem is not None else 1)
            if engname == "sp":
                for d in final_waits:
                    src = ops[d]
                    e.wait_ge(sems[src.semid], src.semval)

        @block.tensor
        def _(e):
            run("pe", e)

        @block.scalar
        def _(e):
            run("act", e)

        @block.vector
        def _(e):
            run("dve", e)

        @block.gpsimd
        def _(e):
            run("pool", e)

        @block.sync
        def _(e):
            run("sp", e)


def natten_win(m, kt):
    q = np.arange(128)
    k = np.arange(128)
    r = 2 * m + q // 64
    qc = q % 64
    rs = np.clip(r - 4, 0, 24)
    cs = np.clip(qc - 8, 0, 48)
    kr = 2 * kt + k // 64
    kc = k % 64
    rowok = (kr[:, None] >= rs[None, :]) & (kr[:, None] < rs[None, :] + 8)
    colok = (kc[:, None] >= cs[None, :]) & (kc[:, None] < cs[None, :] + 16)
    return (rowok & colok).astype(np.float32)


def c_key_tiles(m):
    if m <= 1:
        return [0, 1, 2, 3]
    if m >= 14:
        return [12, 13, 14, 15]
    return [m - 2, m - 1, m, m + 1, m + 2]


def c_mask_classes():
    classes = []
    index = {}
    for m in range(NT):
        for kt in c_key_tiles(m):
            mk = natten_win(m, kt)
            key = mk.tobytes()
            found = None
            for ci, (kb, _) in enumerate(classes):
                if kb == key:
                    found = ci
                    break
            if found is None:
                classes.append((key, mk))
                found = len(classes) - 1
            index[(m, kt)] = found
    return np.stack([c[1] for c in classes], 0), index


C_MASKS, C_MASK_INDEX = c_mask_classes()
N_CMASK = C_MASKS.shape[0]


def host_consts():
    k = np.arange(128)[:, None]
    q = np.arange(128)[None, :]
    dist = np.zeros((128, 3, 128), np.float32)
    for i, dm in enumerate((-1, 0, 1)):
        rel = np.abs(dm * 128 + k - q).astype(np.float32)
        dist[:, i, :] = np.where(rel <= 128, rel, BIG)
    sel = np.zeros((16, 16, 128), np.float32)
    for e in range(16):
        sel[e, e, :] = 1.0
    return {
        "ident": np.eye(128, dtype=np.float32),
        "iota_row": np.broadcast_to(np.arange(256, dtype=np.float32)[None, :], (128, 256)).copy(),
        "iota_part": np.stack([np.arange(128), np.arange(128) + 128], 1).astype(np.float32),
        "sel": sel,
        "distB": dist,
        "maskC": np.ascontiguousarray(C_MASKS.transpose(1, 0, 2)),
    }


def host_gains(norm_mix_g, norm_ffn_g, ple_norm_g):
    g = np.stack([norm_mix_g, norm_ffn_g, ple_norm_g], axis=1)
    return np.ascontiguousarray(g.reshape(DEPTH, 3, 8, 128).transpose(0, 1, 3, 2))


def host_rpb_tables(rpb):
    k = np.arange(128)
    q = np.arange(128)
    out = np.zeros((4, 128, 7, 4, 128), np.float32)
    for di, dm in enumerate(range(-3, 4)):
        dr = (2 * dm + k[:, None] // 64) - (q[None, :] // 64)
        dc = (k[:, None] % 64) - (q[None, :] % 64)
        ok = (np.abs(dr) <= 7) & (np.abs(dc) <= 15)
        ri = np.clip(dr + 7, 0, 14)
        ci = np.clip(dc + 15, 0, 30)
        for h in range(16):
            tab = rpb[h][ri, ci]
            tab = np.where(ok, tab, np.float32(0.0))
            hh = h % 4
            pp = (hh % 2) * 2 + hh // 2
            out[h // 4, :, di, pp, :] = tab
    return out.reshape(4, 128, 7 * 4 * 128)


def alibi_slope(h):
    return float(np.float32(2.0 ** (-8.0 * (h + 1) / 16)))


class Ctx:
    pass


def build_program(n_seq=SEQ_PER_CORE, layers=(0, 1, 2, 3), parts=("mix", "ffn", "ple")):
    nc = bass.Bass("TRN2", target_bir_lowering=False)
    P = Prog()
    c = Ctx()
    stack = ExitStack()

    def dram(name, shape, dt=F32, kind="ExternalInput"):
        return nc.dram_tensor(name, list(shape), dt, kind=kind).ap()

    c.x = dram("x", [n_seq, S, D])
    c.out = dram("out", [n_seq, S, D], kind="ExternalOutput")
    c.gains = dram("gains", [DEPTH, 3, 128, 8])
    c.ident = dram("ident", [128, 128])
    c.iota_row_d = dram("iota_row", [128, 256])
    c.iota_part_d = dram("iota_part", [128, 2])
    c.sel_d = dram("sel", [16, 16, 128])
    c.distB_d = dram("distB", [128, 3, 128])
    c.maskC_d = dram("maskC", [128, N_CMASK, 128])
    L = {}
    for i in layers:
        d = Ctx()
        L[i] = d
        kind = i % 3
        d.kind = kind
        if "ple" in parts:
            d.p = dram("p_L%d" % i, [n_seq, S, PLE])
            d.ple_gate_w = dram("ple_gate_w_L%d" % i, [D, D])
            d.ple_proj_w = dram("ple_proj_w_L%d" % i, [PLE, D])
        if "ffn" in parts:
            d.router_w = dram("router_w_L%d" % i, [D, NE])
            d.wg = dram("exp_w_gate_L%d" % i, [NE, D, D])
            d.wu = dram("exp_w_up_L%d" % i, [NE, D, D])
            d.wd = dram("exp_w_down_L%d" % i, [NE, D, D])
        if "mix" in parts:
            d.w_out = dram("w_out_L%d" % i, [D, D])
            if kind == 0:
                d.w_in = dram("w_in_L%d" % i, [D, 2048])
                d.vnorm_g = dram("a_vnorm_g_L%d" % i, [1, D])
                d.w_sT = dram("a_w_sT_L%d" % i, [8, 128, 128])
                d.b_s = dram("a_b_s_L%d" % i, [1, D])
            elif kind == 1:
                d.w_in = dram("w_in_L%d" % i, [D, 1536])
                d.qk_g = dram("qk_g_L%d" % i, [128, 2])
                d.sink = dram("b_sink_L%d" % i, [1, 16])
            else:
                d.w_in = dram("w_in_L%d" % i, [D, 3072])
                d.qk_g = dram("qk_g_L%d" % i, [128, 2])
                d.rpbt = dram("c_rpbt_L%d" % i, [4, 128, 7 * 4 * 128])

    def sb(name, shape, dt):
        return stack.enter_context(nc.sbuf_tensor(name, list(shape), dt))

    def ps(name, shape, dt):
        return stack.enter_context(nc.psum_tensor(name, list(shape), dt))

    c.h = sb("h", [128, NT, D], F32)
    c.R1 = sb("R1", [128, 8 * S], BF16)
    c.R2 = sb("R2", [128, 8 * S], BF16)
    c.W = [sb("W%d" % i, [128, 8, 512], BF16) for i in range(6)]
    c.R3 = sb("R3", [128, 10240], BF16)
    c.gains_sb = sb("gains_sb", [128, DEPTH * 3 * 8], F32)
    c.ident_f = sb("ident_f", [128, 128], F32)
    c.ident_b = sb("ident_b", [128, 128], BF16)
    c.iota_row = sb("iota_row_sb", [128, 256], F32)
    c.iota_part = sb("iota_part_sb", [128, 2], F32)
    c.sel = sb("sel_sb", [16, 16, 128], BF16)
    c.ones_row = sb("ones_row", [1, 128], BF16)
    c.mhalf = sb("mhalf", [128, 32], F32)
    c.eps6 = sb("eps6", [128, 1], F32)
    c.stat = sb("stat", [128, 112], F32)
    c.aff_tok = sb("aff_tok", [128, NT, NE], F32)
    c.slot_tok = sb("slot_tok", [128, NT, NE], F32)
    c.rw = sb("rw", [128, 8, NE], F32)
    c.rwg = sb("rwg", [128, 8, NE], F32)
    c.qkg = sb("qkg", [128, 4], F32)
    c.esink = sb("esink", [128, 16], F32)
    c.psb = [ps("psb%d" % i, [128, 512], F32) for i in range(8)]

    def r2(off, n, dt):
        v = c.R2[:, off // 2:(off + n) // 2]
        return v if dt == BF16 else v.bitcast(F32)

    def r3(off, n, dt, parts=128):
        v = c.R3[0:parts, off // 2:(off + n) // 2]
        return v if dt == BF16 else v.bitcast(F32)

    xnT = c.R1[:].rearrange("p (c t) -> p c t", c=8)
    hs = c.R1[:].rearrange("p (t d) -> p t d", t=NT)
    mixT = c.R2[:].rearrange("p (c t) -> p c t", c=8)

    def psbf(b):
        return c.psb[b][:].bitcast(BF16)

    def MM(out, lhsT, rhs, start, stop, r, w, skip=False):
        return P.add("pe", lambda e: e.matmul(out, lhsT=lhsT, rhs=rhs, start=start, stop=stop, skip_group_check=skip),
                     r=r, w=w)

    def TR(out, in_, ident, r, w):
        return P.add("pe", lambda e: e.transpose(out=out, in_=in_, identity=ident), r=r, w=w)

    def ACT(out, in_, func, r, w, **kw):
        return P.add("act", lambda e: e.activation(out=out, in_=in_, func=func, **kw), r=r, w=w)

    def TT(eng, out, in0, in1, op, r, w):
        return P.add(eng, lambda e: e.tensor_tensor(out=out, in0=in0, in1=in1, op=op), r=r, w=w)

    def TS(eng, out, in0, s1, s2, op0, op1, r, w):
        if op1 is None:
            return P.add(eng, lambda e: e.tensor_scalar(out=out, in0=in0, scalar1=s1, scalar2=None, op0=op0), r=r, w=w)
        return P.add(eng, lambda e: e.tensor_scalar(out=out, in0=in0, scalar1=s1, scalar2=s2, op0=op0, op1=op1), r=r, w=w)

    def STT(out, in0, scalar, in1, op0, op1, r, w):
        return P.add("dve", lambda e: e.scalar_tensor_tensor(out=out, in0=in0, scalar=scalar, in1=in1, op0=op0, op1=op1),
                     r=r, w=w)

    def CP(eng, out, in_, r, w):
        return P.add(eng, lambda e: e.tensor_copy(out=out, in_=in_), r=r, w=w)

    def DMA(eng, out, in_, r, w, dsem):
        return P.add(eng, lambda e: e.dma_start(out=out, in_=in_), r=r, w=w, dsem=dsem)

    DMA("sp", c.gains_sb[:].rearrange("p (a c) -> p a c", c=8), c.gains.rearrange("l k p c -> p (l k) c"),
        [], ["gains_sb"], "c_gains")
    DMA("sp", c.ident_f[:], c.ident, [], ["ident_f"], "c_ident")
    DMA("sp", c.iota_row[:], c.iota_row_d, [], ["iota_row"], "c_iotar")
    DMA("sp", c.iota_part[:], c.iota_part_d, [], ["iota_part"], "c_iotap")
    DMA("pool", c.sel[:], c.sel_d, [], ["sel"], "c_sel")
    CP("dve", c.ident_b[:], c.ident_f[:], ["ident_f"], ["ident_b"])
    P.add("pool", lambda e: e.memset(c.ones_row[:], 1.0), w=["ones_row"])
    P.add("pool", lambda e: e.memset(c.mhalf[:], -0.5), w=["mhalf"])
    P.add("pool", lambda e: e.memset(c.eps6[:], 1e-6), w=["eps6"])

    def gain_ap(layer, which, dc):
        o = (layer * 3 + which) * 8 + dc
        return c.gains_sb[:, o:o + 1]

    def load_w(slot, src2d, kc, n0, n=512, col0=0):
        dst = c.W[slot][:, 0:kc, col0:col0 + n]
        return DMA("pool", dst, src2d[:, n0:n0 + n].rearrange("(c p) n -> p c n", p=128), [], [("W", slot)], ("W", slot))

    def rstd_rms(dst_col0):
        junk = r3(0, 2048, BF16)
        for tt in range(NT):
            ACT(junk, c.h[:, tt, :], AF.Square, [("h", tt)], ["junk", ("ss", tt)], accum_out=c.stat[:, tt:tt + 1])
        TS("pool", c.stat[:, 16:32], c.stat[:, 0:16], 1.0 / D, 1e-6, ALU.mult, ALU.add,
           [("ss", t) for t in range(NT)], ["ms"])
        TT("pool", c.stat[:, dst_col0:dst_col0 + 16], c.stat[:, 16:32], c.mhalf[:, 0:16], ALU.pow, ["ms", "mhalf"], ["rstd"])

    def rms_to_xnT(layer, which):
        P.barrier()
        rstd_rms(32)
        hsb = r3(2048, 4096, BF16).rearrange("p (s d) -> p s d", s=2)
        for g4 in range(NT // 4):
            for j in range(4):
                tt = g4 * 4 + j
                slot = tt % 2
                TS("dve", hsb[:, slot, :], c.h[:, tt, :], c.stat[:, 32 + tt:33 + tt], None, ALU.mult, None,
                   [("h", tt), "rstd"], [("hsb", slot)])
                for dc in range(8):
                    TR(psbf(dc)[:, j * 128:(j + 1) * 128], hsb[:, slot, dc * 128:(dc + 1) * 128], c.ident_b[:],
                       [("hsb", slot), "ident_b"], [("ps", dc)])
            for dc in range(8):
                if dc % 2 == 0:
                    ACT(xnT[:, dc, g4 * 512:(g4 + 1) * 512], psbf(dc)[:, 0:512], AF.Copy,
                        [("ps", dc), "gains_sb"], [("xnT", g4)], scale=gain_ap(layer, which, dc))
                else:
                    TS("dve", xnT[:, dc, g4 * 512:(g4 + 1) * 512], psbf(dc)[:, 0:512], gain_ap(layer, which, dc), None,
                       ALU.mult, None, [("ps", dc), "gains_sb"], [("xnT", g4)])

    def ple_block(layer, seq):
        d = L[layer]
        rms_to_xnT(layer, 2)
        pbuf = r2(0, 16384, F32).rearrange("p (t k) -> p t k", k=PLE)
        pT = r2(16384, 8192, BF16).rearrange("p (k t) -> p k t", k=2)
        hsb = r3(2048, 4096, BF16).rearrange("p (s d) -> p s d", s=2)
        tmpA = r3(6144, 4096, F32).rearrange("p (s d) -> p s d", s=2)
        tmpB = r3(10240, 4096, F32).rearrange("p (s d) -> p s d", s=2)
        load_w(0, d.ple_gate_w, 8, 0)
        load_w(1, d.ple_gate_w, 8, 512)
        load_w(2, d.ple_proj_w, 2, 0)
        load_w(3, d.ple_proj_w, 2, 512)
        DMA("sp", pbuf, d.p[seq].rearrange("(t p) k -> p t k", p=128), [], ["pbuf"], "pbuf")
        for tt in range(NT):
            slot = tt % 2
            CP("dve", hsb[:, slot, 0:PLE], pbuf[:, tt, :], ["pbuf"], [("hsb", slot)])
            bank = tt % 2
            for kc in range(2):
                TR(psbf(bank)[:, kc * 128:(kc + 1) * 128], hsb[:, slot, kc * 128:(kc + 1) * 128], c.ident_b[:],
                   [("hsb", slot), "ident_b"], [("ps", bank)])
            ACT(pT[:, :, tt * 128:(tt + 1) * 128], psbf(bank)[:, 0:256].rearrange("p (k t) -> p k t", k=2), AF.Copy,
                [("ps", bank)], ["pT"])
        n = 0
        for tt in range(NT):
            for half in range(2):
                bG = 2 + (n % 3) * 2
                bP = bG + 1
                s2 = n % 2
                n += 1
                for kc in range(8):
                    MM(c.psb[bG][:], xnT[:, kc, tt * 128:(tt + 1) * 128], c.W[half][:, kc, :], kc == 0, kc == 7,
                       [("xnT", tt // 4), ("W", half)], [("ps", bG)])
                for kc in range(2):
                    MM(c.psb[bP][:], pT[:, kc, tt * 128:(tt + 1) * 128], c.W[2 + half][:, kc, :], kc == 0, kc == 1,
                       ["pT", ("W", 2 + half)], [("ps", bP)])
                ACT(tmpA[:, s2, :], c.psb[bG][:], AF.Sigmoid, [("ps", bG)], [("tmpA", s2)])
                TT("dve", tmpB[:, s2, :], tmpA[:, s2, :], c.psb[bP][:], ALU.mult, [("tmpA", s2), ("ps", bP)], [("tmpB", s2)])
                hv = c.h[:, tt, half * 512:(half + 1) * 512]
                TT("dve", hv, hv, tmpB[:, s2, :], ALU.add, [("tmpB", s2), ("h", tt)], [("h", tt)])

    def wout_full(layer):
        n = 0
        for tt in range(NT):
            for half in range(2):
                b = n % 4
                n += 1
                for kc in range(8):
                    MM(c.psb[b][:], mixT[:, kc, tt * 128:(tt + 1) * 128], c.W[4 + half][:, kc, :], kc == 0, kc == 7,
                       [("mixT", tt // 4), ("W", 4 + half)], [("ps", b)])
                hv = c.h[:, tt, half * 512:(half + 1) * 512]
                TT("dve", hv, hv, c.psb[b][:], ALU.add, [("ps", b), ("h", tt)], [("h", tt)])

    def mixer_a(layer, seq):
        d = L[layer]
        rms_to_xnT(layer, 0)
        load_w(2, d.w_in, 8, 1024)
        load_w(3, d.w_in, 8, 1536)
        load_w(0, d.w_in, 8, 0)
        load_w(1, d.w_in, 8, 512)
        load_w(4, d.w_out, 8, 0)
        load_w(5, d.w_out, 8, 512)
        vg = r3(6144, 4096, F32)
        wsT = r3(10240, 2048, BF16).rearrange("p (g t) -> p g t", g=8)
        bs = r3(12288, 2048, BF16, parts=1)
        vt = r3(14336, 4096, F32)
        vn = r3(18432, 2048, BF16)
        DMA("sp", vg, d.vnorm_g[0:1, :].to_broadcast([128, D]), [], ["vg"], "a_vg")
        DMA("pool", wsT, d.w_sT.rearrange("g s t -> s g t"), [], ["wsT"], "a_wsT")
        DMA("pool", bs, d.b_s, [], ["bs"], "a_bs")
        st6 = c.stat[:, 48:60].rearrange("p (a b) -> p a b", a=2)
        mv = c.stat[:, 60:62]
        for tt in range(NT):
            for half in range(2):
                b = half
                for kc in range(8):
                    MM(c.psb[b][:], xnT[:, kc, tt * 128:(tt + 1) * 128], c.W[2 + half][:, kc, :], kc == 0, kc == 7,
                       [("xnT", tt // 4), ("W", 2 + half)], [("ps", b)])
                ACT(vt[:, half * 512:(half + 1) * 512], c.psb[b][:], GELU, [("ps", b)], ["vt"])
                P.add("dve", lambda e, half=half: e.bn_stats(out=st6[:, half, :], in_=vt[:, half * 512:(half + 1) * 512]),
                      r=["vt"], w=["st6"])
            P.add("dve", lambda e: e.bn_aggr(out=mv, in_=c.stat[:, 48:60]), r=["st6"], w=["mv"])
            TS("pool", c.stat[:, 62:63], c.stat[:, 61:62], 1e-5, None, ALU.add, None, ["mv"], ["lnv"])
            TT("pool", c.stat[:, 63:64], c.stat[:, 62:63], c.mhalf[:, 0:1], ALU.pow, ["lnv", "mhalf"], ["lnr"])
            TS("dve", vt, vt, c.stat[:, 60:61], c.stat[:, 63:64], ALU.subtract, ALU.mult, ["vt", "mv", "lnr"], ["vt"])
            TT("dve", vn, vt, vg, ALU.mult, ["vt", "vg"], ["vn"])
            for g in range(8):
                b = 2 + g // 4
                col = (g % 4) * 128
                MM(c.psb[b][:, col:col + 128], vn[:, g * 128:(g + 1) * 128], wsT[:, g, :], True, False,
                   ["vn", "wsT"], [("ps", b)])
                MM(c.psb[b][:, col:col + 128], c.ones_row[0:1, :], bs[0:1, g * 128:(g + 1) * 128], False, True,
                   ["ones_row", "bs"], [("ps", b)])
            for b2 in range(2):
                outv = mixT[:, b2 * 4:(b2 + 1) * 4, tt * 128:(tt + 1) * 128]
                inv = c.psb[2 + b2][:].rearrange("p (g t) -> p g t", g=4)
                if b2 == 0:
                    ACT(outv, inv, AF.Copy, [("ps", 2 + b2)], [("mixT", tt // 4)])
                else:
                    CP("dve", outv, inv, [("ps", 2 + b2)], [("mixT", tt // 4)])
        P.barrier()
        gu = r3(14336, 4096, F32).rearrange("p (s d) -> p s d", s=2)
        n = 0
        for fc in range(8):
            for tq in range(4):
                b = n % 4
                s2 = n % 2
                n += 1
                for kc in range(8):
                    MM(c.psb[b][:], c.W[fc // 4][:, kc, (fc % 4) * 128:(fc % 4 + 1) * 128], xnT[:, kc, tq * 512:(tq + 1) * 512],
                       kc == 0, kc == 7, [("xnT", tq), ("W", fc // 4)], [("ps", b)])
                ACT(gu[:, s2, :], c.psb[b][:], GELU, [("ps", b)], [("gu", s2)])
                mv_ = mixT[:, fc, tq * 512:(tq + 1) * 512]
                TT("dve", mv_, mv_, gu[:, s2, :], ALU.mult, [("gu", s2), ("mixT", tq)], [("mixT", tq)])
        wout_full(layer)

    def attention(layer, seq):
        d = L[layer]
        isB = (d.kind == 1)
        sdep = SCORE_DEPTH[d.kind]
        rms_to_xnT(layer, 0)
        P.barrier()
        qT = r2(0, 8192, BF16).rearrange("p (c t) -> p c t", c=2)
        kT = r2(8192, 8192, BF16).rearrange("p (c t) -> p c t", c=2)
        V = r2(16384, 8448, BF16).rearrange("p (t h e) -> p t h e", t=NT, h=4)
        sq = r3(0, 2048, F32)
        qtok = r3(2048, 1024, BF16).rearrange("p (s d) -> p s d", s=2)
        ktok = r3(3072, 1024, BF16).rearrange("p (s d) -> p s d", s=2)
        tS = r3(4096, 6144, F32).rearrange("p (s h q) -> p s h q", s=3, h=4)
        Eb = r3(10240, 3072, BF16).rearrange("p (s h q) -> p s h q", s=3, h=4)
        PTb = r3(13312, 3072, BF16).rearrange("p (s h q) -> p s h q", s=3, h=4)
        otok = r3(16384, 1024, BF16).rearrange("p (s d) -> p s d", s=2)
        oT = r3(17408, 1024, BF16).rearrange("p (s k t) -> p s k t", s=2, k=2)
        distB = maskC = None
        b4 = c.W[4][:].rearrange("p a b -> p (a b)").bitcast(F32).rearrange("p (m h q) -> p m h q", m=4, h=4)
        b5 = c.W[5][:].rearrange("p a b -> p (a b)").bitcast(F32).rearrange("p (m h q) -> p m h q", m=4, h=4)
        if isB:
            distB = r3(18432, 1536, F32).rearrange("p (a q) -> p a q", a=3)
            DMA("sp", distB, c.distB_d, [], ["tab"], "tabB")
            DMA("sp", c.esink[:], d.sink[0:1, :].to_broadcast([128, 16]), [], ["esink"], "sink")
            ACT(c.esink[:], c.esink[:], AF.Exp, ["esink"], ["esink"])
        else:
            maskC = r2(24832, N_CMASK * 256, BF16).rearrange("p (a q) -> p a q", a=N_CMASK)
            DMA("pool", maskC, c.maskC_d, [], ["tab"], "tabC")
        DMA("sp", c.qkg[:, 0:2], d.qk_g, [], ["qkg"], "qkg")
        TS("pool", c.qkg[:, 2:3], c.qkg[:, 0:1], 0.125, None, ALU.mult, None, ["qkg"], ["qkg2"])
        P.add("pool", lambda e: e.memset(V[:, :, :, 64:65], 1.0), w=["V"])
        load_w(2, d.w_out, 8, 0)
        load_w(3, d.w_out, 8, 512)
        for gi in range(4):
            if isB:
                load_w(0, d.w_in, 8, gi * 256, n=256)
                load_w(1, d.w_in, 8, 1024 + gi * 64, n=64, col0=0)
                load_w(1, d.w_in, 8, 1280 + gi * 64, n=64, col0=64)
                for dmi in range(3):
                    for pp in range(4):
                        TS("dve", b4[:, dmi, pp, :], distB[:, dmi, :], -alibi_slope(gi * 4 + 2 * (pp % 2) + pp // 2), None,
                           ALU.mult, None, ["tab"], [("W", 4)])
            else:
                load_w(0, d.w_in, 8, gi * 256, n=256, col0=0)
                load_w(0, d.w_in, 8, 1024 + gi * 256, n=256, col0=256)
                load_w(1, d.w_in, 8, 2048 + gi * 256, n=256, col0=0)
                src = d.rpbt[gi].rearrange("p (m h q) -> p m h q", m=7, h=4)
                DMA("sp", b4, src[:, 0:4], [], [("W", 4)], ("W", 4))
                DMA("sp", b5[:, 0:3], src[:, 4:7], [], [("W", 5)], ("W", 5))
            nk = 64 if isB else 256
            nh = 4 + nk // 64
            nkp = 1 if isB else 2

            def qkv_mm(tt):
                s2 = tt % 2
                bq, bkv = s2, 2 + s2
                for kc in range(8):
                    MM(c.psb[bq][:, 0:256], xnT[:, kc, tt * 128:(tt + 1) * 128], c.W[0][:, kc, 0:256], kc == 0, kc == 7,
                       [("xnT", tt // 4), ("W", 0)], [("ps", bq)])
                if isB:
                    for kc in range(8):
                        MM(c.psb[bkv][:, 0:128], xnT[:, kc, tt * 128:(tt + 1) * 128], c.W[1][:, kc, 0:128], kc == 0, kc == 7,
                           [("xnT", tt // 4), ("W", 1)], [("ps", bkv)])
                else:
                    for kc in range(8):
                        MM(c.psb[bkv][:, 0:256], xnT[:, kc, tt * 128:(tt + 1) * 128], c.W[0][:, kc, 256:512], kc == 0, kc == 7,
                           [("xnT", tt // 4), ("W", 0)], [("ps", bkv)])
                    for kc in range(8):
                        MM(c.psb[bkv][:, 256:512], xnT[:, kc, tt * 128:(tt + 1) * 128], c.W[1][:, kc, 0:256], kc == 0, kc == 7,
                           [("xnT", tt // 4), ("W", 1)], [("ps", bkv)])

            def qkv_post(tt):
                s2 = tt % 2
                bq, bkv = s2, 2 + s2
                if isB:
                    kps = c.psb[bkv][:, 0:64]
                    vps = c.psb[bkv][:, 64:128]
                else:
                    kps = c.psb[bkv][:, 0:256]
                    vps = c.psb[bkv][:, 256:512]
                sqv = sq[:, s2 * 0:512] if False else sq
                ACT(sq[:, 0:256], c.psb[bq][:, 0:256], AF.Square, [("ps", bq)], ["sq"])
                ACT(sq[:, 256:256 + nk], kps, AF.Square, [("ps", bkv)], ["sq"])
                ssq = c.stat[:, 64:64 + nh]
                P.add("dve", lambda e: e.tensor_reduce(
                    out=ssq, in_=sq[:, 0:nh * 64].rearrange("p (h e) -> p h e", e=64), axis=AX.X, op=ALU.add),
                    r=["sq"], w=["ssq"])
                ACT(c.stat[:, 72:72 + nh], ssq, AF.Ln, ["ssq", "eps6"], ["qms"], scale=1.0 / 64, bias=c.eps6[:])
                ACT(c.stat[:, 80:80 + nh], c.stat[:, 72:72 + nh], AF.Exp, ["qms"], ["qrs"], scale=-0.5)
                TT("dve", qtok[:, s2, :].rearrange("p (h e) -> p h e", e=64),
                   c.psb[bq][:, 0:256].rearrange("p (h e) -> p h e", e=64),
                   c.stat[:, 80:84].unsqueeze(2).to_broadcast([128, 4, 64]), ALU.mult,
                   [("ps", bq), "qrs"], [("qtok", s2)])
                if isB:
                    for dup in range(2):
                        TS("dve", ktok[:, s2, dup * 64:(dup + 1) * 64], kps, c.stat[:, 84:85], None, ALU.mult, None,
                           [("ps", bkv), "qrs"], [("ktok", s2)])
                    ACT(V[:, tt, 0, 0:64], vps, AF.Copy, [("ps", bkv)], ["V"])
                else:
                    TT("dve", ktok[:, s2, :].rearrange("p (h e) -> p h e", e=64),
                       kps.rearrange("p (h e) -> p h e", e=64),
                       c.stat[:, 84:88].unsqueeze(2).to_broadcast([128, 4, 64]), ALU.mult,
                       [("ps", bkv), "qrs"], [("ktok", s2)])
                    ACT(V[:, tt, :, 0:64], vps.rearrange("p (h e) -> p h e", e=64), AF.Copy, [("ps", bkv)], ["V"])

            def qkv_tr(tt):
                s2 = tt % 2
                bt = 4 + s2
                for pr in range(2):
                    TR(psbf(bt)[:, pr * 128:(pr + 1) * 128], qtok[:, s2, pr * 128:(pr + 1) * 128], c.ident_b[:],
                       [("qtok", s2), "ident_b"], [("ps", bt)])
                for pr in range(nkp):
                    TR(psbf(bt)[:, 256 + pr * 128:256 + (pr + 1) * 128], ktok[:, s2, pr * 128:(pr + 1) * 128], c.ident_b[:],
                       [("ktok", s2), "ident_b"], [("ps", bt)])
                ACT(qT[:, :, tt * 128:(tt + 1) * 128], psbf(bt)[:, 0:256].rearrange("p (c t) -> p c t", c=2), AF.Copy,
                    [("ps", bt), "qkg2"], ["qT"], scale=c.qkg[:, 2:3])
                TS("dve", kT[:, 0:nkp, tt * 128:(tt + 1) * 128],
                   psbf(bt)[:, 256:256 + nkp * 128].rearrange("p (c t) -> p c t", c=nkp), c.qkg[:, 1:2], None, ALU.mult, None,
                   [("ps", bt), "qkg"], ["kT"])

            if DBG_ATT >= 1:
                qkv_mm(0)
                for tt in range(NT):
                    if tt + 1 < NT:
                        qkv_mm(tt + 1)
                    qkv_post(tt)
                    qkv_tr(tt)

            steps = []
            for m in range(NT if DBG_ATT >= 2 else 0):
                kts = [kt for kt in (m - 1, m, m + 1) if 0 <= kt < NT] if isB else c_key_tiles(m)
                for ki, kt in enumerate(kts):
                    steps.append((m, ki, kt, len(kts)))

            def att_scores(si):
                m, ki, kt, nkt = steps[si]
                for pp in range(4):
                    hf, j = pp // 2, pp % 2
                    bsc = (si % sdep) * 2 + hf
                    kpr = 0 if isB else j
                    MM(c.psb[bsc][:, j * 128:(j + 1) * 128],
                       kT[hf * 64:(hf + 1) * 64, kpr, kt * 128:(kt + 1) * 128],
                       qT[hf * 64:(hf + 1) * 64, j, m * 128:(m + 1) * 128], True, True,
                       ["qT", "kT"], [("ps", bsc)])

            def att_bias(si):
                m, ki, kt, nkt = steps[si]
                s3 = si % 3
                dmi = (kt - m + 1) if isB else (kt - m + 3)
                btab = (b4[:, dmi] if dmi < 4 else b5[:, dmi - 4])
                for hf in range(2):
                    bsc = (si % sdep) * 2 + hf
                    TT("dve", tS[:, s3, 2 * hf:2 * hf + 2, :],
                       c.psb[bsc][:, 0:256].rearrange("p (h q) -> p h q", h=2), btab[:, 2 * hf:2 * hf + 2, :], ALU.add,
                       [("ps", bsc), ("W", 4), ("W", 5)], [("tS", s3, hf)])

            def att_exp(si):
                m, ki, kt, nkt = steps[si]
                s3 = si % 3
                if isB:
                    ACT(PTb[:, s3], tS[:, s3], AF.Exp, [("tS", s3, 0), ("tS", s3, 1)], [("PT", s3)])
                else:
                    ACT(Eb[:, s3], tS[:, s3], AF.Exp, [("tS", s3, 0), ("tS", s3, 1)], [("E", s3)])
                    ci = C_MASK_INDEX[(m, kt)]
                    TT("dve", PTb[:, s3], Eb[:, s3], maskC[:, ci:ci + 1, :].to_broadcast([128, 4, 128]), ALU.mult,
                       [("E", s3), "tab"], [("PT", s3)])

            def att_pv(si):
                m, ki, kt, nkt = steps[si]
                s2 = si % 3
                bo = (4 + m % 2) if sdep == 2 else 6
                ops_ = c.psb[bo][:, 0:260].rearrange("p (h e) -> p h e", e=65)
                for pp in range(4 if DBG_ATT >= 3 else 0):
                    hh = 2 * (pp % 2) + pp // 2
                    vh = 0 if isB else hh
                    MM(ops_[:, hh, :], PTb[:, s2, pp, :], V[:, kt, vh, 0:65], ki == 0 and pp == 0, ki == nkt - 1,
                       [("PT", s2), "V"], [("ps", bo)], skip=True)

            def fin_a(m):
                bo = (4 + m % 2) if sdep == 2 else 6
                ops_ = c.psb[bo][:, 0:260].rearrange("p (h e) -> p h e", e=65)
                o2 = m % 2
                den = c.stat[:, 88 + 8 * o2:92 + 8 * o2]
                rden = c.stat[:, 92 + 8 * o2:96 + 8 * o2]
                if isB:
                    TT("dve", den, ops_[:, :, 64], c.esink[:, gi * 4:(gi + 1) * 4], ALU.add, [("ps", bo), "esink"], [("den", o2)])
                else:
                    CP("dve", den, ops_[:, :, 64], [("ps", bo)], [("den", o2)])
                P.add("dve", lambda e: e.reciprocal(out=rden, in_=den), r=[("den", o2)], w=[("rden", o2)])
                TT("dve", otok[:, o2, :].rearrange("p (h e) -> p h e", e=64), ops_[:, :, 0:64],
                   rden.unsqueeze(2).to_broadcast([128, 4, 64]), ALU.mult,
                   [("ps", bo), ("rden", o2)], [("otok", o2)])

            def fin_b(m):
                o2 = m % 2
                bt = 6 if sdep == 2 else 7
                for pr in range(2):
                    TR(psbf(bt)[:, pr * 128:(pr + 1) * 128], otok[:, o2, pr * 128:(pr + 1) * 128], c.ident_b[:],
                       [("otok", o2), "ident_b"], [("ps", bt)])
                ACT(oT[:, o2], psbf(bt)[:, 0:256].rearrange("p (k t) -> p k t", k=2), AF.Copy, [("ps", bt)], [("oT", o2)])

            def fin_c(m, half):
                o2 = m % 2
                bw = 7
                for k2 in range(2):
                    MM(c.psb[bw][:], oT[:, o2, k2, :], c.W[2 + half][:, gi * 2 + k2, :], k2 == 0, k2 == 1,
                       [("oT", o2), ("W", 2 + half)], [("ps", bw)])
                hv = c.h[:, m, half * 512:(half + 1) * 512]
                TT("dve", hv, hv, c.psb[bw][:], ALU.add, [("ps", bw), ("h", m)], [("h", m)])

            for si0 in range(min(2, len(steps))):
                att_scores(si0)
            if steps:
                att_bias(0)
            deferred = []
            for si in range(len(steps)):
                if si + 2 < len(steps):
                    att_scores(si + 2)
                if si + 1 < len(steps):
                    att_bias(si + 1)
                att_exp(si)
                due = [x for x in deferred if x[0] <= si]
                deferred = [x for x in deferred if x[0] > si]
                for _, fn_, args_ in due:
                    fn_(*args_)
                att_pv(si)
                m, ki, kt, nkt = steps[si]
                if ki == nkt - 1 and DBG_ATT >= 4:
                    deferred.append((si + 1, fin_a, (m,)))
                    deferred.append((si + 2, fin_b, (m,)))
                    deferred.append((si + 3, fin_c, (m, 0)))
                    deferred.append((si + 4, fin_c, (m, 1)))
            for _, fn_, args_ in deferred:
                fn_(*args_)

    def ffn(layer, seq):
        d = L[layer]
        P.barrier()
        DMA("sp", c.rw[:], d.router_w.rearrange("(c p) e -> p c e", p=128), [], ["rw"], "rw")
        o = (layer * 3 + 1) * 8
        TT("dve", c.rwg[:], c.rw[:], c.gains_sb[:, o:o + 8].unsqueeze(2).to_broadcast([128, 8, NE]), ALU.mult,
           ["rw", "gains_sb"], ["rwg"])
        wslot = {"g": (0, 1), "u": (2, 3), "d": (4, 5)}

        def load_expert(e, which, half):
            src = {"g": d.wg, "u": d.wu, "d": d.wd}[which][e]
            load_w(wslot[which][half], src, 8, half * 512)

        for which in ("g", "u", "d"):
            for half in range(2):
                load_expert(0, which, half)
        rstd_rms(32)
        hsf = r3(2048, 8192, F32).rearrange("p (s d) -> p s d", s=2)
        hsfT = r3(10240, 8192, F32).rearrange("p (s c t) -> p s c t", s=2, c=8)
        bL = 7
        for tt in range(NT):
            s2 = tt % 2
            TS("dve", hs[:, tt, :], c.h[:, tt, :], c.stat[:, 32 + tt:33 + tt], None, ALU.mult, None,
               [("h", tt), "rstd"], [("hs", tt)])
            ACT(hsf[:, s2, :], c.h[:, tt, :], AF.Copy, [("h", tt), "rstd"], [("hsf", s2)], scale=c.stat[:, 32 + tt:33 + tt])
            for dc in range(8):
                b = s2 * 2 + dc // 4
                TR(c.psb[b][:, (dc % 4) * 128:(dc % 4 + 1) * 128], hsf[:, s2, dc * 128:(dc + 1) * 128], c.ident_f[:],
                   [("hsf", s2), "ident_f"], [("ps", b)])
            for b2 in range(2):
                b = s2 * 2 + b2
                outv = hsfT[:, s2, b2 * 4:(b2 + 1) * 4, :]
                inv = c.psb[b][:].rearrange("p (c t) -> p c t", c=4)
                if b2 == 0:
                    ACT(outv, inv, AF.Copy, [("ps", b)], [("hsfT", s2)])
                else:
                    CP("dve", outv, inv, [("ps", b)], [("hsfT", s2)])
            for dc in range(8):
                MM(c.psb[bL][:, tt * 16:(tt + 1) * 16], hsfT[:, s2, dc, :], c.rwg[:, dc, :], dc == 0, dc == 7,
                   [("hsfT", s2), "rwg"], [("ps", bL)])
        lg = c.psb[bL][:, 0:256].rearrange("p (t e) -> p t e", e=NE)
        mx = c.stat[:, 0:16]
        P.add("dve", lambda e: e.tensor_reduce(out=mx, in_=lg, axis=AX.X, op=ALU.max), r=[("ps", bL)], w=["mx"])
        TT("dve", c.aff_tok[:], lg, mx.unsqueeze(2).to_broadcast([128, NT, NE]), ALU.subtract, [("ps", bL), "mx"], ["aff"])
        ACT(c.aff_tok[:], c.aff_tok[:], AF.Exp, ["aff"], ["aff"])
        sm = c.stat[:, 16:32]
        P.add("dve", lambda e: e.tensor_reduce(out=sm, in_=c.aff_tok[:], axis=AX.X, op=ALU.add), r=["aff"], w=["sm"])
        P.add("dve", lambda e: e.reciprocal(out=sm, in_=sm), r=["sm"], w=["sm"])
        TT("dve", c.aff_tok[:], c.aff_tok[:], sm.unsqueeze(2).to_broadcast([128, NT, NE]), ALU.mult, ["aff", "sm"], ["aff"])
        P.barrier()
        affT = r2(0, 8192, F32)[0:16, :]
        work = r2(8192, 8192, F32)[0:16, :]
        cum = r2(16384, 8192, F32)[0:16, :]
        slotbf = r3(0, 4096, BF16)[0:16, :]
        for q4 in range(4):
            for j in range(4):
                tt = q4 * 4 + j
                TR(c.psb[q4][0:16, j * 128:(j + 1) * 128], c.aff_tok[:, tt, :], c.ident_f[:], ["aff", "ident_f"], [("ps", q4)])
            ACT(affT[:, q4 * 512:(q4 + 1) * 512], c.psb[q4][0:16, :], AF.Copy, [("ps", q4)], ["affT"])
        CP("dve", work, affT, ["affT"], ["work"])
        m8 = c.stat[0:16, 48:56]
        for it in range(CAP // 8):
            P.add("dve", lambda e: e.max(out=m8, in_=work), r=["work"], w=["m8"])
            if it < CAP // 8 - 1:
                P.add("dve", lambda e: e.match_replace(out=work, in_to_replace=m8, in_values=work, imm_value=-1.0),
                      r=["work", "m8"], w=["work"])
        TS("dve", work, affT, c.stat[0:16, 55:56], None, ALU.is_ge, None, ["affT", "m8"], ["work"])
        P.add("dve", lambda e: e.tensor_tensor_scan(out=cum, data0=work, data1=work, initial=0.0, op0=ALU.add, op1=ALU.max),
              r=["work"], w=["cum"])
        STT(cum, cum, -513.0, work, ALU.add, ALU.mult, ["cum", "work"], ["cum"])
        TS("dve", cum, cum, 512.0, None, ALU.add, None, ["cum"], ["cum"])
        CP("dve", slotbf, cum, ["cum"], ["slotbf"])
        bS = 4
        for tt in range(NT):
            TR(c.psb[bS][:, tt * 16:(tt + 1) * 16], cum[:, tt * 128:(tt + 1) * 128], c.ident_f[0:16, 0:16],
               ["cum", "ident_f"], [("ps", bS)])
        CP("dve", c.slot_tok[:].rearrange("p t e -> p (t e)"), c.psb[bS][:, 0:256], [("ps", bS)], ["slot_tok"])
        P.barrier()
        Pm = r2(0, 8192, BF16).rearrange("p (t s) -> p t s", t=NT)
        PT = r2(8192, 8192, BF16).rearrange("p (s t) -> p s t", s=2)
        xsT = r2(16384, 4096, BF16).rearrange("p (c s) -> p c s", c=8)
        hT = r2(20480, 4096, BF16).rearrange("p (c s) -> p c s", c=8)
        Y = r2(24576, 4096, BF16).rearrange("p (s d) -> p s d", s=2)
        sg = r3(4096, 2048, F32).rearrange("p (s d) -> p s d", s=2)
        Pm2 = r3(6144, 8192, BF16).rearrange("p (t s) -> p t s", t=NT)
        Pms = (Pm, Pm2)
        def build_pm(ex):
            TT("dve", Pms[ex % 2], c.iota_row[:].unsqueeze(1).to_broadcast([128, NT, CAP]),
               c.slot_tok[:, :, ex:ex + 1].to_broadcast([128, NT, CAP]), ALU.is_equal,
               ["iota_row", "slot_tok"], [("Pm", ex % 2)])

        build_pm(0)
        for ex in range(NE):
            Pm = Pms[ex % 2]
            pmk = ("Pm", ex % 2)
            for dc in range(8):
                b = 4 + (dc // 2) % 2
                col = (dc % 2) * 256
                for tt in range(NT):
                    MM(c.psb[b][:, col:col + 256], hs[:, tt, dc * 128:(dc + 1) * 128], Pm[:, tt, :], tt == 0, tt == NT - 1,
                       [("hs", tt), pmk], [("ps", b)])
                ACT(xsT[:, dc, :], c.psb[b][:, col:col + 256], AF.Copy, [("ps", b), "gains_sb"], [("xsT", dc)],
                    scale=gain_ap(layer, 1, dc))
            for fc in range(8):
                half, fo = fc // 4, (fc % 4) * 128
                b = 6 + fc % 2
                for dc in range(8):
                    MM(c.psb[b][:, 0:256], c.W[0 + half][:, dc, fo:fo + 128], xsT[:, dc, :], dc == 0, dc == 7,
                       [("xsT", dc), ("W", 0 + half)], [("ps", b)])
                for dc in range(8):
                    MM(c.psb[b][:, 256:512], c.W[2 + half][:, dc, fo:fo + 128], xsT[:, dc, :], dc == 0, dc == 7,
                       [("xsT", dc), ("W", 2 + half)], [("ps", b)])
                s2 = fc % 2
                ACT(sg[:, s2, :], c.psb[b][:, 0:256], AF.Silu, [("ps", b)], [("sg", s2)])
                TT("dve", hT[:, fc, :], sg[:, s2, :], c.psb[b][:, 256:512], ALU.mult, [("sg", s2), ("ps", b)], [("hT", fc)])
                if fc == 3 and ex + 1 < NE:
                    load_expert(ex + 1, "g", 0)
                    load_expert(ex + 1, "u", 0)
            if ex + 1 < NE:
                load_expert(ex + 1, "g", 1)
                load_expert(ex + 1, "u", 1)
                build_pm(ex + 1)
            for q4 in range(4):
                MM(c.psb[q4][:], c.sel[:, ex, :], slotbf[:, q4 * 512:(q4 + 1) * 512], True, True,
                   ["sel", "slotbf"], [("ps", q4)])
                for st in range(2):
                    TS("dve", PT[:, st, q4 * 512:(q4 + 1) * 512], c.psb[q4][:], c.iota_part[:, st:st + 1], None,
                       ALU.is_equal, None, [("ps", q4), "iota_part"], [("PT", st, q4)])
            n = 0
            for half in range(2):
                for st in range(2):
                    b = 4 + n % 2
                    n += 1
                    for fc in range(8):
                        MM(c.psb[b][:], hT[:, fc, st * 128:(st + 1) * 128], c.W[4 + half][:, fc, :], fc == 0, fc == 7,
                           [("hT", fc), ("W", 4 + half)], [("ps", b)])
                    ACT(Y[:, st, half * 512:(half + 1) * 512], c.psb[b][:], AF.Copy, [("ps", b)], [("Y", st, half)])
                if ex + 1 < NE:
                    load_expert(ex + 1, "d", half)
            n = 0
            for tt in range(NT):
                for half in range(2):
                    b = n % 4
                    n += 1
                    for st in range(2):
                        MM(c.psb[b][:], PT[:, st, tt * 128:(tt + 1) * 128], Y[:, st, half * 512:(half + 1) * 512], st == 0, st == 1,
                           [("PT", st, tt // 4), ("Y", st, half)], [("ps", b)])
                    hv = c.h[:, tt, half * 512:(half + 1) * 512]
                    STT(hv, c.psb[b][:], c.aff_tok[:, tt, ex:ex + 1], hv, ALU.mult, ALU.add,
                        [("ps", b), "aff", ("h", tt)], [("h", tt)])

    finals = []
    for seq in range(n_seq):
        for q in range(4):
            DMA("sp", c.h[:, q * 4:(q + 1) * 4, :], c.x[seq, q * 512:(q + 1) * 512, :].rearrange("(t p) d -> p t d", p=128),
                [], [("h", q * 4 + j) for j in range(4)], ("hload", q))
        for layer in layers:
            if "mix" in parts:
                if L[layer].kind == 0:
                    mixer_a(layer, seq)
                else:
                    attention(layer, seq)
            if "ffn" in parts:
                ffn(layer, seq)
            if "ple" in parts:
                ple_block(layer, seq)
        P.barrier()
        for q in range(4):
            finals.append(DMA("sp", c.out[seq, q * 512:(q + 1) * 512, :].rearrange("(t p) d -> p t d", p=128),
                              c.h[:, q * 4:(q + 1) * 4, :], [("h", q * 4 + j) for j in range(4)], [], ("hstore", q)))

    n_ops = len(P.ops)
    P.emit(nc, stack, final_waits=finals)
    stack.close()
    return nc, n_ops


def core_inputs(inp, seqs, layers, parts=("mix", "ffn", "ple"), x_override=None, consts=None):
    f = np.float32
    im = dict(consts if consts is not None else host_consts())
    xs = inp["x"] if x_override is None else x_override
    im["x"] = np.ascontiguousarray(xs[seqs])
    im["gains"] = host_gains(inp["norm_mix_g"], inp["norm_ffn_g"], inp["ple_norm_g"])
    for i in layers:
        kind, j = i % 3, i // 3
        if "ple" in parts:
            im["p_L%d" % i] = np.ascontiguousarray(inp["p"][i][seqs])
            im["ple_gate_w_L%d" % i] = inp["ple_gate_w"][i]
            im["ple_proj_w_L%d" % i] = inp["ple_proj_w"][i]
        if "ffn" in parts:
            im["router_w_L%d" % i] = inp["router_w"][i]
            im["exp_w_gate_L%d" % i] = inp["exp_w_gate"][i]
            im["exp_w_up_L%d" % i] = inp["exp_w_up"][i]
            im["exp_w_down_L%d" % i] = inp["exp_w_down"][i]
        if "mix" in parts:
            im["w_out_L%d" % i] = inp["w_out"][i]
            if kind == 0:
                im["w_in_L%d" % i] = inp["a_w_in"][j]
                im["a_vnorm_g_L%d" % i] = np.ascontiguousarray(inp["a_vnorm_g"][j].reshape(1, D))
                im["a_w_sT_L%d" % i] = np.ascontiguousarray(inp["a_w_s"][j].transpose(0, 2, 1))
                im["a_b_s_L%d" % i] = np.ascontiguousarray(inp["a_b_s"][j].reshape(1, D))
            elif kind == 1:
                im["w_in_L%d" % i] = inp["b_w_in"][j]
                im["qk_g_L%d" % i] = np.ascontiguousarray(
                    np.stack([np.tile(inp["b_qnorm_g"][j], 2), np.tile(inp["b_knorm_g"][j], 2)], 1).astype(f))
                im["b_sink_L%d" % i] = np.ascontiguousarray(inp["b_sink"][j].reshape(1, 16))
            else:
                im["w_in_L%d" % i] = inp["c_w_in"][j]
                im["qk_g_L%d" % i] = np.ascontiguousarray(
                    np.stack([np.tile(inp["c_qnorm_g"][j], 2), np.tile(inp["c_knorm_g"][j], 2)], 1).astype(f))
                im["c_rpbt_L%d" % i] = host_rpb_tables(inp["c_rpb"][j])
    return {k: np.ascontiguousarray(v, dtype=f) for k, v in im.items()}


LAUNCH_GROUPS = ((0, 1, 2, 3),)


def kernel(**inputs):
    inp = {k: np.asarray(v) for k, v in inputs.items()}
    consts = host_consts()
    h = np.ascontiguousarray(inp["x"], dtype=np.float32)
    for layers in LAUNCH_GROUPS:
        nc, _ = build_program(SEQ_PER_CORE, layers)
        in_maps = [core_inputs(inp, list(range(cid * SEQ_PER_CORE, (cid + 1) * SEQ_PER_CORE)), layers,
                               x_override=h, consts=consts) for cid in range(N_CORES)]
        res = run_bass_kernel_spmd(nc, in_maps, core_ids=list(range(N_CORES)))
        h = np.concatenate([np.asarray(res.results[cid]["out"]) for cid in range(N_CORES)], axis=0)
    return h.astype(np.float32)
```
